# Optimizing a Trainium2 kernel written in Bass

```python
import math
import jax
import jax.numpy as jnp
from jax import lax
import numpy as np

D_MODEL = 1024
BATCH = 8
SEQ = 2048
DEPTH = 2

GRID_W = 64
CTX_LEN = 256
Q_BLOCK = 128
ROPE_BASE = 10000.0
EPS = 1e-6
N_BRANCH = 4
A_HEADS = 4
A_QK_DIM = 64
A_V_DIM = 128
B_HEADS = 8
B_HEAD_DIM = 64
NA_KH = 8
NA_KW = 16
C_HEADS = 8
C_KV_HEADS = 2
C_HEAD_DIM = 64
D_HEADS = 8
MLA_Q_LORA = 256
MLA_KV_LORA = 128
MLA_NOPE = 64
MLA_ROPE = 32
MLA_V = 64
FFN_DENSE = 2816
N_EXPERTS = 8
TOP_K = 2
FFN_EXPERT = 3584

IN_WIDTHS = (
    A_HEADS * 2 * A_QK_DIM, A_HEADS * 2 * A_QK_DIM, A_HEADS * A_V_DIM,
    B_HEADS * B_HEAD_DIM, B_HEADS * B_HEAD_DIM, B_HEADS * B_HEAD_DIM,
    C_HEADS * C_HEAD_DIM, C_KV_HEADS * C_HEAD_DIM, C_KV_HEADS * C_HEAD_DIM,
    MLA_Q_LORA, MLA_KV_LORA, MLA_ROPE,
    N_BRANCH * D_MODEL,
)
D_IN = sum(IN_WIDTHS)

kernel_name = 'hybrid_gated_dit_trunk'


def split_in(p):
    cuts = [int(v) for v in np.cumsum(IN_WIDTHS)[:-1]]
    return jnp.split(p, cuts, axis=-1)


def rmsnorm(x, g):
    xf = x.astype(jnp.float32)
    y = xf * lax.rsqrt(jnp.mean(xf * xf, axis=-1, keepdims=True) + EPS)
    return (y * g.astype(jnp.float32)).astype(x.dtype)


def to_heads(z, n_heads):
    zb, zs, zw = z.shape
    return z.reshape(zb, zs, n_heads, zw // n_heads).transpose(0, 2, 1, 3)


def from_heads(z):
    zb, zh, zs, zd = z.shape
    return z.transpose(0, 2, 1, 3).reshape(zb, zs, zh * zd)


def axial_rope_tables(seq_len, d_rot):
    t = jnp.arange(seq_len)
    row = (t // GRID_W).astype(jnp.float32)
    col = (t % GRID_W).astype(jnp.float32)
    d_ax = d_rot // 2
    inv = ROPE_BASE ** (-jnp.arange(0, d_ax, 2, dtype=jnp.float32) / d_ax)
    ang_r = row[:, None] * inv[None]
    ang_c = col[:, None] * inv[None]
    ang = jnp.concatenate([ang_r, ang_r, ang_c, ang_c], axis=-1)
    return jnp.cos(ang), jnp.sin(ang)


def _rotate_half(z):
    h = z.shape[-1] // 2
    return jnp.concatenate([-z[..., h:], z[..., :h]], axis=-1)


def apply_rope(x, cos, sin):
    d_ax = x.shape[-1] // 2
    xf = x.astype(jnp.float32)
    rot = jnp.concatenate([_rotate_half(xf[..., :d_ax]), _rotate_half(xf[..., d_ax:])], axis=-1)
    return (xf * cos + rot * sin).astype(x.dtype)


def block_attention(q, k, v, coefs, scale):
    bq, hq, mq, sq, dk = q.shape
    nblk = sq // Q_BLOCK
    qb = jnp.moveaxis(q.reshape(bq, hq, mq, nblk, Q_BLOCK, dk), 3, 0)

    def one_block(qblk):
        s = jnp.einsum('bhmqd,bhmkd->bhmqk', qblk, k, preferred_element_type=jnp.float32) * scale
        p = jax.nn.softmax(s, axis=-1)
        w = jnp.einsum('bhmqk,m->bhqk', p, coefs)
        return jnp.einsum('bhqk,bhkd->bhqd', w.astype(v.dtype), v)

    out = lax.map(one_block, qb)
    return jnp.moveaxis(out, 0, 2).reshape(bq, hq, sq, v.shape[-1])


def neighbourhood_attention(q, k, v, k_ctx, v_ctx, rpb, rows):
    bq, hq, sq, d = q.shape
    kh = min(NA_KH, rows)
    kw = NA_KW
    scale = d ** -0.5
    c_len = k_ctx.shape[2]
    qg = q.reshape(bq, hq, rows, GRID_W, d)
    kg = k.reshape(bq, hq, rows, GRID_W, d)
    vg = v.reshape(bq, hq, rows, GRID_W, d)
    r = jnp.arange(rows)
    row_start = jnp.clip(r - kh // 2, 0, rows - kh)
    col = jnp.arange(GRID_W)
    col_start = jnp.clip(col - kw // 2, 0, GRID_W - kw)
    in_win = (col[None, :] >= col_start[:, None]) & (col[None, :] < col_start[:, None] + kw)
    dc = jnp.clip(col[None, :] - col[:, None], -(kw - 1), kw - 1) + (NA_KW - 1)

    def one_row(args):
        q_row, start, r_q = args
        k_rows = lax.dynamic_slice_in_dim(kg, start, kh, axis=2)
        v_rows = lax.dynamic_slice_in_dim(vg, start, kh, axis=2)
        dr = start + jnp.arange(kh) - r_q + (NA_KH - 1)
        bias = rpb[:, dr[None, :, None], dc[:, None, :]]
        s_win = jnp.einsum('bhqd,bhkwd->bhqkw', q_row, k_rows, preferred_element_type=jnp.float32) * scale
        s_win = s_win + bias[None].astype(jnp.float32)
        s_win = jnp.where(in_win[:, None, :], s_win, -jnp.inf).reshape(bq, hq, GRID_W, kh * GRID_W)
        s_ctx = jnp.einsum('bhqd,bhcd->bhqc', q_row, k_ctx, preferred_element_type=jnp.float32) * scale
        p = jax.nn.softmax(jnp.concatenate([s_ctx, s_win], axis=-1), axis=-1)
        p_ctx = p[..., :c_len].astype(v.dtype)
        p_win = p[..., c_len:].reshape(bq, hq, GRID_W, kh, GRID_W).astype(v.dtype)
        return (jnp.einsum('bhqc,bhcd->bhqd', p_ctx, v_ctx)
                + jnp.einsum('bhqkw,bhkwd->bhqd', p_win, v_rows))

    out = lax.map(one_row, (jnp.moveaxis(qg, 2, 0), row_start, r))
    return jnp.moveaxis(out, 0, 2).reshape(bq, hq, sq, d)


def hybrid_mixer(h, hc, want_ctx, rows, rope_a, rope_c, rope_d, lam_init,
                 w_in, lam_q1, lam_k1, lam_q2, lam_k2, g_diff_sub, na_rpb,
                 g_qnorm, g_knorm, g_q_lora, w_uq, g_kv_lora, w_ukv,
                 w_br_a, w_br_b, w_br_c, w_br_d, w_out):
    f32 = jnp.float32
    ones1 = jnp.ones((1,), f32)
    aq, ak, av, bq, bk, bv, cq, ck, cv, dqa, dkva, dkr, gates = split_in(jnp.einsum('bsd,de->bse', h, w_in))
    aq_c, ak_c, av_c, bq_c, bk_c, bv_c, cq_c, ck_c, cv_c, dqa_c, dkva_c, dkr_c, gates_c = split_in(
        jnp.einsum('bsd,de->bse', hc, w_in))

    def diff_qk(z):
        zb, zs, _ = z.shape
        return z.reshape(zb, zs, A_HEADS, 2, A_QK_DIM).transpose(0, 2, 3, 1, 4)

    lam = (jnp.exp(jnp.sum(lam_q1.astype(f32) * lam_k1.astype(f32)))
           - jnp.exp(jnp.sum(lam_q2.astype(f32) * lam_k2.astype(f32))) + lam_init)
    coefs = jnp.stack([jnp.ones_like(lam), -lam])
    scale_a = A_QK_DIM ** -0.5

    def diff_out(o):
        return from_heads(rmsnorm(o, g_diff_sub) * (1.0 - lam_init))

    qa = apply_rope(diff_qk(aq), *rope_a)
    ka = apply_rope(diff_qk(ak), *rope_a)
    va = to_heads(av, A_HEADS)
    ka_c = diff_qk(ak_c)
    va_c = to_heads(av_c, A_HEADS)
    oa = diff_out(block_attention(qa, jnp.concatenate([ka_c, ka], axis=3),
                                  jnp.concatenate([va_c, va], axis=2), coefs, scale_a))

    kb_c = to_heads(bk_c, B_HEADS)
    vb_c = to_heads(bv_c, B_HEADS)
    ob = from_heads(neighbourhood_attention(to_heads(bq, B_HEADS), to_heads(bk, B_HEADS), to_heads(bv, B_HEADS),
                                            kb_c, vb_c, na_rpb, rows))

    rep = C_HEADS // C_KV_HEADS
    scale_c = C_HEAD_DIM ** -0.5
    qc = apply_rope(rmsnorm(to_heads(cq, C_HEADS), g_qnorm), *rope_c)
    kc = apply_rope(rmsnorm(to_heads(ck, C_KV_HEADS), g_knorm), *rope_c)
    vc = to_heads(cv, C_KV_HEADS)
    kc_c = rmsnorm(to_heads(ck_c, C_KV_HEADS), g_knorm)
    vc_c = to_heads(cv_c, C_KV_HEADS)
    kc_all = jnp.repeat(jnp.concatenate([kc_c, kc], axis=2), rep, axis=1)
    vc_all = jnp.repeat(jnp.concatenate([vc_c, vc], axis=2), rep, axis=1)
    oc = from_heads(block_attention(qc[:, :, None], kc_all[:, :, None], vc_all, ones1, scale_c))

    scale_d = (MLA_NOPE + MLA_ROPE) ** -0.5

    def mla_q(qa_in, rope):
        q = to_heads(jnp.einsum('bsr,re->bse', rmsnorm(qa_in, g_q_lora), w_uq), D_HEADS)
        q_nope, q_rope = q[..., :MLA_NOPE], q[..., MLA_NOPE:]
        if rope is not None:
            q_rope = apply_rope(q_rope, *rope)
        return jnp.concatenate([q_nope, q_rope], axis=-1)

    def mla_kv(kva_in, kr_in, rope):
        kv = to_heads(jnp.einsum('bsr,re->bse', rmsnorm(kva_in, g_kv_lora), w_ukv), D_HEADS)
        k_nope, v = kv[..., :MLA_NOPE], kv[..., MLA_NOPE:]
        k_rope = kr_in[:, None]
        if rope is not None:
            k_rope = apply_rope(k_rope, *rope)
        k_rope = jnp.broadcast_to(k_rope, k_nope.shape[:-1] + (MLA_ROPE,))
        return jnp.concatenate([k_nope, k_rope], axis=-1), v

    qd = mla_q(dqa, rope_d)
    kd, vd = mla_kv(dkva, dkr, rope_d)
    kd_c, vd_c = mla_kv(dkva_c, dkr_c, None)
    od = from_heads(block_attention(qd[:, :, None], jnp.concatenate([kd_c, kd], axis=2)[:, :, None],
                                    jnp.concatenate([vd_c, vd], axis=2), ones1, scale_d))

    def merge(o_a, o_b, o_c, o_d, g):
        zb, zs, _ = g.shape
        gs = jax.nn.sigmoid(g.astype(f32)).astype(o_a.dtype).reshape(zb, zs, N_BRANCH, D_MODEL)
        m = (gs[:, :, 0] * jnp.einsum('bse,ed->bsd', o_a, w_br_a)
             + gs[:, :, 1] * jnp.einsum('bse,ed->bsd', o_b, w_br_b)
             + gs[:, :, 2] * jnp.einsum('bse,ed->bsd', o_c, w_br_c)
             + gs[:, :, 3] * jnp.einsum('bse,ed->bsd', o_d, w_br_d))
        return jnp.einsum('bsd,de->bse', m, w_out)

    y = merge(oa, ob, oc, od, gates)
    if not want_ctx:
        return y, None
    oa_c = diff_out(block_attention(diff_qk(aq_c), ka_c, va_c, coefs, scale_a))
    ob_c = from_heads(block_attention(to_heads(bq_c, B_HEADS)[:, :, None], kb_c[:, :, None], vb_c, ones1,
                                      B_HEAD_DIM ** -0.5))
    qc_c = rmsnorm(to_heads(cq_c, C_HEADS), g_qnorm)
    oc_c = from_heads(block_attention(qc_c[:, :, None], jnp.repeat(kc_c, rep, axis=1)[:, :, None],
                                      jnp.repeat(vc_c, rep, axis=1), ones1, scale_c))
    od_c = from_heads(block_attention(mla_q(dqa_c, None)[:, :, None], kd_c[:, :, None], vd_c, ones1, scale_d))
    y_c = merge(oa_c, ob_c, oc_c, od_c, gates_c)
    return y, y_c


def swiglu(h, w1, w3, w2):
    a = jnp.einsum('bsd,df->bsf', h, w1)
    b = jnp.einsum('bsd,df->bsf', h, w3)
    return jnp.einsum('bsf,fd->bsd', jax.nn.silu(a) * b, w2)


def moe_swiglu(h, w_router, w1, w3, w2):
    logits = jnp.einsum('bsd,de->bse', h, w_router, preferred_element_type=jnp.float32)
    top_v, top_i = lax.top_k(logits, TOP_K)
    wts = jax.nn.softmax(top_v, axis=-1)
    combine = jnp.sum(jax.nn.one_hot(top_i, N_EXPERTS, dtype=jnp.float32) * wts[..., None], axis=-2)
    combine = combine.astype(h.dtype)
    out = jnp.zeros_like(h)
    for e in range(N_EXPERTS):
        out = out + combine[..., e:e + 1] * swiglu(h, w1[e], w3[e], w2[e])
    return out


def setup_inputs(seed: int = 0) -> dict:
    key = jax.random.key(seed)
    ks = iter(jax.random.split(key, 48))

    def nrm(shape, scale):
        return jax.random.normal(next(ks), shape, jnp.float32) * scale

    def gain(shape):
        return 1.0 + nrm(shape, 0.05)

    L = DEPTH
    n_dense = (DEPTH + 1) // 2
    n_moe = DEPTH // 2
    D = D_MODEL
    return {
        'x': nrm((BATCH, SEQ, D), 1.0),
        'c': nrm((BATCH, D), 1.0),
        'ctx': nrm((BATCH, CTX_LEN, D), 1.0),
        'c_ctx': nrm((D,), 1.0),
        'w_ada': nrm((L, D, 6 * D), 0.5 * D ** -0.5),
        'b_ada': nrm((L, 6 * D), 0.01),
        'g_mix_pre': gain((L, D)),
        'g_mix_post': gain((L, D)),
        'g_ffn_pre': gain((L, D)),
        'g_ffn_post': gain((L, D)),
        'w_in': nrm((L, D, D_IN), D ** -0.5),
        'lam_q1': nrm((L, A_QK_DIM), 0.1),
        'lam_k1': nrm((L, A_QK_DIM), 0.1),
        'lam_q2': nrm((L, A_QK_DIM), 0.1),
        'lam_k2': nrm((L, A_QK_DIM), 0.1),
        'g_diff_sub': gain((L, A_V_DIM)),
        'na_rpb': nrm((L, B_HEADS, 2 * NA_KH - 1, 2 * NA_KW - 1), 0.1),
        'g_qnorm': gain((L, C_HEAD_DIM)),
        'g_knorm': gain((L, C_HEAD_DIM)),
        'g_q_lora': gain((L, MLA_Q_LORA)),
        'w_uq': nrm((L, MLA_Q_LORA, D_HEADS * (MLA_NOPE + MLA_ROPE)), MLA_Q_LORA ** -0.5),
        'g_kv_lora': gain((L, MLA_KV_LORA)),
        'w_ukv': nrm((L, MLA_KV_LORA, D_HEADS * (MLA_NOPE + MLA_V)), MLA_KV_LORA ** -0.5),
        'w_br_a': nrm((L, A_HEADS * A_V_DIM, D), (A_HEADS * A_V_DIM) ** -0.5),
        'w_br_b': nrm((L, B_HEADS * B_HEAD_DIM, D), (B_HEADS * B_HEAD_DIM) ** -0.5),
        'w_br_c': nrm((L, C_HEADS * C_HEAD_DIM, D), (C_HEADS * C_HEAD_DIM) ** -0.5),
        'w_br_d': nrm((L, D_HEADS * MLA_V, D), (D_HEADS * MLA_V) ** -0.5),
        'w_out': nrm((L, D, D), D ** -0.5),
        'w1_dense': nrm((n_dense, D, FFN_DENSE), D ** -0.5),
        'w3_dense': nrm((n_dense, D, FFN_DENSE), D ** -0.5),
        'w2_dense': nrm((n_dense, FFN_DENSE, D), FFN_DENSE ** -0.5),
        'w_router': nrm((n_moe, D, N_EXPERTS), D ** -0.5),
        'w1_moe': nrm((n_moe, N_EXPERTS, D, FFN_EXPERT), D ** -0.5),
        'w3_moe': nrm((n_moe, N_EXPERTS, D, FFN_EXPERT), D ** -0.5),
        'w2_moe': nrm((n_moe, N_EXPERTS, FFN_EXPERT, D), FFN_EXPERT ** -0.5),
    }


def reference(x, c, ctx, c_ctx, w_ada, b_ada, g_mix_pre, g_mix_post, g_ffn_pre, g_ffn_post,
              w_in, lam_q1, lam_k1, lam_q2, lam_k2, g_diff_sub, na_rpb, g_qnorm, g_knorm,
              g_q_lora, w_uq, g_kv_lora, w_ukv, w_br_a, w_br_b, w_br_c, w_br_d, w_out,
              w1_dense, w3_dense, w2_dense, w_router, w1_moe, w3_moe, w2_moe):
    S = x.shape[1]
    rows = S // GRID_W
    rope_a = axial_rope_tables(S, A_QK_DIM)
    rope_c = axial_rope_tables(S, C_HEAD_DIM)
    rope_d = axial_rope_tables(S, MLA_ROPE)
    s_c = jax.nn.silu(c)
    s_cc = jax.nn.silu(c_ctx)

    def channel_mixer(z, i):
        j = i // 2
        if i % 2 == 0:
            return swiglu(z, w1_dense[j], w3_dense[j], w2_dense[j])
        return moe_swiglu(z, w_router[j], w1_moe[j], w3_moe[j], w2_moe[j])

    for i in range(DEPTH):
        last = i == DEPTH - 1
        mod = jnp.einsum('bd,de->be', s_c, w_ada[i]) + b_ada[i]
        sh1, sc1, g1, sh2, sc2, g2 = [m[:, None, :] for m in jnp.split(mod, 6, axis=-1)]
        mod_c = jnp.einsum('d,de->e', s_cc, w_ada[i]) + b_ada[i]
        csh1, csc1, cg1, csh2, csc2, cg2 = jnp.split(mod_c, 6, axis=-1)
        lam_init = 0.8 - 0.6 * math.exp(-0.3 * i)

        h = rmsnorm(x, g_mix_pre[i]) * (1.0 + sc1) + sh1
        hc = rmsnorm(ctx, g_mix_pre[i]) * (1.0 + csc1) + csh1
        y, y_c = hybrid_mixer(h, hc, not last, rows, rope_a, rope_c, rope_d, lam_init,
                              w_in[i], lam_q1[i], lam_k1[i], lam_q2[i], lam_k2[i], g_diff_sub[i], na_rpb[i],
                              g_qnorm[i], g_knorm[i], g_q_lora[i], w_uq[i], g_kv_lora[i], w_ukv[i],
                              w_br_a[i], w_br_b[i], w_br_c[i], w_br_d[i], w_out[i])
        x = x + g1 * rmsnorm(y, g_mix_post[i])
        h = rmsnorm(x, g_ffn_pre[i]) * (1.0 + sc2) + sh2
        x = x + g2 * rmsnorm(channel_mixer(h, i), g_ffn_post[i])
        if not last:
            ctx = ctx + cg1 * rmsnorm(y_c, g_mix_post[i])
            hc = rmsnorm(ctx, g_ffn_pre[i]) * (1.0 + csc2) + csh2
            ctx = ctx + cg2 * rmsnorm(channel_mixer(hc, i), g_ffn_post[i])
    return x
```

```python
import contextlib
import math
import numpy as np
import concourse.bass as bass
import concourse.mybir as mybir
from concourse.bass_utils import run_bass_kernel_spmd

F32 = mybir.dt.float32
BF16 = mybir.dt.bfloat16
I32 = mybir.dt.int32
AF = mybir.ActivationFunctionType
ALU = mybir.AluOpType
AX = mybir.AxisListType

D = 1024
S = 2048
C = 256
T = S + C
NT = T // 128
DEPTH = 2
D_IN = 8352
EPS = 1e-6
FFN_DENSE = 2816
N_EXPERTS = 8
FFN_EXPERT = 3584
O_AQ, O_AK, O_AV = 0, 512, 1024
O_BQ, O_BK, O_BV = 1536, 2048, 2560
O_CQ, O_CK, O_CV = 3072, 3584, 3712
O_DQA, O_DKVA, O_DKR = 3840, 4096, 4224
O_G = 4256
TBLK = [(0, 256), (256, 512), (768, 512), (1280, 512), (1792, 512)]


class Buf:
    __slots__ = ("w", "r")

    def __init__(self):
        self.w = None
        self.r = {}


class Stream:
    def __init__(self, name, sem):
        self.name = name
        self.sem = sem
        self.count = 0
        self.waited = {}
        self.ops = []


class FW:
    def __init__(self, nc, es):
        self.nc = nc
        self.es = es
        self.st = {}
        for n in ("pe", "act", "dve", "pool", "sp"):
            self.st[n] = Stream(n, es.enter_context(nc.semaphore("c_" + n)))
        self.dpool = {}
        for q, n in (("sp", 12), ("pool", 8), ("act", 4)):
            self.dpool[q] = [[es.enter_context(nc.semaphore(f"d_{q}{i}")), 0] for i in range(n)]
        self.dnext = {"sp": 0, "pool": 0, "act": 0}
        self.nbuf = 0
        self.frozen = False
        self.stop = None

    def mark(self, name):
        if self.stop is not None and name == self.stop:
            self.frozen = True

    def _wait(self, s, ev):
        sem, val = ev
        k = id(sem)
        if s.waited.get(k, 0) < val:
            s.waited[k] = val
            s.ops.append(("w", sem, val))

    def _deps(self, s, reads, writes, pe_acc=False):
        for b in reads:
            if b.w is not None:
                self._wait(s, b.w)
        for b in writes:
            if b.w is not None and not (pe_acc and b.w[0] is s.sem):
                self._wait(s, b.w)
            for ev in b.r.values():
                self._wait(s, ev)

    def op(self, eng, fn, reads=(), writes=(), inc=True, pe_acc=False):
        if self.frozen:
            return
        s = self.st[eng]
        self._deps(s, reads, writes, pe_acc)
        ev = (s.sem, s.count + 1)
        if inc:
            s.count += 1
        s.ops.append(("i", fn, inc))
        for b in writes:
            b.w = ev
            b.r = {}
        for b in reads:
            b.r[id(s.sem)] = ev

    def dma(self, q, out, in_, reads=(), writes=(), **kw):
        if self.frozen:
            return
        s = self.st[q]
        pool = self.dpool[q]
        i = self.dnext[q]
        self.dnext[q] = (i + 1) % len(pool)
        ent = pool[i]
        sem = ent[0]
        if ent[1] > 0:
            self._wait(s, (sem, ent[1]))
        self._deps(s, reads, writes)
        ent[1] += 16
        ev = (sem, ent[1])
        s.ops.append(("d", out, in_, sem, kw))
        for b in writes:
            b.w = ev
            b.r = {}
        for b in reads:
            b.r[id(sem)] = ev
        return ev

    def all_events(self):
        evs = []
        for n, s in self.st.items():
            if s.count > 0:
                evs.append((s.sem, s.count))
        for q, pool in self.dpool.items():
            for sem, val in pool:
                if val > 0:
                    evs.append((sem, val))
        return evs

    def barrier(self, only=None):
        if self.frozen:
            return
        evs = self.all_events()
        for n, s in self.st.items():
            if only is not None and n not in only:
                continue
            for ev in evs:
                if ev[0] is s.sem:
                    continue
                self._wait(s, ev)

    def emit(self):
        nc = self.nc
        hmap = {"pe": "tensor", "act": "scalar", "dve": "vector", "pool": "gpsimd", "sp": "sync"}
        with nc.Block() as block:
            for n, s in self.st.items():
                def body(e, s=s):
                    for o in s.ops:
                        if o[0] == "w":
                            e.wait_ge(o[1], o[2])
                        elif o[0] == "i":
                            ins = o[1](e)
                            if o[2]:
                                ins.then_inc(s.sem, 1)
                        else:
                            e.dma_start(out=o[1], in_=o[2], **o[4]).then_inc(o[3], 16)
                getattr(block, hmap[n])(body)


class K:
    pass


def build_program(dbg=None):
    nc = bass.Bass("TRN2", target_bir_lowering=False)
    es = contextlib.ExitStack()
    with es:
        _build(nc, es, dbg)
    return nc


def _dram_inputs(nc):
    L = DEPTH
    specs = {
        "x": [S, D], "ctx": [C, D], "cvec": [2, D],
        "w_ada": [L, D, 6 * D], "b_ada": [L, 6 * D],
        "g_mix_pre": [L, D], "g_mix_post": [L, D], "g_ffn_pre": [L, D], "g_ffn_post": [L, D],
        "w_in": [L, D, D_IN],
        "lam_q1": [L, 64], "lam_k1": [L, 64], "lam_q2": [L, 64], "lam_k2": [L, 64],
        "g_diff_sub": [L, 128], "na_rpb": [L, 8, 15, 31],
        "g_qnorm": [L, 64], "g_knorm": [L, 64], "g_q_lora": [L, 256],
        "w_uq": [L, 256, 768], "g_kv_lora": [L, 128], "w_ukv": [L, 128, 1024],
        "w_br_a": [L, 512, D], "w_br_b": [L, 512, D], "w_br_c": [L, 512, D], "w_br_d": [L, 512, D],
        "w_out": [L, D, D],
        "w1_dense": [1, D, FFN_DENSE], "w3_dense": [1, D, FFN_DENSE], "w2_dense": [1, FFN_DENSE, D],
        "w_router": [1, D, N_EXPERTS],
        "w1_moe": [1, N_EXPERTS, D, FFN_EXPERT], "w3_moe": [1, N_EXPERTS, D, FFN_EXPERT],
        "w2_moe": [1, N_EXPERTS, FFN_EXPERT, D],
    }
    return {k: nc.dram_tensor(k, v, F32, kind="ExternalInput").ap() for k, v in specs.items()}


def _build(nc, es, dbg):
    fw = FW(nc, es)
    if dbg is not None:
        fw.stop = dbg.get("stop")
    I = _dram_inputs(nc)
    out = nc.dram_tensor("out", [S, D], F32, kind="ExternalOutput").ap()
    dbg_out = None
    if dbg is not None:
        dbg_out = nc.dram_tensor("dbg", list(dbg["shape"]), F32, kind="ExternalOutput").ap()
    Xd = nc.dram_tensor("Xres", [T, D], F32).ap()

    def sb(name, shape, dt):
        return es.enter_context(nc.sbuf_tensor(name, list(shape), dt))

    PSall = es.enter_context(nc.psum_tensor("psall", [128, 4096], F32))
    PS = [PSall[:, i * 512:(i + 1) * 512] for i in range(8)]
    PSB = [Buf() for _ in range(8)]

    ident = sb("ident", [128, 128], F32)
    ones_f = sb("ones_f", [128, 128], F32)
    cb = Buf()
    iot = sb("iot", [128, 128], F32)
    fw.op("pool", lambda e: e.iota(iot[:], [[1, 128]], base=0, channel_multiplier=-1,
                                  allow_small_or_imprecise_dtypes=True), writes=[cb])
    fw.op("dve", lambda e: e.tensor_single_scalar(out=ident[:], in_=iot[:], scalar=0.0, op=ALU.is_equal),
          reads=[cb], writes=[cb])
    fw.op("dve", lambda e: e.memset(ones_f[:], 1.0), writes=[cb])
    fw.mark("const0")

    colv = sb("colv", [128, 8, 8], F32)
    gb = sb("gb", [128, 4, D], F32)
    sTb = sb("sTb", [128, 8, 2, 128], BF16)
    sTf = sb("sTf", [128, 16], F32)
    cv_row = sb("cv_row", [48, 128], F32)
    modc = sb("modc", [128, 48, 2], F32)
    badc = sb("badc", [128, 48], F32)
    gcol = sb("gcol", [128, 4, 8], F32)
    XX = sb("XX", [128, 4, D], F32)
    xt = [XX[:, i, :] for i in range(2)]
    xtb = [Buf(), Buf()]
    xn = [XX[:, 2 + i, :] for i in range(2)]
    xnb = [Buf(), Buf()]
    stat = sb("stat", [128, 64], F32)
    statb = Buf()
    hT = sb("hT", [128, 8, T], BF16)
    hTb = [Buf() for _ in range(NT)]
    wt = [sb(f"wt{i}", [128, 8, 512], BF16) for i in range(3)]
    wtb = [Buf() for _ in range(3)]
    wcnt = [0]

    mb = Buf()

    def load_w(src, ncols, krows=1024):
        i = wcnt[0] % 3
        wcnt[0] += 1
        nk = krows // 128
        fw.dma("pool", wt[i][:, 0:nk, 0:ncols], src.rearrange("(k p) c -> p k c", p=128), writes=[wtb[i]])
        return wt[i], wtb[i]

    def to_cols(src_rows_ap, nrows, dst, ps_i=0):
        fw.dma("sp", cv_row[0:nrows, :], src_rows_ap, writes=[mb])
        fw.op("pe", lambda e: e.transpose(PS[ps_i][:, 0:nrows], cv_row[0:nrows, :], ident[0:nrows, 0:nrows]),
              reads=[mb, cb], writes=[PSB[ps_i]])
        fw.op("dve", lambda e: e.tensor_copy(out=dst, in_=PS[ps_i][:, 0:nrows]), reads=[PSB[ps_i]], writes=[mb])

    to_cols(I["cvec"].rearrange("j (k d) -> (j k) d", d=128), 16, sTf[:, 0:16])
    fw.op("act", lambda e: e.activation(out=sTf[:, 0:16], in_=sTf[:, 0:16], func=AF.Silu), reads=[mb], writes=[mb])
    for j in range(2):
        for kc in range(8):
            fw.op("dve", lambda e, j=j, kc=kc: e.tensor_scalar(
                out=sTb[:, kc, j, :], in0=ones_f[:, :], scalar1=sTf[:, j * 8 + kc:j * 8 + kc + 1], scalar2=None,
                op0=ALU.mult), reads=[mb, cb], writes=[mb])

    fw.mark("stb")

    def layer_vectors(li):
        to_cols(I["b_ada"][li].rearrange("(r d) -> r d", d=128), 48, badc[:, :])
        for gi, nm in enumerate(("g_mix_pre", "g_mix_post", "g_ffn_pre", "g_ffn_post")):
            to_cols(I[nm][li].rearrange("(r d) -> r d", d=128), 8, gcol[:, gi, :])
        for piece in range(12):
            w, wb = load_w(I["w_ada"][li, :, piece * 512:(piece + 1) * 512], 512)
            for q in range(4):
                ech = piece * 4 + q
                for kc in range(8):
                    fw.op("pe", lambda e, q=q, kc=kc, ech=ech, w=w: e.matmul(
                        PS[1][:, ech * 2:ech * 2 + 2], w[:, kc, q * 128:(q + 1) * 128], sTb[:, kc, :, 0],
                        start=(kc == 0), stop=(kc == 7)),
                        reads=[wb, mb], writes=[PSB[1]], inc=(kc == 7 and q == 3), pe_acc=True)
            part = piece // 2
            if part in (2, 5):
                half = piece % 2
                for j in range(2):
                    pj = 2 + j
                    for kc in range(8):
                        fw.op("pe", lambda e, kc=kc, j=j, pj=pj, w=w: e.matmul(
                            PS[pj][:, :], sTb[:, kc, j, :], w[:, kc, :], start=(kc == 0), stop=(kc == 7)),
                            reads=[wb, mb], writes=[PSB[pj]], inc=(kc == 7), pe_acc=True)
                    idx = (0 if part == 2 else 2) + j
                    bsel = j
                    fw.dma("sp", xt[bsel][:, 0:512],
                           I["b_ada"][li:li + 1, piece * 512:(piece + 1) * 512].broadcast_to([128, 512]),
                           writes=[xtb[bsel]])
                    gname = "g_mix_post" if part == 2 else "g_ffn_post"
                    fw.dma("sp", xt[bsel][:, 512:1024],
                           I[gname][li:li + 1, half * 512:(half + 1) * 512].broadcast_to([128, 512]),
                           writes=[xtb[bsel]])
                    fw.op("dve", lambda e, pj=pj, bsel=bsel: e.tensor_tensor(
                        out=xn[bsel][:, 0:512], in0=PS[pj][:, :], in1=xt[bsel][:, 0:512], op=ALU.add),
                        reads=[PSB[pj], xtb[bsel]], writes=[xnb[bsel]])
                    fw.op("dve", lambda e, idx=idx, half=half, bsel=bsel: e.tensor_tensor(
                        out=gb[:, idx, half * 512:(half + 1) * 512], in0=xn[bsel][:, 0:512],
                        in1=xt[bsel][:, 512:1024], op=ALU.mult),
                        reads=[xnb[bsel], xtb[bsel]], writes=[mb])
        fw.op("dve", lambda e: e.tensor_copy(out=modc[:].rearrange("p a j -> p (a j)"), in_=PS[1][:, 0:96]),
              reads=[PSB[1]], writes=[mb])
        for j in range(2):
            for h, (gi, sci, shi) in enumerate(((0, 1, 0), (2, 4, 3))):
                setA = h * 4 + j * 2
                fw.op("dve", lambda e, j=j, sci=sci, setA=setA: e.scalar_tensor_tensor(
                    out=colv[:, setA, :], in0=modc[:, sci * 8:(sci + 1) * 8, j], scalar=1.0,
                    in1=badc[:, sci * 8:(sci + 1) * 8], op0=ALU.add, op1=ALU.add), reads=[mb], writes=[mb])
                fw.op("dve", lambda e, gi=gi, setA=setA: e.tensor_tensor(
                    out=colv[:, setA, :], in0=colv[:, setA, :], in1=gcol[:, gi, :], op=ALU.mult),
                    reads=[mb], writes=[mb])
                fw.op("dve", lambda e, j=j, shi=shi, setA=setA: e.tensor_tensor(
                    out=colv[:, setA + 1, :], in0=modc[:, shi * 8:(shi + 1) * 8, j],
                    in1=badc[:, shi * 8:(shi + 1) * 8], op=ALU.add), reads=[mb], writes=[mb])

    def norm_phase(li, which, src_ap_fn, tiles):
        for n, tt in enumerate(tiles):
            b = n % 2
            isctx = tt < 2
            fw.dma("sp", xt[b][:], src_ap_fn(tt), writes=[xtb[b]])
            fw.op("act", lambda e, b=b, tt=tt: e.activation(out=xn[b][:], in_=xt[b][:], func=AF.Square,
                                                          accum_out=stat[:, 0:1]),
                  reads=[xtb[b]], writes=[xnb[b], statb])
            fw.op("act", lambda e: e.activation(out=stat[:, 1:2], in_=stat[:, 0:1], func=AF.Ln,
                                                scale=1.0 / D, bias=epsc[:, 0:1]), reads=[statb, cb], writes=[statb])
            fw.op("act", lambda e: e.activation(out=stat[:, 2:3], in_=stat[:, 1:2], func=AF.Exp, scale=-0.5),
                  reads=[statb], writes=[statb])
            fw.op("dve", lambda e, b=b: e.tensor_scalar(out=xn[b][:], in0=xt[b][:], scalar1=stat[:, 2:3],
                                                       scalar2=None, op0=ALU.mult),
                  reads=[xtb[b], statb], writes=[xnb[b]])
            setA = which * 4 + (2 if isctx else 0)
            for half in range(2):
                pb = 6 + half
                for q in range(4):
                    kc = half * 4 + q
                    fw.op("pe", lambda e, b=b, kc=kc, q=q, pb=pb: e.transpose(
                        PS[pb][:, q * 128:(q + 1) * 128], xn[b][:, kc * 128:(kc + 1) * 128], ident[:]),
                        reads=[xnb[b], cb], writes=[PSB[pb]], inc=(q == 3), pe_acc=True)
                for q in range(4):
                    kc = half * 4 + q
                    if q % 2 == 0:
                        fw.op("act", lambda e, kc=kc, q=q, pb=pb, tt=tt, setA=setA: e.activation(
                            out=hT[:, kc, tt * 128:(tt + 1) * 128], in_=PS[pb][:, q * 128:(q + 1) * 128],
                            func=AF.Identity, scale=colv[:, setA, kc:kc + 1], bias=colv[:, setA + 1, kc:kc + 1]),
                            reads=[PSB[pb], mb], writes=[hTb[tt]])
                    else:
                        fw.op("dve", lambda e, kc=kc, q=q, pb=pb, tt=tt, setA=setA: e.tensor_scalar(
                            out=hT[:, kc, tt * 128:(tt + 1) * 128], in0=PS[pb][:, q * 128:(q + 1) * 128],
                            scalar1=colv[:, setA, kc:kc + 1], scalar2=colv[:, setA + 1, kc:kc + 1],
                            op0=ALU.mult, op1=ALU.add),
                            reads=[PSB[pb], mb], writes=[hTb[tt]])

    epsc = sb("epsc", [128, 1], F32)
    fw.op("dve", lambda e: e.memset(epsc[:], EPS), writes=[cb])

    def x_src0(tt):
        return I["ctx"][tt * 128:(tt + 1) * 128, :] if tt < 2 else I["x"][(tt - 2) * 128:(tt - 1) * 128, :]


    ARENA_ELEMS = 44200
    AR = sb("arena", [128, ARENA_ELEMS], BF16)
    aoff = [0]

    def carve(nelem_bf16):
        o = aoff[0]
        aoff[0] += nelem_bf16
        assert aoff[0] <= ARENA_ELEMS, aoff[0]
        return AR[:, o:o + nelem_bf16]

    QT = carve(4 * T).rearrange("p (a t) -> p a t", a=4)
    KT = carve(4 * T).rearrange("p (a t) -> p a t", a=4)
    VA = carve(NT * 520).rearrange("p (a t) -> p a t", a=NT)
    OTs1 = carve(4 * 512).rearrange("p (a t) -> p a t", a=4)
    ON = carve(4 * 512).rearrange("p (a t) -> p a t", a=4)
    OFr = carve(2048)
    OF = OFr.bitcast(F32).rearrange("p (m q d) -> p m q d", m=2, q=4)
    TB2r = AR[:, aoff[0]:aoff[0] + 8192]
    TB2 = TB2r.rearrange("p (i h q) -> p i h q", i=16, h=8)
    TM = [carve(1024).bitcast(F32) for i in range(4)]
    RS = [carve(1024).bitcast(F32) for i in range(2)]
    SQ = [carve(512) for i in range(2)]
    RAW = [carve(512) for i in range(2)]
    ETp = [carve(1024) for i in range(2)]
    ET = [ETp[0][:, 0:512], ETp[0][:, 512:1024], ETp[1][:, 0:512], ETp[1][:, 512:1024]]
    KRr = AR[:, 4 * T * 2 + NT * 520 + 2048: 4 * T * 2 + NT * 520 + 2048 + 4096]
    XXb = XX[:].rearrange("p a d -> p (a d)").bitcast(BF16)
    DQ = XXb[:, 0:2 * T].rearrange("p (a t) -> p a t", a=2)
    DKV = XXb[:, 2 * T:3 * T]
    QTb = [Buf() for _ in TBLK]
    KTb = [Buf() for _ in TBLK]
    VAb = [Buf() for _ in range(NT)]
    ETb = [Buf() for _ in range(4)]
    ecnt = [0]
    RAWb = [Buf(), Buf()]
    SQb = [Buf(), Buf()]
    RSb = [Buf(), Buf()]
    TMb = [Buf() for _ in range(4)]
    rcnt = [0]
    ONb = Buf()
    OFb = Buf()
    OTs = [OTs1]
    OTsb = [Buf()]
    otcnt = [0]
    OTd = nc.dram_tensor("OTd", [16, 128, T], BF16).ap()
    identb = sb("identb", [128, 128], BF16)
    onesb = sb("onesb", [128, 128], BF16)
    bd64 = sb("bd64", [128, 128], BF16)
    pm16 = sb("pm16", [128, 128], BF16)
    pm8 = sb("pm8", [128, 128], BF16)
    cos16 = sb("cos16", [128, S], BF16)
    sin16 = sb("sin16", [128, S], BF16)
    cos8 = sb("cos8", [128, S], BF16)
    sin8 = sb("sin8", [128, S], BF16)
    pcol = sb("pcol", [128, 16], F32)
    pcoli = sb("pcoli", [128, 8], I32)
    mcoli = XX[:, 0, 384:512].bitcast(I32)
    mcolf = XX[:, 0, 256:384]
    mtmp = XX[:, 0, 0:128]
    mtmp2 = XX[:, 0, 128:256]
    brv = sb("brv", [128, 16], F32)
    brb = Buf()
    lamrow = TM[0][:, 0:256].rearrange("p (a d) -> p a d", a=4)
    psA = [0]
    psX = [0]

    def ps_main():
        i = psA[0] % 4
        psA[0] += 1
        return i

    def ps_aux():
        i = 4 + psX[0] % 3
        psX[0] += 1
        return i

    fw.op("dve", lambda e: e.tensor_copy(out=identb[:], in_=ident[:]), reads=[cb], writes=[cb])
    fw.op("dve", lambda e: e.memset(onesb[:], 1.0), writes=[cb])
    fw.op("dve", lambda e: e.memset(bd64[:], 0.0), writes=[cb])
    fw.op("dve", lambda e: e.memset(bd64[0:64, 0:64], 1.0), writes=[cb])
    fw.op("dve", lambda e: e.memset(bd64[64:128, 64:128], 1.0), writes=[cb])
    fw.op("pool", lambda e: e.iota(mcoli[:], [[1, 128]], base=0, channel_multiplier=0), writes=[cb])
    fw.op("pool", lambda e: e.iota(pcoli[:, 0:1], [[0, 1]], base=0, channel_multiplier=1), writes=[cb])

    def build_pm(pm, h):
        fw.op("dve", lambda e: e.tensor_single_scalar(out=mcoli[:], in_=mcoli[:], scalar=h, op=ALU.bitwise_and),
              reads=[cb], writes=[cb])
        fw.op("dve", lambda e: e.tensor_copy(out=mcolf[:], in_=mcoli[:]), reads=[cb], writes=[cb])
        fw.op("dve", lambda e: e.tensor_single_scalar(out=mcolf[:], in_=mcolf[:], scalar=0.5, op=ALU.is_gt),
              reads=[cb], writes=[cb])
        fw.op("dve", lambda e: e.tensor_single_scalar(out=mtmp[:], in_=iot[:], scalar=float(h), op=ALU.is_equal),
              reads=[cb], writes=[cb])
        fw.op("dve", lambda e: e.tensor_tensor(out=mtmp[:], in0=mtmp[:], in1=mcolf[:], op=ALU.mult),
              reads=[cb], writes=[cb])
        fw.op("dve", lambda e: e.tensor_single_scalar(out=mtmp2[:], in_=iot[:], scalar=float(-h), op=ALU.is_equal),
              reads=[cb], writes=[cb])
        fw.op("dve", lambda e: e.tensor_scalar(out=mcolf[:], in0=mcolf[:], scalar1=-1.0, scalar2=1.0,
                                               op0=ALU.mult, op1=ALU.add), reads=[cb], writes=[cb])
        fw.op("dve", lambda e: e.tensor_tensor(out=mtmp2[:], in0=mtmp2[:], in1=mcolf[:], op=ALU.mult),
              reads=[cb], writes=[cb])
        fw.op("dve", lambda e: e.tensor_tensor(out=pm[:], in0=mtmp[:], in1=mtmp2[:], op=ALU.subtract),
              reads=[cb], writes=[cb])
        fw.op("pool", lambda e: e.iota(mcoli[:], [[1, 128]], base=0, channel_multiplier=0), reads=[cb], writes=[cb])

    fw.mark("cmat")
    build_pm(pm16, 16)
    build_pm(pm8, 8)
    fw.mark("pm")

    def build_rope(cosT, sinT, h):
        f32v = AR[:, 0:4 * T].bitcast(F32)
        f32w = AR[:, 4 * T:8 * T].bitcast(F32)
        Rr, Cc = f32v[:, 0:S], f32v[:, S:2 * S]
        U, Fr = f32w[:, 0:S], f32w[:, S:2 * S]
        Ui = AR[:, 8 * T:8 * T + 2 * S].bitcast(I32)
        fw.op("dve", lambda e: e.tensor_single_scalar(out=pcoli[:, 1:2], in_=pcoli[:, 0:1], scalar=h - 1,
                                                      op=ALU.bitwise_and), reads=[cb], writes=[cb])
        fw.op("dve", lambda e: e.tensor_single_scalar(out=pcoli[:, 2:3], in_=pcoli[:, 0:1], scalar=2 * h,
                                                      op=ALU.bitwise_and), reads=[cb], writes=[cb])
        fw.op("dve", lambda e: e.tensor_single_scalar(out=pcoli[:, 3:4], in_=pcoli[:, 0:1], scalar=h,
                                                      op=ALU.bitwise_and), reads=[cb], writes=[cb])
        fw.op("dve", lambda e: e.tensor_copy(out=pcol[:, 1:4], in_=pcoli[:, 1:4]), reads=[cb], writes=[cb])
        fw.op("act", lambda e: e.activation(out=pcol[:, 4:5], in_=pcol[:, 1:2], func=AF.Exp,
                                            scale=-math.log(10000.0) / h), reads=[cb], writes=[cb])
        fw.op("dve", lambda e: e.tensor_single_scalar(out=pcol[:, 5:6], in_=pcol[:, 2:3], scalar=0.5, op=ALU.is_gt),
              reads=[cb], writes=[cb])
        fw.op("dve", lambda e: e.tensor_scalar(out=pcol[:, 6:7], in0=pcol[:, 3:4], scalar1=0.5, scalar2=2.0,
                                               op0=ALU.is_gt, op1=ALU.mult), reads=[cb], writes=[cb])
        fw.op("dve", lambda e: e.tensor_scalar(out=pcol[:, 6:7], in0=pcol[:, 6:7], scalar1=-1.0, scalar2=None,
                                               op0=ALU.add), reads=[cb], writes=[cb])
        fw.op("pool", lambda e: e.iota(Rr, [[1, 32], [0, 64]], base=0, channel_multiplier=0,
                                      allow_small_or_imprecise_dtypes=True), reads=[cb], writes=[cb])
        fw.op("pool", lambda e: e.iota(Cc, [[0, 32], [1, 64]], base=0, channel_multiplier=0,
                                      allow_small_or_imprecise_dtypes=True), reads=[cb], writes=[cb])
        fw.op("dve", lambda e: e.tensor_tensor(out=Cc, in0=Cc, in1=Rr, op=ALU.subtract), reads=[cb], writes=[cb])
        fw.op("dve", lambda e: e.scalar_tensor_tensor(out=Rr, in0=Cc, scalar=pcol[:, 5:6], in1=Rr,
                                                      op0=ALU.mult, op1=ALU.add), reads=[cb], writes=[cb])
        for which, dst, off in ((0, sinT, 0.5), (1, cosT, 0.75)):
            fw.op("dve", lambda e: e.tensor_scalar(out=U, in0=Rr, scalar1=pcol[:, 4:5], scalar2=1.0 / (2 * math.pi),
                                                   op0=ALU.mult, op1=ALU.mult), reads=[cb], writes=[cb])
            fw.op("dve", lambda e, off=off: e.tensor_scalar(out=U, in0=U, scalar1=off, scalar2=None, op0=ALU.add),
                  reads=[cb], writes=[cb])
            fw.op("dve", lambda e: e.tensor_copy(out=Ui, in_=U), reads=[cb], writes=[cb])
            fw.op("dve", lambda e: e.tensor_copy(out=Fr, in_=Ui), reads=[cb], writes=[cb])
            fw.op("dve", lambda e: e.tensor_tensor(out=Fr, in0=U, in1=Fr, op=ALU.subtract), reads=[cb], writes=[cb])
            fw.op("dve", lambda e: e.tensor_single_scalar(out=U, in_=Fr, scalar=0.0, op=ALU.is_lt),
                  reads=[cb], writes=[cb])
            fw.op("dve", lambda e: e.tensor_tensor(out=Fr, in0=Fr, in1=U, op=ALU.add), reads=[cb], writes=[cb])
            fw.op("dve", lambda e: e.tensor_single_scalar(out=U, in_=Fr, scalar=1.0, op=ALU.is_ge),
                  reads=[cb], writes=[cb])
            fw.op("dve", lambda e: e.tensor_tensor(out=Fr, in0=Fr, in1=U, op=ALU.subtract), reads=[cb], writes=[cb])
            fw.op("dve", lambda e: e.tensor_scalar(out=Fr, in0=Fr, scalar1=-0.5, scalar2=2 * math.pi * (1 - 1e-6),
                                                   op0=ALU.add, op1=ALU.mult), reads=[cb], writes=[cb])
            fw.op("act", lambda e, dst=dst: e.activation(out=dst[:], in_=Fr, func=AF.Sin),
                  reads=[cb], writes=[cb])

    build_rope(cos16, sin16, 16)
    build_rope(cos8, sin8, 8)
    fw.barrier()
    fw.mark("rope")

    def evac_copy(n, dst, src, reads, writes):
        if n % 2 == 0:
            fw.op("act", lambda e: e.activation(out=dst, in_=src, func=AF.Copy), reads=reads, writes=writes)
        else:
            fw.op("dve", lambda e: e.tensor_copy(out=dst, in_=src), reads=reads, writes=writes)

    def blocks_for(li):
        return list(range(5))

    def proj_fm(li, col0, nchunks, post, tbs, wsrc=None, krows=1024, rhs_fn=None, rbufs_fn=None, group=1):
        c = 0
        while c < nchunks:
            nload = min(4, nchunks - c)
            src = (wsrc if wsrc is not None else I["w_in"][li])[:, col0 + c * 128: col0 + (c + nload) * 128]
            w, wb = load_w(src, nload * 128, krows)
            nk = krows // 128
            for g0 in range(0, nload, group):
                for tb in tbs:
                    t0, tn = TBLK[tb]
                    pis = []
                    for gi in range(group):
                        cc = g0 + gi
                        pi = ps_main()
                        pis.append(pi)
                        for kc in range(nk):
                            rhs = rhs_fn(kc, t0, tn) if rhs_fn else hT[:, kc, t0:t0 + tn]
                            rb = rbufs_fn(tb) if rbufs_fn else [hTb[t0 // 128 + q] for q in range(tn // 128)]
                            fw.op("pe", lambda e, pi=pi, cc=cc, kc=kc, rhs=rhs, tn=tn, w=w, nk=nk: e.matmul(
                                PS[pi][:, 0:tn], w[:, kc, cc * 128:(cc + 1) * 128], rhs,
                                start=(kc == 0), stop=(kc == nk - 1)),
                                reads=[wb] + rb, writes=[PSB[pi]], inc=(kc == nk - 1), pe_acc=True)
                    post(c + g0, tb, pis)
            c += nload

    def post_plain(dst, dstb):
        cnt = [0]

        def f(ci, tb, pis):
            t0, tn = TBLK[tb]
            cnt[0] += 1
            evac_copy(cnt[0], dst[:, ci, t0:t0 + tn], PS[pis[0]][:, 0:tn], [PSB[pis[0]]], [dstb[tb]])
        return f

    def rope_apply(src_ps, src_psb, raw_i, dst, dstb, t0, tn, cosT, sinT, pm, prange=(0, 128)):
        p0, p1 = prange
        s0 = t0 - C
        pa = ps_aux()
        fw.op("pe", lambda e: e.matmul(PS[pa][p0:p1, 0:tn], pm[p0:p1, p0:p1], RAW[raw_i][p0:p1, 0:tn],
                                       start=True, stop=True),
              reads=[RAWb[raw_i], cb], writes=[PSB[pa]])
        a, b = rcnt[0] % 4, (rcnt[0] + 1) % 4
        rcnt[0] += 2
        if src_ps is not None:
            fw.op("dve", lambda e: e.tensor_tensor(out=TM[a][p0:p1, 0:tn], in0=src_ps[p0:p1, 0:tn],
                                                   in1=cosT[p0:p1, s0:s0 + tn], op=ALU.mult),
                  reads=[src_psb, cb], writes=[TMb[a]])
        else:
            fw.op("pool", lambda e: e.tensor_tensor(out=TM[a][p0:p1, 0:tn], in0=RAW[raw_i][p0:p1, 0:tn],
                                                    in1=cosT[p0:p1, s0:s0 + tn], op=ALU.mult),
                  reads=[RAWb[raw_i], cb], writes=[TMb[a]])
        fw.op("dve", lambda e: e.tensor_tensor(out=TM[b][p0:p1, 0:tn], in0=PS[pa][p0:p1, 0:tn],
                                               in1=sinT[p0:p1, s0:s0 + tn], op=ALU.mult),
              reads=[PSB[pa], cb], writes=[TMb[b]])
        fw.op("pool", lambda e: e.tensor_tensor(out=dst[p0:p1], in0=TM[a][p0:p1, 0:tn], in1=TM[b][p0:p1, 0:tn],
                                                op=ALU.add),
              reads=[TMb[a], TMb[b]], writes=[dstb])

    def post_rope(dst, dstb, cosT, sinT, pm):
        cnt = [0]

        def f(ci, tb, pis):
            t0, tn = TBLK[tb]
            pi = pis[0]
            if tb == 0:
                cnt[0] += 1
                evac_copy(cnt[0], dst[:, ci, t0:t0 + tn], PS[pi][:, 0:tn], [PSB[pi]], [dstb[tb]])
                return
            r = cnt[0] % 2
            cnt[0] += 1
            fw.op("act", lambda e: e.activation(out=RAW[r][:, 0:tn], in_=PS[pi][:, 0:tn], func=AF.Copy),
                  reads=[PSB[pi]], writes=[RAWb[r]])
            rope_apply(None, None, r, dst[:, ci, t0:t0 + tn], dstb[tb], t0, tn, cosT, sinT, pm)
        return f

    def post_norm(dst, dstb, redmat, cnt_feat, gcol_fn, rope=None, dst_idx=None):
        cnt = [0]

        def f(ci, tb, pis):
            t0, tn = TBLK[tb]
            pa = ps_aux()
            sqs = []
            for n, pi in enumerate(pis):
                r = cnt[0] % 2
                cnt[0] += 1
                sqs.append(r)
                fw.op("act", lambda e, r=r, pi=pi: e.activation(out=SQ[r][:, 0:tn], in_=PS[pi][:, 0:tn],
                                                                func=AF.Square),
                      reads=[PSB[pi]], writes=[SQb[r]])
            for n, r in enumerate(sqs):
                fw.op("pe", lambda e, r=r, n=n: e.matmul(PS[pa][:, 0:tn], redmat[:, :], SQ[r][:, 0:tn],
                                                         start=(n == 0), stop=(n == len(sqs) - 1)),
                      reads=[SQb[r], cb], writes=[PSB[pa]], inc=(n == len(sqs) - 1), pe_acc=True)
            rs = cnt[0] % 2
            fw.op("act", lambda e: e.activation(out=RS[rs][:, 0:tn], in_=PS[pa][:, 0:tn], func=AF.Ln,
                                                scale=1.0 / cnt_feat, bias=epsc[:, 0:1]),
                  reads=[PSB[pa], cb], writes=[RSb[rs]])
            fw.op("act", lambda e: e.activation(out=RS[rs][:, 0:tn], in_=RS[rs][:, 0:tn], func=AF.Exp, scale=-0.5),
                  reads=[RSb[rs]], writes=[RSb[rs]])
            for n, pi in enumerate(pis):
                cidx = (ci + n) if dst_idx is None else dst_idx(ci + n)
                g = gcol_fn(ci + n)
                if rope is None or tb == 0:
                    fw.op("dve", lambda e, pi=pi, cidx=cidx, g=g: e.scalar_tensor_tensor(
                        out=dst[:, cidx, t0:t0 + tn], in0=PS[pi][:, 0:tn], scalar=g, in1=RS[rs][:, 0:tn],
                        op0=ALU.mult, op1=ALU.mult), reads=[PSB[pi], RSb[rs], brb], writes=[dstb[tb]])
                else:
                    r = cnt[0] % 2
                    cnt[0] += 1
                    fw.op("dve", lambda e, pi=pi, r=r, g=g: e.scalar_tensor_tensor(
                        out=RAW[r][:, 0:tn], in0=PS[pi][:, 0:tn], scalar=g, in1=RS[rs][:, 0:tn],
                        op0=ALU.mult, op1=ALU.mult), reads=[PSB[pi], RSb[rs], brb], writes=[RAWb[r]])
                    cosT, sinT, pm = rope
                    rope_apply(None, None, r, dst[:, cidx, t0:t0 + tn], dstb[tb], t0, tn, cosT, sinT, pm)
        return f

    def proj_v(li, col0, nheads, dv, wsrc=None):
        ncols = nheads * dv
        src = (wsrc if wsrc is not None else I["w_in"][li])[:, col0:col0 + ncols]
        w, wb = load_w(src, ncols)
        for tt in range(NT):
            pi = ps_main()
            for kc in range(8):
                fw.op("pe", lambda e, pi=pi, kc=kc, tt=tt: e.matmul(
                    PS[pi][:, 0:ncols], hT[:, kc, tt * 128:(tt + 1) * 128], w[:, kc, 0:ncols],
                    start=(kc == 0), stop=(kc == 7)),
                    reads=[wb, hTb[tt]], writes=[PSB[pi]], inc=(kc == 7), pe_acc=True)
            va = VA[:, tt, 0:nheads * (dv + 1)].rearrange("p (h d) -> p h d", d=dv + 1)
            evac_copy(tt, va[:, :, 0:dv], PS[pi][:, 0:ncols].rearrange("p (h d) -> p h d", d=dv),
                      [PSB[pi]], [VAb[tt]])
            fw.op("pool", lambda e, va=va: e.memset(va[:, :, dv:dv + 1], 1.0), writes=[VAb[tt]])

    pvset = [0]

    def attn_head(steps, qap_fn, kap_fn, vcol, dv, scale, q0, nq, kcs, finish, vap_fn=None):
        nqt = nq // 128
        pset = pvset[0] % 2
        pvset[0] += 1
        pvb = [4 + 2 * pset, 5 + 2 * pset]
        W = dv + 1
        for n, kc in enumerate(kcs):
            steps.append(dict(k=kap_fn(kc), q=qap_fn(q0, nq), nq=nq, scale=scale, nqt=nqt, pvb=pvb, W=W,
                              v=(vap_fn(kc) if vap_fn else VA[:, kc, vcol:vcol + W]), first=(n == 0),
                              last=(n == len(kcs) - 1), finish=finish, mask=None))

    def run_steps(steps, look=3):
        def qk(st):
            si = ecnt[0] % 4
            ecnt[0] += 1
            st["si"] = si
            fw.op("pe", lambda e: e.matmul(PS[si][:, 0:st["nq"]], st["k"], st["q"], start=True, stop=True),
                  reads=[], writes=[PSB[si]])

        def rest(st):
            si = st["si"]
            nq, nqt, W, pvb = st["nq"], st["nqt"], st["W"], st["pvb"]
            fw.op("act", lambda e: e.activation(out=ET[si][:, 0:nq], in_=PS[si][:, 0:nq], func=AF.Exp,
                                                scale=st["scale"]),
                  reads=[PSB[si]], writes=[ETb[si]])
            for qt in range(nqt):
                bk = pvb[qt // 2]
                col = (qt % 2) * W
                stf = st["first"] and (qt % 2 == 0)
                last = st["last"]
                fw.op("pe", lambda e, bk=bk, col=col, qt=qt, stf=stf, last=last: e.matmul(
                    PS[bk][:, col:col + W], ET[si][:, qt * 128:(qt + 1) * 128], st["v"],
                    start=stf, stop=last, skip_group_check=True),
                    reads=[ETb[si]], writes=[PSB[bk]], inc=(last and (qt == nqt - 1 or qt % 2 == 1)), pe_acc=True)
            if st["last"]:
                st["finish"](pvb, nqt, W)

        n = len(steps)
        look = 4
        for i in range(min(look, n)):
            qk(steps[i])
        for i in range(0, n, 2):
            rest(steps[i])
            if i + 1 < n:
                rest(steps[i + 1])
            for j in (i + look, i + look + 1):
                if j < n:
                    qk(steps[j])

    def interleave(a_, b_):
        o_ = []
        for x_, y_ in zip(a_, b_):
            o_ += [x_, y_]
        return o_

    def finish_plain(h, dv):
        def f(pvb, nqt, W):
            for half in range((nqt + 1) // 2):
                bk = pvb[half]
                nq2 = min(2, nqt - half * 2)
                acc = PS[bk][:, 0:nq2 * W].rearrange("p (q w) -> p q w", w=W)
                fw.op("dve", lambda e, acc=acc, nq2=nq2: e.reciprocal(out=stat[:, 8:8 + nq2], in_=acc[:, :, dv]),
                      reads=[PSB[bk]], writes=[statb])
                fw.op("dve", lambda e, acc=acc, nq2=nq2, half=half: e.tensor_tensor(
                    out=ON[:, half * 2:half * 2 + nq2, h * dv:(h + 1) * dv], in0=acc[:, :, 0:dv],
                    in1=stat[:, 8:8 + nq2].unsqueeze(2).broadcast_to([128, nq2, dv]), op=ALU.mult),
                    reads=[PSB[bk], statb], writes=[ONb])
        return f

    def flush_o(branch, q0, nq, scale_col=None, ecs=(0, 1, 2, 3)):
        nqt = nq // 128
        si = 0
        for ec in ecs:
            pb = 0
            psb16 = PS[pb][:].bitcast(BF16)
            for qt in range(nqt):
                fw.op("pe", lambda e, qt=qt, ec=ec: e.transpose(psb16[:, qt * 128:(qt + 1) * 128],
                                                               ON[:, qt, ec * 128:(ec + 1) * 128], identb[:]),
                      reads=[ONb, cb], writes=[PSB[pb]], inc=(qt == nqt - 1), pe_acc=True)
            if scale_col is None:
                evac_copy(ec, OTs[si][:, ec, 0:nq], psb16[:, 0:nq], [PSB[pb]], [OTsb[si]])
            else:
                fw.op("dve", lambda e, ec=ec: e.tensor_scalar(out=OTs[si][:, ec, 0:nq], in0=psb16[:, 0:nq],
                                                             scalar1=scale_col, scalar2=None, op0=ALU.mult),
                      reads=[PSB[pb], brb], writes=[OTsb[si]])
        fw.dma("sp", OTd[branch * 4 + ecs[0]:branch * 4 + ecs[-1] + 1, :, q0:q0 + nq].rearrange("c p q -> p c q"),
               OTs[si][:, ecs[0]:ecs[-1] + 1, 0:nq], reads=[OTsb[si]])

    ALLK = list(range(NT))
    CTXK = [0, 1]

    def qblocks(li):
        return [0, 1, 2, 3, 4] if li == 0 else [1, 2, 3, 4]

    def branch_a(li):
        lam_init = 0.8 - 0.6 * math.exp(-0.3 * li)
        for n, nm in enumerate(("lam_q1", "lam_k1", "lam_q2", "lam_k2")):
            fw.dma("sp", lamrow[:, n, :], I[nm][li:li + 1, :].broadcast_to([128, 64]), writes=[brb])
        fw.dma("sp", brv[:, 5:6], I["g_diff_sub"][li:li + 1, :].rearrange("o d -> d o"), writes=[brb],
               allow_slow_non_contiguous=True)
        for n in range(2):
            fw.op("dve", lambda e, n=n: e.tensor_tensor(out=lamrow[:, 2 * n, :], in0=lamrow[:, 2 * n, :],
                                                       in1=lamrow[:, 2 * n + 1, :], op=ALU.mult),
                  reads=[brb], writes=[brb])
            fw.op("dve", lambda e, n=n: e.reduce_sum(out=stat[:, 20 + n:21 + n], in_=lamrow[:, 2 * n, :],
                                                    axis=AX.X), reads=[brb], writes=[brb])
        fw.op("act", lambda e: e.activation(out=stat[:, 20:22], in_=stat[:, 20:22], func=AF.Exp),
              reads=[brb], writes=[brb])
        fw.op("dve", lambda e: e.tensor_tensor(out=stat[:, 22:23], in0=stat[:, 21:22], in1=stat[:, 20:21],
                                               op=ALU.subtract), reads=[brb], writes=[brb])
        fw.op("dve", lambda e: e.tensor_scalar(out=brv[:, 6:7], in0=stat[:, 22:23], scalar1=-lam_init,
                                               scalar2=None, op0=ALU.add), reads=[brb], writes=[brb])
        fw.op("dve", lambda e: e.tensor_scalar(out=brv[:, 5:6], in0=brv[:, 5:6], scalar1=1.0 - lam_init,
                                               scalar2=None, op0=ALU.mult), reads=[brb], writes=[brb])
        fw.barrier()
        fw.mark("a_lam")
        proj_fm(li, O_AQ, 4, post_rope(QT, QTb, cos16, sin16, pm16), qblocks(li))
        fw.barrier()
        fw.mark("a_q")
        proj_fm(li, O_AK, 4, post_rope(KT, KTb, cos16, sin16, pm16), range(5))
        proj_v(li, O_AV, 4, 128)
        fw.barrier()
        fw.mark("a_kv")

        def finish_a(h, m):
            def f(pvb, nqt, W):
                for half in range((nqt + 1) // 2):
                    bk = pvb[half]
                    nq2 = min(2, nqt - half * 2)
                    acc = PS[bk][:, 0:nq2 * W].rearrange("p (q w) -> p q w", w=W)
                    fw.op("dve", lambda e, acc=acc, nq2=nq2: e.reciprocal(out=stat[:, 8:8 + nq2], in_=acc[:, :, 128]),
                          reads=[PSB[bk]], writes=[statb])
                    fw.op("dve", lambda e, acc=acc, nq2=nq2, half=half: e.tensor_tensor(
                        out=OF[:, m, half * 2:half * 2 + nq2, :], in0=acc[:, :, 0:128],
                        in1=stat[:, 8:8 + nq2].unsqueeze(2).broadcast_to([128, nq2, 128]), op=ALU.mult),
                        reads=[PSB[bk], statb], writes=[OFb])
                if m == 1:
                    fw.op("dve", lambda e: e.scalar_tensor_tensor(
                        out=OF[:, 0, 0:nqt, :], in0=OF[:, 1, 0:nqt, :], scalar=brv[:, 6:7], in1=OF[:, 0, 0:nqt, :],
                        op0=ALU.mult, op1=ALU.add), reads=[OFb, brb], writes=[OFb])
                    fw.op("pool", lambda e: e.tensor_tensor(out=OF[:, 1, 0:nqt, :], in0=OF[:, 0, 0:nqt, :],
                                                            in1=OF[:, 0, 0:nqt, :], op=ALU.mult),
                          reads=[OFb], writes=[OFb])
                    fw.op("dve", lambda e: e.reduce_sum(out=stat[:, 16:16 + nqt], in_=OF[:, 1, 0:nqt, :], axis=AX.X),
                          reads=[OFb], writes=[statb])
                    fw.op("act", lambda e: e.activation(out=stat[:, 16:16 + nqt], in_=stat[:, 16:16 + nqt],
                                                        func=AF.Ln, scale=1.0 / 128, bias=epsc[:, 0:1]),
                          reads=[statb, cb], writes=[statb])
                    fw.op("act", lambda e: e.activation(out=stat[:, 16:16 + nqt], in_=stat[:, 16:16 + nqt],
                                                        func=AF.Exp, scale=-0.5), reads=[statb], writes=[statb])
                    fw.op("dve", lambda e: e.tensor_tensor(
                        out=ON[:, 0:nqt, h * 128:(h + 1) * 128], in0=OF[:, 0, 0:nqt, :],
                        in1=stat[:, 16:16 + nqt].unsqueeze(2).broadcast_to([128, nqt, 128]), op=ALU.mult),
                        reads=[OFb, statb], writes=[ONb])
            return f

        for tb in qblocks(li):
            q0, nq = TBLK[tb]
            kcs = CTXK if tb == 0 else ALLK
            steps = []
            for h in range(4):
                sm = [[], []]
                for m in range(2):
                    attn_head(sm[m], lambda a, n, h=h, m=m: QT[m * 64:(m + 1) * 64, h, a:a + n],
                              lambda kc, h=h, m=m: KT[m * 64:(m + 1) * 64, h, kc * 128:(kc + 1) * 128],
                              h * 129, 128, 0.125, q0, nq, kcs, finish_a(h, m))
                steps += interleave(sm[0], sm[1])
            run_steps(steps)
            fw.barrier()
            fw.mark("a_att%d" % tb)
            flush_o(0, q0, nq, scale_col=brv[:, 5:6])
            fw.barrier()
            fw.mark("a_fl%d" % tb)
        fw.barrier()

    KRt = sb("KRt", [32, T], BF16)
    DQb = [Buf() for _ in TBLK]
    DKVb = [Buf() for _ in TBLK]
    KRb = Buf()

    def branch_d(li):
        fw.dma("sp", brv[:, 2:4], I["g_q_lora"][li].rearrange("(c d) -> d c", d=128), writes=[brb],
               allow_slow_non_contiguous=True)
        fw.dma("sp", brv[:, 4:5], I["g_kv_lora"][li:li + 1, :].rearrange("o d -> d o"), writes=[brb],
               allow_slow_non_contiguous=True)
        DKV3 = DKV.rearrange("p (a t) -> p a t", a=1)
        KR = KRt[:, :]
        proj_fm(li, O_DQA, 2, post_norm(DQ, DQb, onesb, 256, lambda c: brv[:, 2 + c:3 + c]), qblocks(li), group=2)
        proj_fm(li, O_DKVA, 1, post_norm(DKV3, DKVb, onesb, 128, lambda c: brv[:, 4:5]), range(5))
        w, wb = load_w(I["w_in"][li][:, O_DKR:O_DKR + 32], 32)
        for tb in range(5):
            t0, tn = TBLK[tb]
            pi = ps_main()
            for kc in range(8):
                fw.op("pe", lambda e, pi=pi, kc=kc, t0=t0, tn=tn, w=w: e.matmul(
                    PS[pi][0:32, 0:tn], w[:, kc, 0:32], hT[:, kc, t0:t0 + tn], start=(kc == 0), stop=(kc == 7)),
                    reads=[wb] + [hTb[t0 // 128 + q] for q in range(tn // 128)], writes=[PSB[pi]],
                    inc=(kc == 7), pe_acc=True)
            if tb == 0:
                fw.op("act", lambda e, pi=pi, t0=t0, tn=tn: e.activation(out=KR[0:32, t0:t0 + tn],
                                                                       in_=PS[pi][0:32, 0:tn], func=AF.Copy),
                      reads=[PSB[pi]], writes=[KRb])
            else:
                r = tb % 2
                fw.op("act", lambda e, pi=pi, r=r, tn=tn: e.activation(out=RAW[r][0:32, 0:tn], in_=PS[pi][0:32, 0:tn],
                                                                     func=AF.Copy), reads=[PSB[pi]], writes=[RAWb[r]])
                rope_apply(None, None, r, KR[:, t0:t0 + tn], KRb, t0, tn, cos8, sin8, pm8, prange=(0, 32))
        iq = wcnt[0] % 3
        wcnt[0] += 1
        wuq = wt[iq][:].rearrange("p k c -> p (k c)")[:, 0:1536].rearrange("p (k c) -> p k c", k=2)
        fw.dma("pool", wuq, I["w_uq"][li].rearrange("(k p) c -> p k c", p=128), writes=[wtb[iq]])
        ik = wcnt[0] % 3
        wcnt[0] += 1
        wukv = wt[ik][:].rearrange("p k c -> p (k c)")[:, 0:1024]
        fw.dma("pool", wukv, I["w_ukv"][li], writes=[wtb[ik]])
        wv = wukv.rearrange("p (h e) -> p h e", e=128)[:, :, 64:128]
        for tt in range(NT):
            pi = ps_main()
            fw.op("pe", lambda e, pi=pi, tt=tt: e.matmul(PS[pi][:, :], DKV[:, tt * 128:(tt + 1) * 128], wv,
                                                         start=True, stop=True),
                  reads=[wtb[ik], DKVb[0], DKVb[1], DKVb[2], DKVb[3], DKVb[4]], writes=[PSB[pi]])
            va = VA[:, tt, 0:520].rearrange("p (h d) -> p h d", d=65)
            evac_copy(tt, va[:, :, 0:64], PS[pi][:, :].rearrange("p (h d) -> p h d", d=64), [PSB[pi]], [VAb[tt]])
            fw.op("pool", lambda e, va=va: e.memset(va[:, :, 64:65], 1.0), writes=[VAb[tt]])
        sc_d = 96.0 ** -0.5
        for g in range(2):
            for hl in range(4):
                h = g * 4 + hl
                for tb in range(5):
                    t0, tn = TBLK[tb]
                    pi = ps_main()
                    fw.op("pe", lambda e, pi=pi, h=h, t0=t0, tn=tn: e.matmul(
                        PS[pi][0:64, 0:tn], wukv[:, h * 128:h * 128 + 64], DKV[:, t0:t0 + tn], start=True, stop=True),
                        reads=[wtb[ik], DKVb[tb]], writes=[PSB[pi]])
                    evac_copy(tb, KT[0:64, hl, t0:t0 + tn], PS[pi][0:64, 0:tn], [PSB[pi]], [KTb[tb]])
                    if tb not in qblocks(li):
                        continue
                    pq = ps_main()
                    for c in range(2):
                        fw.op("pe", lambda e, pq=pq, c=c, h=h, t0=t0, tn=tn: e.matmul(
                            PS[pq][0:96, 0:tn], wuq[:, c, h * 96:(h + 1) * 96], DQ[:, c, t0:t0 + tn],
                            start=(c == 0), stop=(c == 1)),
                            reads=[wtb[iq], DQb[tb]], writes=[PSB[pq]], inc=(c == 1), pe_acc=True)
                    if tb == 0:
                        evac_copy(hl, QT[0:96, hl, t0:t0 + tn], PS[pq][0:96, 0:tn], [PSB[pq]], [QTb[tb]])
                    else:
                        evac_copy(hl, QT[0:64, hl, t0:t0 + tn], PS[pq][0:64, 0:tn], [PSB[pq]], [QTb[tb]])
                        r = (hl + tb) % 2
                        fw.op("act", lambda e, pq=pq, r=r, tn=tn: e.activation(
                            out=RAW[r][64:96, 0:tn], in_=PS[pq][64:96, 0:tn], func=AF.Copy),
                            reads=[PSB[pq]], writes=[RAWb[r]])
                        pa = ps_aux()
                        fw.op("pe", lambda e, pa=pa, r=r, tn=tn: e.matmul(
                            PS[pa][64:96, 0:tn], pm8[64:96, 64:96], RAW[r][64:96, 0:tn], start=True, stop=True),
                            reads=[RAWb[r], cb], writes=[PSB[pa]])
                        a_, b_ = rcnt[0] % 4, (rcnt[0] + 1) % 4
                        rcnt[0] += 2
                        s0 = t0 - C
                        fw.op("pool", lambda e, r=r, a_=a_, s0=s0, tn=tn: e.tensor_tensor(
                            out=TM[a_][64:96, 0:tn], in0=RAW[r][64:96, 0:tn], in1=cos8[64:96, s0:s0 + tn],
                            op=ALU.mult), reads=[RAWb[r], cb], writes=[TMb[a_]])
                        fw.op("dve", lambda e, pa=pa, b_=b_, s0=s0, tn=tn: e.tensor_tensor(
                            out=TM[b_][64:96, 0:tn], in0=PS[pa][64:96, 0:tn], in1=sin8[64:96, s0:s0 + tn],
                            op=ALU.mult), reads=[PSB[pa], cb], writes=[TMb[b_]])
                        fw.op("pool", lambda e, a_=a_, b_=b_, hl=hl, t0=t0, tn=tn: e.tensor_tensor(
                            out=QT[64:96, hl, t0:t0 + tn], in0=TM[a_][64:96, 0:tn], in1=TM[b_][64:96, 0:tn],
                            op=ALU.add), reads=[TMb[a_], TMb[b_]], writes=[QTb[tb]])
            fw.barrier()
            for hl in range(4):
                fw.dma("sp", KT[64:96, hl, :], KR[0:32, :])
            fw.barrier()
            for tb in qblocks(li):
                q0, nq = TBLK[tb]
                kcs = CTXK if tb == 0 else ALLK
                steps = []
                for hl in range(4):
                    h = g * 4 + hl
                    attn_head(steps, lambda a, n, hl=hl: QT[0:96, hl, a:a + n],
                              lambda kc, hl=hl: KT[0:96, hl, kc * 128:(kc + 1) * 128],
                              h * 65, 64, sc_d, q0, nq, kcs, finish_plain(h, 64))
                run_steps(steps)
                flush_o(3, q0, nq, ecs=(2 * g, 2 * g + 1))
            fw.barrier()

    def build_tb2(li):
        rp32 = ET[0].bitcast(F32)
        rpb16 = ET[1]
        IE = ET[2]
        winf = OFr.bitcast(F32)
        fw.dma("sp", rp32[0:31, 0:120], I["na_rpb"][li].rearrange("h r c -> c (h r)"), writes=[ETb[0]],
               allow_slow_non_contiguous=True)
        fw.op("dve", lambda e: e.tensor_copy(out=rpb16[0:31, 0:120], in_=rp32[0:31, 0:120]),
              reads=[ETb[0]], writes=[ETb[1]])
        fw.op("dve", lambda e: e.tensor_single_scalar(out=IE[0:31, 0:128], in_=iot[0:31, :], scalar=48.0,
                                                      op=ALU.is_equal), reads=[cb], writes=[ETb[2]])
        fw.op("dve", lambda e: e.tensor_copy(out=pcol[:, 7:8], in_=pcoli[:, 0:1]), reads=[cb], writes=[cb])
        qc_ = winf[0:64, 0:64]
        fw.op("dve", lambda e: e.tensor_scalar(out=qc_, in0=iot[0:64, 0:64], scalar1=pcol[0:64, 7:8], scalar2=-8.0,
                                               op0=ALU.add, op1=ALU.add), reads=[cb], writes=[OFb])
        fw.op("dve", lambda e: e.tensor_scalar(out=qc_, in0=qc_, scalar1=0.0, scalar2=48.0,
                                               op0=ALU.max, op1=ALU.min), reads=[OFb], writes=[OFb])
        fw.op("dve", lambda e: e.tensor_scalar(out=qc_, in0=qc_, scalar1=pcol[0:64, 7:8], scalar2=None,
                                               op0=ALU.subtract), reads=[OFb, cb], writes=[OFb])
        m1 = winf[0:64, 64:128]
        fw.op("dve", lambda e: e.tensor_single_scalar(out=m1, in_=qc_, scalar=0.0, op=ALU.is_le),
              reads=[OFb], writes=[OFb])
        fw.op("dve", lambda e: e.tensor_single_scalar(out=qc_, in_=qc_, scalar=-16.0, op=ALU.is_gt),
              reads=[OFb], writes=[OFb])
        fw.op("dve", lambda e: e.tensor_tensor(out=m1, in0=m1, in1=qc_, op=ALU.mult), reads=[OFb], writes=[OFb])
        tbb = Buf()
        for q0 in range(0, 64, 4):
            pi = ps_main()
            for ql in range(4):
                qc = q0 + ql
                fw.op("pe", lambda e, pi=pi, ql=ql, qc=qc: e.matmul(
                    PS[pi][0:64, ql * 120:(ql + 1) * 120], IE[0:31, 63 - qc:127 - qc], rpb16[0:31, 0:120],
                    start=True, stop=True), reads=[ETb[1], ETb[2]], writes=[PSB[pi]], inc=(ql == 3), pe_acc=True)
            fw.op("act", lambda e, pi=pi, q0=q0: e.activation(
                out=TB2[0:64, 1:16, :, q0:q0 + 4].rearrange("p r h q -> p q h r"),
                in_=PS[pi][0:64, 0:480].rearrange("p (q h r) -> p q h r", q=4, h=8), func=AF.Exp),
                reads=[PSB[pi]], writes=[tbb])
        fw.op("dve", lambda e: e.tensor_tensor(
            out=TB2[0:64, 1:16, :, :].rearrange("p i h q -> p (i h) q"),
            in0=TB2[0:64, 1:16, :, :].rearrange("p i h q -> p (i h) q"),
            in1=m1.unsqueeze(1).broadcast_to([64, 120, 64]), op=ALU.mult), reads=[tbb, OFb], writes=[tbb])
        fw.op("dve", lambda e: e.memset(TB2[0:64, 0, :, :], 0.0), writes=[tbb])
        fw.op("dve", lambda e: e.memset(TB2[64:128, 15, :, :], 0.0), writes=[tbb])
        fw.dma("sp", TB2[64:128, 0:15, :, :], TB2[0:64, 1:16, :, :], reads=[tbb], writes=[tbb])

    def branch_b(li):
        proj_fm(li, O_BQ, 4, post_plain(QT, QTb), qblocks(li))
        proj_fm(li, O_BK, 4, post_plain(KT, KTb), range(5))
        proj_v(li, O_BV, 8, 64)
        fw.barrier()
        fw.mark("b_proj")
        build_tb2(li)
        fw.barrier()
        fw.mark("b_tb2")
        if li == 0:
            steps = []
            for hg in range(4):
                sm = [[], []]
                for hh in range(2):
                    h = 2 * hg + hh
                    pb = (h % 2) * 64
                    attn_head(sm[hh], lambda a, n, h=h, pb=pb: QT[pb:pb + 64, h // 2, a:a + n],
                              lambda kc, h=h, pb=pb: KT[pb:pb + 64, h // 2, kc * 128:(kc + 1) * 128],
                              h * 65, 64, 0.125, 0, 256, CTXK, finish_plain(h, 64))
                steps += interleave(sm[0], sm[1])
            run_steps(steps)
            flush_o(1, 0, 256)
            fw.barrier()
            fw.mark("b_ctx")
        items = []
        for a in range(16):
            pset = pvset[0] % 2
            pvset[0] += 1
            pvb = [4 + 2 * pset, 5 + 2 * pset]
            for rl in range(2):
                r = 2 * a + rl
                s_ = min(max(r - 4, 0), 24)
                chunks = [("c", 0), ("c", 1)] + [("w", ap) for ap in range(s_ // 2, (s_ + 7) // 2 + 1)]
                for n, (kind, idx) in enumerate(chunks):
                    items.append(dict(a=a, rl=rl, r=r, s=s_, n=n, kind=kind, idx=idx, pvb=pvb,
                                      last=(n == len(chunks) - 1)))

        def b_qk(it, i):
            bpair = ((0, 1), (2, 3))[i % 2]
            it["bpair"] = bpair
            kcol = it["idx"] * 128 if it["kind"] == "c" else C + it["idx"] * 128
            qcol = C + it["r"] * 64
            for h in range(8):
                hp = (h % 2) * 64
                bk_s = bpair[h % 2]
                g_ = h // 2
                fw.op("pe", lambda e, bk_s=bk_s, g_=g_, h=h, hp=hp, kcol=kcol, qcol=qcol: e.matmul(
                    PS[bk_s][:, g_ * 64:(g_ + 1) * 64], KT[hp:hp + 64, h // 2, kcol:kcol + 128],
                    QT[hp:hp + 64, h // 2, qcol:qcol + 64], start=True, stop=True),
                    reads=[], writes=[PSB[bk_s]], inc=(h >= 6), pe_acc=True)

        def b_rest(it, i):
            si = i % 3
            bpair = it["bpair"]
            r, s_, idx, kind, pvb = it["r"], it["s"], it["idx"], it["kind"], it["pvb"]
            po = it["rl"] * 64
            tt = idx if kind == "c" else 2 + idx
            for par in range(2):
                bk_s = bpair[par]
                fw.op("act", lambda e, si=si, bk_s=bk_s, par=par: e.activation(
                    out=ET[si][:, :].rearrange("p (g two q) -> p g two q", two=2, q=64)[:, :, par, :],
                    in_=PS[bk_s][:, 0:256].rearrange("p (g q) -> p g q", q=64), func=AF.Exp, scale=0.125),
                    reads=[PSB[bk_s]], writes=[ETb[si]])
            if kind == "w":
                dr0 = 2 * idx - r + 7
                fw.op("dve", lambda e, si=si, dr0=dr0: e.tensor_tensor(
                    out=ET[si][:, :], in0=ET[si][:, :],
                    in1=TB2[:, dr0 + 1, :, :].rearrange("p h q -> p (h q)"), op=ALU.mult),
                    reads=[ETb[si]], writes=[ETb[si]])
                if 2 * idx < s_:
                    fw.op("pool", lambda e, si=si: e.memset(ET[si][0:64, :], 0.0), writes=[ETb[si]])
                if 2 * idx + 1 >= s_ + 8:
                    fw.op("pool", lambda e, si=si: e.memset(ET[si][64:128, :], 0.0), writes=[ETb[si]])
            n, last = it["n"], it["last"]
            for h in range(8):
                bk = pvb[h // 4]
                col = (h % 4) * 65
                fw.op("pe", lambda e, si=si, h=h, bk=bk, col=col, tt=tt, n=n, last=last, po=po: e.matmul(
                    PS[bk][po:po + 64, col:col + 65], ET[si][:, h * 64:(h + 1) * 64],
                    VA[:, tt, h * 65:(h + 1) * 65], start=(n == 0 and h % 4 == 0), stop=last,
                    skip_group_check=True),
                    reads=[ETb[si]], writes=[PSB[bk]], inc=(last and h % 4 == 3), pe_acc=True)
            if last and it["rl"] == 1:
                a = it["a"]
                qt = a % 4
                for half in range(2):
                    bk = pvb[half]
                    acc = PS[bk][:, 0:260].rearrange("p (h w) -> p h w", w=65)
                    fw.op("dve", lambda e, acc=acc: e.reciprocal(out=stat[:, 8:12], in_=acc[:, :, 64]),
                          reads=[PSB[bk]], writes=[statb])
                    fw.op("dve", lambda e, acc=acc, half=half, qt=qt: e.tensor_tensor(
                        out=ON[:, qt, half * 256:(half + 1) * 256].rearrange("p (h d) -> p h d", d=64),
                        in0=acc[:, :, 0:64], in1=stat[:, 8:12].unsqueeze(2).broadcast_to([128, 4, 64]), op=ALU.mult),
                        reads=[PSB[bk], statb], writes=[ONb])
                if qt == 3:
                    flush_o(1, C + (a // 4) * 512, 512)

        b_qk(items[0], 0)
        for i, it in enumerate(items):
            will_flush = it["last"] and it["rl"] == 1 and it["a"] % 4 == 3
            if i + 1 < len(items) and not will_flush:
                b_qk(items[i + 1], i + 1)
            b_rest(it, i)
            if i + 1 < len(items) and will_flush:
                b_qk(items[i + 1], i + 1)
        fw.barrier()

    def branch_c(li):
        fw.dma("sp", brv[0:64, 0:1], I["g_qnorm"][li:li + 1, :].rearrange("o d -> d o"), writes=[brb],
               allow_slow_non_contiguous=True)
        fw.dma("sp", brv[64:128, 0:1], I["g_qnorm"][li:li + 1, :].rearrange("o d -> d o"), writes=[brb],
               allow_slow_non_contiguous=True)
        fw.dma("sp", brv[0:64, 1:2], I["g_knorm"][li:li + 1, :].rearrange("o d -> d o"), writes=[brb],
               allow_slow_non_contiguous=True)
        fw.dma("sp", brv[64:128, 1:2], I["g_knorm"][li:li + 1, :].rearrange("o d -> d o"), writes=[brb],
               allow_slow_non_contiguous=True)
        rope = (cos16, sin16, pm16)
        proj_fm(li, O_CQ, 4, post_norm(QT, QTb, bd64, 64, lambda c: brv[:, 0:1], rope), qblocks(li))
        proj_fm(li, O_CK, 1, post_norm(KT, KTb, bd64, 64, lambda c: brv[:, 1:2], rope), range(5))
        proj_v(li, O_CV, 2, 64)
        fw.barrier()
        fw.dma("sp", KT[64:128, 1, :], KT[0:64, 0, :])
        fw.dma("sp", KT[0:64, 1, :], KT[64:128, 0, :])
        fw.barrier()
        for tb in qblocks(li):
            q0, nq = TBLK[tb]
            kcs = CTXK if tb == 0 else ALLK
            steps = []
            for hg in range(4):
                sm = [[], []]
                for hh in range(2):
                    h = 2 * hg + hh
                    kvh = h // 4
                    pb = (h % 2) * 64
                    kch = 0 if kvh * 64 == pb else 1
                    attn_head(sm[hh], lambda a, n, h=h, pb=pb: QT[pb:pb + 64, h // 2, a:a + n],
                              lambda kc, kch=kch, pb=pb: KT[pb:pb + 64, kch, kc * 128:(kc + 1) * 128],
                              kvh * 65, 64, 0.125, q0, nq, kcs, finish_plain(h, 64))
                steps += interleave(sm[0], sm[1])
            run_steps(steps)
            flush_o(2, q0, nq)
        fw.barrier()

    mT = AR[:, 0:8 * T].rearrange("p (a t) -> p a t", a=8)
    mTb = [Buf() for _ in TBLK]
    OTt = [AR[:, 18432 + i * 8192:18432 + (i + 1) * 8192].rearrange("p (c q) -> p c q", c=16) for i in range(2)]
    OTtb = [Buf(), Buf()]
    wbr = [AR[:, 34816 + i * 2048:34816 + (i + 1) * 2048].rearrange("p (c q) -> p c q", c=16) for i in range(2)]
    wbrb = [Buf(), Buf()]
    SGm = [AR[:, 38912 + i * 512:38912 + (i + 1) * 512] for i in range(2)]
    SGmb = [Buf(), Buf()]
    ACCm = [AR[:, 39936 + i * 1024:39936 + (i + 1) * 1024].bitcast(F32) for i in range(2)]
    ACCmb = [Buf(), Buf()]
    TMPm = AR[:, 41984:43008].bitcast(F32)
    TMPmb = Buf()
    wout = AR[:, 18432:18432 + 8192].rearrange("p (k c) -> p k c", k=8)
    woutb = Buf()

    def merge_phase(li):
        tbs = qblocks(li)
        brn = ("w_br_a", "w_br_b", "w_br_c", "w_br_d")
        cnt = 0
        acn = 0
        for jp in range(4):
            wgs = []
            for jl in range(2):
                j = 2 * jp + jl
                i = wcnt[0] % 3
                wcnt[0] += 1
                wg, wgb = wt[i], wtb[i]
                wgs.append((wg, wgb))
                for br in range(4):
                    c0 = O_G + br * 1024 + j * 128
                    fw.dma("pool", wg[:, :, br * 128:(br + 1) * 128],
                           I["w_in"][li][:, c0:c0 + 128].rearrange("(k p) c -> p k c", p=128), writes=[wgb])
                for br in range(4):
                    fw.dma("pool", wbr[jl][:, br * 4:(br + 1) * 4, :],
                           I[brn[br]][li][:, j * 128:(j + 1) * 128].rearrange("(e p) c -> p e c", p=128),
                           writes=[wbrb[jl]])
            for tb in tbs:
                t0, tn = TBLK[tb]
                ob = cnt % 2
                cnt += 1
                fw.dma("sp", OTt[ob][:, :, 0:tn], OTd[:, :, t0:t0 + tn].rearrange("c p q -> p c q"), writes=[OTtb[ob]])
                for jl in range(2):
                    j = 2 * jp + jl
                    wg, wgb = wgs[jl]
                    ac = acn % 2
                    acn += 1
                    for br in range(4):
                        pg = ps_main()
                        for kc in range(8):
                            fw.op("pe", lambda e, pg=pg, kc=kc, br=br, t0=t0, tn=tn, wg=wg: e.matmul(
                                PS[pg][:, 0:tn], wg[:, kc, br * 128:(br + 1) * 128], hT[:, kc, t0:t0 + tn],
                                start=(kc == 0), stop=(kc == 7)),
                                reads=[wgb] + [hTb[t0 // 128 + q] for q in range(tn // 128)], writes=[PSB[pg]],
                                inc=(kc == 7), pe_acc=True)
                        sg = br % 2
                        fw.op("act", lambda e, pg=pg, sg=sg, tn=tn: e.activation(
                            out=SGm[sg][:, 0:tn], in_=PS[pg][:, 0:tn], func=AF.Sigmoid),
                            reads=[PSB[pg]], writes=[SGmb[sg]])
                        pb = ps_aux()
                        for ec in range(4):
                            fw.op("pe", lambda e, pb=pb, ec=ec, br=br, tn=tn, jl=jl, ob=ob: e.matmul(
                                PS[pb][:, 0:tn], wbr[jl][:, br * 4 + ec, :], OTt[ob][:, br * 4 + ec, 0:tn],
                                start=(ec == 0), stop=(ec == 3)),
                                reads=[wbrb[jl], OTtb[ob]], writes=[PSB[pb]], inc=(ec == 3), pe_acc=True)
                        if br == 0:
                            fw.op("dve", lambda e, pb=pb, sg=sg, ac=ac, tn=tn: e.tensor_tensor(
                                out=ACCm[ac][:, 0:tn], in0=PS[pb][:, 0:tn], in1=SGm[sg][:, 0:tn], op=ALU.mult),
                                reads=[PSB[pb], SGmb[sg]], writes=[ACCmb[ac]])
                        else:
                            fw.op("dve", lambda e, pb=pb, sg=sg, tn=tn: e.tensor_tensor(
                                out=TMPm[:, 0:tn], in0=PS[pb][:, 0:tn], in1=SGm[sg][:, 0:tn], op=ALU.mult),
                                reads=[PSB[pb], SGmb[sg]], writes=[TMPmb])
                            if br < 3:
                                fw.op("pool", lambda e, ac=ac, tn=tn: e.tensor_tensor(
                                    out=ACCm[ac][:, 0:tn], in0=ACCm[ac][:, 0:tn], in1=TMPm[:, 0:tn], op=ALU.add),
                                    reads=[TMPmb, ACCmb[ac]], writes=[ACCmb[ac]])
                            else:
                                fw.op("pool", lambda e, ac=ac, tn=tn, j=j, t0=t0: e.tensor_tensor(
                                    out=mT[:, j, t0:t0 + tn], in0=ACCm[ac][:, 0:tn], in1=TMPm[:, 0:tn], op=ALU.add),
                                    reads=[TMPmb, ACCmb[ac]], writes=[mTb[tb]])
        fw.barrier()

    def resid_tile(n, tt, ysrc, ybufs, gidx, xsrc, dsts):
        b = n % 2
        fw.dma("sp", xt[b][:], xsrc, writes=[xtb[b]])
        for half in range(2):
            fw.op("act", lambda e, half=half, b=b: e.activation(
                out=xn[b][:, half * 512:(half + 1) * 512], in_=ysrc[half], func=AF.Square,
                accum_out=stat[:, 24 + half:25 + half]), reads=[ybufs[half]], writes=[xnb[b], statb])
        fw.op("dve", lambda e: e.tensor_tensor(out=stat[:, 26:27], in0=stat[:, 24:25], in1=stat[:, 25:26], op=ALU.add),
              reads=[statb], writes=[statb])
        fw.op("act", lambda e: e.activation(out=stat[:, 27:28], in_=stat[:, 26:27], func=AF.Ln, scale=1.0 / D,
                                            bias=epsc[:, 0:1]), reads=[statb, cb], writes=[statb])
        fw.op("act", lambda e: e.activation(out=stat[:, 27:28], in_=stat[:, 27:28], func=AF.Exp, scale=-0.5),
              reads=[statb], writes=[statb])
        for half in range(2):
            fw.op("dve", lambda e, half=half, b=b: e.scalar_tensor_tensor(
                out=xn[b][:, half * 512:(half + 1) * 512], in0=ysrc[half], scalar=stat[:, 27:28],
                in1=gb[:, gidx, half * 512:(half + 1) * 512], op0=ALU.mult, op1=ALU.mult),
                reads=[ybufs[half], statb, mb], writes=[xnb[b]])
        fw.op("pool", lambda e, b=b: e.tensor_tensor(out=xt[b][:], in0=xt[b][:], in1=xn[b][:], op=ALU.add),
              reads=[xnb[b], xtb[b]], writes=[xtb[b]])
        for d in dsts:
            fw.dma("sp", d, xt[b][:], reads=[xtb[b]])

    def wout_phase(li, xsrc_fn):
        tiles = list(range(NT)) if li == 0 else list(range(2, NT))
        fw.dma("pool", wout[:, :, :], I["w_out"][li].rearrange("(k p) c -> p k c", p=128), writes=[woutb])
        for n, tt in enumerate(tiles):
            pis = []
            for half in range(2):
                pi = ps_main()
                pis.append(pi)
                for kc in range(8):
                    fw.op("pe", lambda e, pi=pi, kc=kc, tt=tt, half=half: e.matmul(
                        PS[pi][:, :], mT[:, kc, tt * 128:(tt + 1) * 128], wout[:, kc, half * 512:(half + 1) * 512],
                        start=(kc == 0), stop=(kc == 7)),
                        reads=[woutb], writes=[PSB[pi]], inc=(kc == 7), pe_acc=True)
            gidx = 1 if tt < 2 else 0
            resid_tile(n, tt, [PS[pis[0]][:, :], PS[pis[1]][:, :]], [PSB[pis[0]], PSB[pis[1]]], gidx,
                       xsrc_fn(tt), [Xd[tt * 128:(tt + 1) * 128, :]])
        fw.barrier()

    NFC = FFN_DENSE // 128
    gTd = AR[:, 0:NFC * 768].rearrange("p (f t) -> p f t", f=NFC)
    gTdb = Buf()
    W2d = AR[:, NFC * 768:NFC * 768 + NFC * 1024].rearrange("p (f c) -> p f c", f=NFC)
    W2db = Buf()
    SLU = [AR[:, NFC * 1792 + i * 512:NFC * 1792 + (i + 1) * 512] for i in range(2)]
    SLUb = [Buf(), Buf()]

    def ffn_dense(li):
        w1, w3, w2 = I["w1_dense"][0], I["w3_dense"][0], I["w2_dense"][0]
        for g0 in range(0, NFC, 8):
            ng = min(8, NFC - g0)
            fw.dma("pool", W2d[:, g0:g0 + ng, :], w2[g0 * 128:(g0 + ng) * 128, :].rearrange("(f p) c -> p f c", p=128),
                   writes=[W2db])
        cnt = 0
        for third in range(3):
            tok0 = third * 768
            for g0 in range(0, NFC, 4):
                ng = min(4, NFC - g0)
                wa, wab = load_w(w1[:, g0 * 128:(g0 + ng) * 128], ng * 128)
                wc, wcb = load_w(w3[:, g0 * 128:(g0 + ng) * 128], ng * 128)
                for c in range(ng):
                    fc = g0 + c
                    for (s0, sn) in ((0, 512), (512, 256)):
                        t0 = tok0 + s0
                        rb = [hTb[t0 // 128 + q] for q in range(sn // 128)]
                        pa_, pb_ = ps_main(), ps_main()
                        for (pp, ww, wwb) in ((pa_, wa, wab), (pb_, wc, wcb)):
                            for kc in range(8):
                                fw.op("pe", lambda e, pp=pp, ww=ww, kc=kc, c=c, t0=t0, sn=sn: e.matmul(
                                    PS[pp][:, 0:sn], ww[:, kc, c * 128:(c + 1) * 128], hT[:, kc, t0:t0 + sn],
                                    start=(kc == 0), stop=(kc == 7)),
                                    reads=[wwb] + rb, writes=[PSB[pp]], inc=(kc == 7), pe_acc=True)
                        sl = cnt % 2
                        cnt += 1
                        fw.op("act", lambda e, pa_=pa_, sl=sl, sn=sn: e.activation(out=SLU[sl][:, 0:sn], in_=PS[pa_][:, 0:sn],
                                                                                 func=AF.Silu),
                              reads=[PSB[pa_]], writes=[SLUb[sl]])
                        fw.op("dve", lambda e, pb_=pb_, sl=sl, sn=sn, fc=fc, s0=s0: e.tensor_tensor(
                            out=gTd[:, fc, s0:s0 + sn], in0=PS[pb_][:, 0:sn], in1=SLU[sl][:, 0:sn], op=ALU.mult),
                            reads=[PSB[pb_], SLUb[sl]], writes=[gTdb])
            for q in range(6):
                tt = third * 6 + q
                pis = []
                for half in range(2):
                    pi = ps_main()
                    pis.append(pi)
                    for fc in range(NFC):
                        fw.op("pe", lambda e, pi=pi, fc=fc, q=q, half=half: e.matmul(
                            PS[pi][:, :], gTd[:, fc, q * 128:(q + 1) * 128], W2d[:, fc, half * 512:(half + 1) * 512],
                            start=(fc == 0), stop=(fc == NFC - 1)),
                            reads=[gTdb, W2db], writes=[PSB[pi]], inc=(fc == NFC - 1), pe_acc=True)
                gidx = 3 if tt < 2 else 2
                resid_tile(q, tt, [PS[pis[0]][:, :], PS[pis[1]][:, :]], [PSB[pis[0]], PSB[pis[1]]], gidx,
                           Xd[tt * 128:(tt + 1) * 128, :], [Xd[tt * 128:(tt + 1) * 128, :]])
        fw.barrier()

    NFE = FFN_EXPERT // 128
    oacc = AR[:, 0:32768].bitcast(F32).rearrange("p (t c) -> p t c", t=16)
    oaccb = [Buf() for _ in range(16)]
    W2m = [AR[:, 32768 + i * 4096:32768 + (i + 1) * 4096].rearrange("p (f c) -> p f c", f=4) for i in range(2)]
    W2mb = [Buf(), Buf()]
    SLm = [AR[:, 40960 + i * 512:40960 + (i + 1) * 512] for i in range(2)]
    SLmb = [Buf(), Buf()]
    comb = AR[:, 41984:41984 + 256].bitcast(F32).rearrange("p (t e) -> p t e", t=16)
    combb = Buf()
    tmpmoe = AR[:, 42240:42240 + 1024].bitcast(F32)
    tmpmoeb = Buf()
    gTm = [cos16, sin16, cos8, sin8]
    gTmb = [Buf() for _ in range(4)]
    LT = [(C + i * 512, 512) for i in range(4)]

    def moe_router():
        wr, wrb = load_w(I["w_router"][0], 8)
        A_, B_ = stat[:, 28:36], stat[:, 36:44]
        for t in range(16):
            tok = C + t * 128
            pi = ps_main()
            for kc in range(8):
                fw.op("pe", lambda e, pi=pi, kc=kc, tok=tok: e.matmul(
                    PS[pi][:, 0:8], hT[:, kc, tok:tok + 128], wr[:, kc, 0:8], start=(kc == 0), stop=(kc == 7)),
                    reads=[wrb, hTb[2 + t]], writes=[PSB[pi]], inc=(kc == 7), pe_acc=True)
            fw.op("dve", lambda e, pi=pi: e.tensor_copy(out=A_, in_=PS[pi][:, 0:8]), reads=[PSB[pi]], writes=[statb])
            fw.op("dve", lambda e: e.reduce_max(out=stat[:, 44:45], in_=A_, axis=AX.X), reads=[statb], writes=[statb])
            fw.op("dve", lambda e: e.tensor_scalar(out=B_, in0=A_, scalar1=stat[:, 44:45], scalar2=None,
                                                   op0=ALU.is_equal), reads=[statb], writes=[statb])
            fw.op("dve", lambda e: e.scalar_tensor_tensor(out=A_, in0=B_, scalar=-1e30, in1=A_, op0=ALU.mult,
                                                          op1=ALU.add), reads=[statb], writes=[statb])
            fw.op("dve", lambda e: e.reduce_max(out=stat[:, 45:46], in_=A_, axis=AX.X), reads=[statb], writes=[statb])
            fw.op("dve", lambda e: e.tensor_scalar(out=A_, in0=A_, scalar1=stat[:, 45:46], scalar2=None,
                                                   op0=ALU.is_equal), reads=[statb], writes=[statb])
            fw.op("dve", lambda e: e.tensor_tensor(out=stat[:, 46:47], in0=stat[:, 45:46], in1=stat[:, 44:45],
                                                   op=ALU.subtract), reads=[statb], writes=[statb])
            fw.op("act", lambda e: e.activation(out=stat[:, 47:48], in_=stat[:, 46:47], func=AF.Exp),
                  reads=[statb], writes=[statb])
            fw.op("dve", lambda e: e.tensor_scalar(out=stat[:, 48:49], in0=stat[:, 47:48], scalar1=1.0, scalar2=None,
                                                   op0=ALU.add), reads=[statb], writes=[statb])
            fw.op("dve", lambda e: e.reciprocal(out=stat[:, 48:49], in_=stat[:, 48:49]), reads=[statb], writes=[statb])
            fw.op("dve", lambda e: e.tensor_tensor(out=stat[:, 49:50], in0=stat[:, 47:48], in1=stat[:, 48:49],
                                                   op=ALU.mult), reads=[statb], writes=[statb])
            fw.op("dve", lambda e: e.tensor_scalar(out=B_, in0=B_, scalar1=stat[:, 48:49], scalar2=None,
                                                   op0=ALU.mult), reads=[statb], writes=[statb])
            fw.op("dve", lambda e, t=t: e.scalar_tensor_tensor(out=comb[:, t, :], in0=A_, scalar=stat[:, 49:50],
                                                              in1=B_, op0=ALU.mult, op1=ALU.add),
                  reads=[statb], writes=[combb])

    def moe_phase():
        moe_router()
        w1, w3, w2 = I["w1_moe"][0], I["w3_moe"][0], I["w2_moe"][0]
        cnt = 0
        first = True
        gi = 0
        for ex in range(N_EXPERTS):
            for g0 in range(0, NFE, 4):
                wa, wab = load_w(w1[ex][:, g0 * 128:(g0 + 4) * 128], 512)
                wc, wcb = load_w(w3[ex][:, g0 * 128:(g0 + 4) * 128], 512)
                wi = gi % 2
                gi += 1
                fw.dma("pool", W2m[wi][:, :, :], w2[ex][g0 * 128:(g0 + 4) * 128, :].rearrange("(f p) c -> p f c", p=128),
                       writes=[W2mb[wi]])
                for c in range(4):
                    for (t0, tn) in LT:
                        rb = [hTb[t0 // 128 + q] for q in range(4)]
                        pa_, pb_ = ps_main(), ps_main()
                        for (pp, ww, wwb) in ((pa_, wa, wab), (pb_, wc, wcb)):
                            for kc in range(8):
                                fw.op("pe", lambda e, pp=pp, ww=ww, kc=kc, c=c, t0=t0: e.matmul(
                                    PS[pp][:, :], ww[:, kc, c * 128:(c + 1) * 128], hT[:, kc, t0:t0 + 512],
                                    start=(kc == 0), stop=(kc == 7)),
                                    reads=[wwb] + rb, writes=[PSB[pp]], inc=(kc == 7), pe_acc=True)
                        sl = cnt % 2
                        cnt += 1
                        fw.op("act", lambda e, pa_=pa_, sl=sl: e.activation(out=SLm[sl][:, :], in_=PS[pa_][:, :],
                                                                          func=AF.Silu),
                              reads=[PSB[pa_]], writes=[SLmb[sl]])
                        fw.op("dve", lambda e, pb_=pb_, sl=sl, c=c, t0=t0: e.tensor_tensor(
                            out=gTm[c][:, t0 - C:t0 - C + 512], in0=PS[pb_][:, :], in1=SLm[sl][:, :], op=ALU.mult),
                            reads=[PSB[pb_], SLmb[sl]], writes=[gTmb[c]])
                for t in range(16):
                    for half in range(2):
                        pi = ps_aux()
                        for c in range(4):
                            fw.op("pe", lambda e, pi=pi, c=c, t=t, half=half, wi=wi: e.matmul(
                                PS[pi][:, :], gTm[c][:, t * 128:(t + 1) * 128], W2m[wi][:, c, half * 512:(half + 1) * 512],
                                start=(c == 0), stop=(c == 3)),
                                reads=[gTmb[c], W2mb[wi]], writes=[PSB[pi]], inc=(c == 3), pe_acc=True)
                        if first:
                            fw.op("dve", lambda e, pi=pi, t=t, half=half, ex=ex: e.tensor_scalar(
                                out=oacc[:, t, half * 512:(half + 1) * 512], in0=PS[pi][:, :],
                                scalar1=comb[:, t, ex:ex + 1], scalar2=None, op0=ALU.mult),
                                reads=[PSB[pi], combb], writes=[oaccb[t]])
                        elif half == 1:
                            fw.op("act", lambda e, pi=pi, t=t, ex=ex: e.activation(
                                out=tmpmoe[:, :], in_=PS[pi][:, :], func=AF.Copy, scale=comb[:, t, ex:ex + 1]),
                                reads=[PSB[pi], combb], writes=[tmpmoeb])
                            fw.op("pool", lambda e, t=t: e.tensor_tensor(
                                out=oacc[:, t, 512:1024], in0=oacc[:, t, 512:1024], in1=tmpmoe[:, :], op=ALU.add),
                                reads=[tmpmoeb, oaccb[t]], writes=[oaccb[t]])
                        else:
                            fw.op("dve", lambda e, pi=pi, t=t, half=half, ex=ex: e.scalar_tensor_tensor(
                                out=oacc[:, t, half * 512:(half + 1) * 512], in0=PS[pi][:, :],
                                scalar=comb[:, t, ex:ex + 1], in1=oacc[:, t, half * 512:(half + 1) * 512],
                                op0=ALU.mult, op1=ALU.add),
                                reads=[PSB[pi], combb, oaccb[t]], writes=[oaccb[t]])
                first = False
        for t in range(16):
            tt = 2 + t
            resid_tile(t, tt, [oacc[:, t, 0:512], oacc[:, t, 512:1024]], [oaccb[t], oaccb[t]], 2,
                       Xd[tt * 128:(tt + 1) * 128, :], [out[t * 128:(t + 1) * 128, :]])
        fw.barrier()

    def layer1():
        xs = lambda tt: Xd[tt * 128:(tt + 1) * 128, :]
        layer_vectors(1)
        fw.barrier()
        norm_phase(1, 0, xs, list(range(NT)))
        fw.barrier()
        branch_a(1)
        branch_b(1)
        branch_c(1)
        branch_d(1)
        fw.mark("t_l1attn")
        merge_phase(1)
        wout_phase(1, xs)
        fw.mark("t_l1mix")
        norm_phase(1, 1, xs, list(range(2, NT)))
        fw.barrier()
        moe_phase()

    def layer0():
        branch_a(0)
        fw.mark("t_a")
        branch_b(0)
        fw.mark("t_b")
        branch_c(0)
        fw.mark("t_c")
        branch_d(0)
        fw.mark("t_l0attn")
        merge_phase(0)
        wout_phase(0, x_src0)
        fw.mark("t_l0mix")
        norm_phase(0, 1, lambda tt: Xd[tt * 128:(tt + 1) * 128, :], list(range(NT)))
        fw.barrier()
        ffn_dense(0)
        fw.mark("t_l0")

    layer_vectors(0)
    fw.barrier()
    fw.mark("lv")
    norm_phase(0, 0, x_src0, list(range(NT)))
    fw.barrier()
    fw.mark("norm")
    if dbg is None or dbg["what"] == "full" or (dbg.get("stop") or "")[:2] in ("t_", "a_"):
        fw.mark("t_pre")
        layer0()
        layer1()
    if dbg is not None and dbg["what"] in ("x1", "x2"):
        branch_a(0)
        branch_b(0)
        branch_c(0)
        branch_d(0)
        merge_phase(0)
        wout_phase(0, x_src0)
        if dbg["what"] == "x2":
            norm_phase(0, 1, lambda tt: Xd[tt * 128:(tt + 1) * 128, :], list(range(NT)))
            fw.barrier()
            ffn_dense(0)
    if dbg is not None and dbg["what"] in ("oc", "qkc"):
        branch_c(0)
    if dbg is not None and dbg["what"] == "oa":
        branch_a(0)
    if dbg is not None and (dbg["what"] == "ob" or (dbg.get("stop") or "").startswith("b")):
        branch_b(0)
    if dbg is not None and dbg["what"] == "od":
        branch_d(0)

    fw.frozen = False
    fw.barrier()
    dcnt = [0]

    def dump(src, dst, rb=()):
        p, n = src.shape[0], src.shape[1]
        for c0 in range(0, n, 1024):
            w = min(1024, n - c0)
            b = dcnt[0] % 2
            dcnt[0] += 1
            fw.op("dve", lambda e, b=b, c0=c0, w=w: e.tensor_copy(out=xn[b][0:p, 0:w], in_=src[:, c0:c0 + w]),
                  reads=list(rb), writes=[xnb[b]])
            fw.dma("sp", dst[:, c0:c0 + w], xn[b][0:p, 0:w], reads=[xnb[b]])

    if dbg is not None and dbg["what"] in ("x1", "x2"):
        for tt in range(NT):
            b = tt % 2
            fw.dma("sp", xt[b][:], Xd[tt * 128:(tt + 1) * 128, :], writes=[xtb[b]])
            fw.dma("sp", dbg_out[tt * 128:(tt + 1) * 128, :], xt[b][:], reads=[xtb[b]])
    if dbg is not None and dbg["what"] == "bpv":
        for i in (3, 4):
            dump(PS[i][:, 0:260], dbg_out[i * 128:(i + 1) * 128, 0:260])
    if dbg is not None and dbg["what"] == "stage":
        dump(ident[:, :], dbg_out[:, :])
    if dbg is not None and dbg["what"] == "qkc":
        for c in range(4):
            dump(QT[:, c, :], dbg_out[c * 128:(c + 1) * 128, :])
        dump(KT[:, 0, :], dbg_out[512:640, :])
        dump(KT[:, 1, :], dbg_out[640:768, :])
    if dbg is not None and dbg["what"] == "rope":
        dump(cos16[:, :], dbg_out[0:128, :])
        dump(sin16[:, :], dbg_out[128:256, :])
        dump(pm16[:, :], dbg_out[256:384, 0:128])
        dump(cos8[:, :], dbg_out[384:512, :])
        dump(sin8[:, :], dbg_out[512:640, :])
        dump(pm8[:, :], dbg_out[640:768, 0:128])
    if dbg is not None and dbg["what"] == "hT":
        for kc in range(8):
            dump(hT[:, kc, :], dbg_out[kc * 128:(kc + 1) * 128, :])
    if dbg is not None and dbg["what"] in ("oa", "ob", "oc", "od"):
        br = "abcd".index(dbg["what"][1])
        for ec in range(4):
            for c0 in range(0, T, 512):
                w = min(512, T - c0)
                fw.dma("sp", OTs[0][:, 0, 0:w], OTd[br * 4 + ec, :, c0:c0 + w], writes=[OTsb[0]])
                dump(OTs[0][:, 0, 0:w], dbg_out[ec * 128:(ec + 1) * 128, c0:c0 + w], rb=[OTsb[0]])
    fw.barrier(only=["sp"])
    fw.emit()


def make_in_maps(inputs, cores):
    maps = []
    shared = {}
    for k, v in inputs.items():
        if k in ("x", "c", "ctx", "c_ctx"):
            continue
        a = np.ascontiguousarray(np.asarray(v, dtype=np.float32))
        shared[k] = a
    for b in cores:
        m = dict(shared)
        m["x"] = np.ascontiguousarray(np.asarray(inputs["x"][b], dtype=np.float32))
        m["ctx"] = np.ascontiguousarray(np.asarray(inputs["ctx"][b], dtype=np.float32))
        m["cvec"] = np.ascontiguousarray(
            np.stack([np.asarray(inputs["c"][b]), np.asarray(inputs["c_ctx"])]).astype(np.float32))
        maps.append(m)
    return maps


_NC_CACHE = {}


def kernel(**inputs):
    if "nc" not in _NC_CACHE:
        _NC_CACHE["nc"] = build_program()
    nc = _NC_CACHE["nc"]
    maps = make_in_maps(inputs, list(range(8)))
    res = run_bass_kernel_spmd(nc, maps, core_ids=list(range(8)))
    return np.stack([np.asarray(r["out"], dtype=np.float32) for r in res.results], axis=0)
```

```python
import contextlib
import math
import numpy as np
import concourse.bass as bass
import concourse.mybir as mybir
from concourse.bass_utils import run_bass_kernel_spmd

F32 = mybir.dt.float32
BF16 = mybir.dt.bfloat16
I32 = mybir.dt.int32
AF = mybir.ActivationFunctionType
ALU = mybir.AluOpType
AX = mybir.AxisListType

D = 1024
S = 2048
C = 256
T = S + C
NT = T // 128
DEPTH = 2
D_IN = 8352
EPS = 1e-6
FFN_DENSE = 2816
N_EXPERTS = 8
FFN_EXPERT = 3584
O_AQ, O_AK, O_AV = 0, 512, 1024
O_BQ, O_BK, O_BV = 1536, 2048, 2560
O_CQ, O_CK, O_CV = 3072, 3584, 3712
O_DQA, O_DKVA, O_DKR = 3840, 4096, 4224
O_G = 4256
TBLK = [(0, 256), (256, 512), (768, 512), (1280, 512), (1792, 512)]


class Buf:
    __slots__ = ("w", "r")

    def __init__(self):
        self.w = None
        self.r = {}


class Stream:
    def __init__(self, name, sem):
        self.name = name
        self.sem = sem
        self.count = 0
        self.waited = {}
        self.ops = []


class FW:
    def __init__(self, nc, es):
        self.nc = nc
        self.es = es
        self.st = {}
        for n in ("pe", "act", "dve", "pool", "sp"):
            self.st[n] = Stream(n, es.enter_context(nc.semaphore("c_" + n)))
        self.dpool = {}
        for q, n in (("sp", 12), ("pool", 8), ("act", 4)):
            self.dpool[q] = [[es.enter_context(nc.semaphore(f"d_{q}{i}")), 0] for i in range(n)]
        self.dnext = {"sp": 0, "pool": 0, "act": 0}
        self.nbuf = 0
        self.frozen = False
        self.stop = None

    def mark(self, name):
        if self.stop is not None and name == self.stop:
            self.frozen = True

    def _wait(self, s, ev):
        sem, val = ev
        k = id(sem)
        if s.waited.get(k, 0) < val:
            s.waited[k] = val
            s.ops.append(("w", sem, val))

    def _deps(self, s, reads, writes, pe_acc=False):
        for b in reads:
            if b.w is not None:
                self._wait(s, b.w)
        for b in writes:
            if b.w is not None and not (pe_acc and b.w[0] is s.sem):
                self._wait(s, b.w)
            for ev in b.r.values():
                self._wait(s, ev)

    def op(self, eng, fn, reads=(), writes=(), inc=True, pe_acc=False):
        if self.frozen:
            return
        s = self.st[eng]
        self._deps(s, reads, writes, pe_acc)
        ev = (s.sem, s.count + 1)
        if inc:
            s.count += 1
        s.ops.append(("i", fn, inc))
        for b in writes:
            b.w = ev
            b.r = {}
        for b in reads:
            b.r[id(s.sem)] = ev

    def dma(self, q, out, in_, reads=(), writes=(), **kw):
        if self.frozen:
            return
        s = self.st[q]
        pool = self.dpool[q]
        i = self.dnext[q]
        self.dnext[q] = (i + 1) % len(pool)
        ent = pool[i]
        sem = ent[0]
        if ent[1] > 0:
            self._wait(s, (sem, ent[1]))
        self._deps(s, reads, writes)
        ent[1] += 16
        ev = (sem, ent[1])
        s.ops.append(("d", out, in_, sem, kw))
        for b in writes:
            b.w = ev
            b.r = {}
        for b in reads:
            b.r[id(sem)] = ev
        return ev

    def all_events(self):
        evs = []
        for n, s in self.st.items():
            if s.count > 0:
                evs.append((s.sem, s.count))
        for q, pool in self.dpool.items():
            for sem, val in pool:
                if val > 0:
                    evs.append((sem, val))
        return evs

    def barrier(self, only=None):
        if self.frozen:
            return
        evs = self.all_events()
        for n, s in self.st.items():
            if only is not None and n not in only:
                continue
            for ev in evs:
                if ev[0] is s.sem:
                    continue
                self._wait(s, ev)

    def emit(self):
        nc = self.nc
        hmap = {"pe": "tensor", "act": "scalar", "dve": "vector", "pool": "gpsimd", "sp": "sync"}
        with nc.Block() as block:
            for n, s in self.st.items():
                def body(e, s=s):
                    for o in s.ops:
                        if o[0] == "w":
                            e.wait_ge(o[1], o[2])
                        elif o[0] == "i":
                            ins = o[1](e)
                            if o[2]:
                                ins.then_inc(s.sem, 1)
                        else:
                            e.dma_start(out=o[1], in_=o[2], **o[4]).then_inc(o[3], 16)
                getattr(block, hmap[n])(body)


class K:
    pass


def build_program(dbg=None):
    nc = bass.Bass("TRN2", target_bir_lowering=False)
    es = contextlib.ExitStack()
    with es:
        _build(nc, es, dbg)
    return nc


def _dram_inputs(nc):
    L = DEPTH
    specs = {
        "x": [S, D], "ctx": [C, D], "cvec": [2, D],
        "w_ada": [L, D, 6 * D], "b_ada": [L, 6 * D],
        "g_mix_pre": [L, D], "g_mix_post": [L, D], "g_ffn_pre": [L, D], "g_ffn_post": [L, D],
        "w_in": [L, D, D_IN],
        "lam_q1": [L, 64], "lam_k1": [L, 64], "lam_q2": [L, 64], "lam_k2": [L, 64],
        "g_diff_sub": [L, 128], "na_rpb": [L, 8, 15, 31],
        "g_qnorm": [L, 64], "g_knorm": [L, 64], "g_q_lora": [L, 256],
        "w_uq": [L, 256, 768], "g_kv_lora": [L, 128], "w_ukv": [L, 128, 1024],
        "w_br_a": [L, 512, D], "w_br_b": [L, 512, D], "w_br_c": [L, 512, D], "w_br_d": [L, 512, D],
        "w_out": [L, D, D],
        "w1_dense": [1, D, FFN_DENSE], "w3_dense": [1, D, FFN_DENSE], "w2_dense": [1, FFN_DENSE, D],
        "w_router": [1, D, N_EXPERTS],
        "w1_moe": [1, N_EXPERTS, D, FFN_EXPERT], "w3_moe": [1, N_EXPERTS, D, FFN_EXPERT],
        "w2_moe": [1, N_EXPERTS, FFN_EXPERT, D],
    }
    return {k: nc.dram_tensor(k, v, F32, kind="ExternalInput").ap() for k, v in specs.items()}


def _build(nc, es, dbg):
    fw = FW(nc, es)
    if dbg is not None:
        fw.stop = dbg.get("stop")
    I = _dram_inputs(nc)
    out = nc.dram_tensor("out", [S, D], F32, kind="ExternalOutput").ap()
    dbg_out = None
    if dbg is not None:
        dbg_out = nc.dram_tensor("dbg", list(dbg["shape"]), F32, kind="ExternalOutput").ap()
    Xd = nc.dram_tensor("Xres", [T, D], F32).ap()

    def sb(name, shape, dt):
        return es.enter_context(nc.sbuf_tensor(name, list(shape), dt))

    PSall = es.enter_context(nc.psum_tensor("psall", [128, 4096], F32))
    PS = [PSall[:, i * 512:(i + 1) * 512] for i in range(8)]
    PSB = [Buf() for _ in range(8)]

    ident = sb("ident", [128, 128], F32)
    ones_f = sb("ones_f", [128, 128], F32)
    cb = Buf()
    iot = sb("iot", [128, 128], F32)
    fw.op("pool", lambda e: e.iota(iot[:], [[1, 128]], base=0, channel_multiplier=-1,
                                  allow_small_or_imprecise_dtypes=True), writes=[cb])
    fw.op("dve", lambda e: e.tensor_single_scalar(out=ident[:], in_=iot[:], scalar=0.0, op=ALU.is_equal),
          reads=[cb], writes=[cb])
    fw.op("dve", lambda e: e.memset(ones_f[:], 1.0), writes=[cb])
    fw.mark("const0")

    colv = sb("colv", [128, 8, 8], F32)
    gb = sb("gb", [128, 4, D], F32)
    sTb = sb("sTb", [128, 8, 2, 128], BF16)
    sTf = sb("sTf", [128, 16], F32)
    cv_row = sb("cv_row", [48, 128], F32)
    modc = sb("modc", [128, 48, 2], F32)
    badc = sb("badc", [128, 48], F32)
    gcol = sb("gcol", [128, 4, 8], F32)
    XX = sb("XX", [128, 4, D], F32)
    xt = [XX[:, i, :] for i in range(2)]
    xtb = [Buf(), Buf()]
    xn = [XX[:, 2 + i, :] for i in range(2)]
    xnb = [Buf(), Buf()]
    stat = sb("stat", [128, 64], F32)
    statb = Buf()
    hT = sb("hT", [128, 8, T], BF16)
    hTb = [Buf() for _ in range(NT)]
    wt = [sb(f"wt{i}", [128, 8, 512], BF16) for i in range(3)]
    wtb = [Buf() for _ in range(3)]
    wcnt = [0]

    mb = Buf()

    def load_w(src, ncols, krows=1024):
        i = wcnt[0] % 3
        wcnt[0] += 1
        nk = krows // 128
        fw.dma("pool", wt[i][:, 0:nk, 0:ncols], src.rearrange("(k p) c -> p k c", p=128), writes=[wtb[i]])
        return wt[i], wtb[i]

    def to_cols(src_rows_ap, nrows, dst, ps_i=0):
        fw.dma("sp", cv_row[0:nrows, :], src_rows_ap, writes=[mb])
        fw.op("pe", lambda e: e.transpose(PS[ps_i][:, 0:nrows], cv_row[0:nrows, :], ident[0:nrows, 0:nrows]),
              reads=[mb, cb], writes=[PSB[ps_i]])
        fw.op("dve", lambda e: e.tensor_copy(out=dst, in_=PS[ps_i][:, 0:nrows]), reads=[PSB[ps_i]], writes=[mb])

    to_cols(I["cvec"].rearrange("j (k d) -> (j k) d", d=128), 16, sTf[:, 0:16])
    fw.op("act", lambda e: e.activation(out=sTf[:, 0:16], in_=sTf[:, 0:16], func=AF.Silu), reads=[mb], writes=[mb])
    for j in range(2):
        for kc in range(8):
            fw.op("dve", lambda e, j=j, kc=kc: e.tensor_scalar(
                out=sTb[:, kc, j, :], in0=ones_f[:, :], scalar1=sTf[:, j * 8 + kc:j * 8 + kc + 1], scalar2=None,
                op0=ALU.mult), reads=[mb, cb], writes=[mb])

    fw.mark("stb")

    def layer_vectors(li):
        to_cols(I["b_ada"][li].rearrange("(r d) -> r d", d=128), 48, badc[:, :])
        for gi, nm in enumerate(("g_mix_pre", "g_mix_post", "g_ffn_pre", "g_ffn_post")):
            to_cols(I[nm][li].rearrange("(r d) -> r d", d=128), 8, gcol[:, gi, :])
        for piece in range(12):
            w, wb = load_w(I["w_ada"][li, :, piece * 512:(piece + 1) * 512], 512)
            for q in range(4):
                ech = piece * 4 + q
                for kc in range(8):
                    fw.op("pe", lambda e, q=q, kc=kc, ech=ech, w=w: e.matmul(
                        PS[1][:, ech * 2:ech * 2 + 2], w[:, kc, q * 128:(q + 1) * 128], sTb[:, kc, :, 0],
                        start=(kc == 0), stop=(kc == 7)),
                        reads=[wb, mb], writes=[PSB[1]], inc=(kc == 7 and q == 3), pe_acc=True)
            part = piece // 2
            if part in (2, 5):
                half = piece % 2
                for j in range(2):
                    pj = 2 + j
                    for kc in range(8):
                        fw.op("pe", lambda e, kc=kc, j=j, pj=pj, w=w: e.matmul(
                            PS[pj][:, :], sTb[:, kc, j, :], w[:, kc, :], start=(kc == 0), stop=(kc == 7)),
                            reads=[wb, mb], writes=[PSB[pj]], inc=(kc == 7), pe_acc=True)
                    idx = (0 if part == 2 else 2) + j
                    bsel = j
                    fw.dma("sp", xt[bsel][:, 0:512],
                           I["b_ada"][li:li + 1, piece * 512:(piece + 1) * 512].broadcast_to([128, 512]),
                           writes=[xtb[bsel]])
                    gname = "g_mix_post" if part == 2 else "g_ffn_post"
                    fw.dma("sp", xt[bsel][:, 512:1024],
                           I[gname][li:li + 1, half * 512:(half + 1) * 512].broadcast_to([128, 512]),
                           writes=[xtb[bsel]])
                    fw.op("dve", lambda e, pj=pj, bsel=bsel: e.tensor_tensor(
                        out=xn[bsel][:, 0:512], in0=PS[pj][:, :], in1=xt[bsel][:, 0:512], op=ALU.add),
                        reads=[PSB[pj], xtb[bsel]], writes=[xnb[bsel]])
                    fw.op("dve", lambda e, idx=idx, half=half, bsel=bsel: e.tensor_tensor(
                        out=gb[:, idx, half * 512:(half + 1) * 512], in0=xn[bsel][:, 0:512],
                        in1=xt[bsel][:, 512:1024], op=ALU.mult),
                        reads=[xnb[bsel], xtb[bsel]], writes=[mb])
        fw.op("dve", lambda e: e.tensor_copy(out=modc[:].rearrange("p a j -> p (a j)"), in_=PS[1][:, 0:96]),
              reads=[PSB[1]], writes=[mb])
        for j in range(2):
            for h, (gi, sci, shi) in enumerate(((0, 1, 0), (2, 4, 3))):
                setA = h * 4 + j * 2
                fw.op("dve", lambda e, j=j, sci=sci, setA=setA: e.scalar_tensor_tensor(
                    out=colv[:, setA, :], in0=modc[:, sci * 8:(sci + 1) * 8, j], scalar=1.0,
                    in1=badc[:, sci * 8:(sci + 1) * 8], op0=ALU.add, op1=ALU.add), reads=[mb], writes=[mb])
                fw.op("dve", lambda e, gi=gi, setA=setA: e.tensor_tensor(
                    out=colv[:, setA, :], in0=colv[:, setA, :], in1=gcol[:, gi, :], op=ALU.mult),
                    reads=[mb], writes=[mb])
                fw.op("dve", lambda e, j=j, shi=shi, setA=setA: e.tensor_tensor(
                    out=colv[:, setA + 1, :], in0=modc[:, shi * 8:(shi + 1) * 8, j],
                    in1=badc[:, shi * 8:(shi + 1) * 8], op=ALU.add), reads=[mb], writes=[mb])

    def norm_phase(li, which, src_ap_fn, tiles):
        for n, tt in enumerate(tiles):
            b = n % 2
            isctx = tt < 2
            fw.dma("sp", xt[b][:], src_ap_fn(tt), writes=[xtb[b]])
            fw.op("act", lambda e, b=b, tt=tt: e.activation(out=xn[b][:], in_=xt[b][:], func=AF.Square,
                                                          accum_out=stat[:, 0:1]),
                  reads=[xtb[b]], writes=[xnb[b], statb])
            fw.op("act", lambda e: e.activation(out=stat[:, 1:2], in_=stat[:, 0:1], func=AF.Ln,
                                                scale=1.0 / D, bias=epsc[:, 0:1]), reads=[statb, cb], writes=[statb])
            fw.op("act", lambda e: e.activation(out=stat[:, 2:3], in_=stat[:, 1:2], func=AF.Exp, scale=-0.5),
                  reads=[statb], writes=[statb])
            fw.op("dve", lambda e, b=b: e.tensor_scalar(out=xn[b][:], in0=xt[b][:], scalar1=stat[:, 2:3],
                                                       scalar2=None, op0=ALU.mult),
                  reads=[xtb[b], statb], writes=[xnb[b]])
            setA = which * 4 + (2 if isctx else 0)
            for half in range(2):
                pb = 6 + half
                for q in range(4):
                    kc = half * 4 + q
                    fw.op("pe", lambda e, b=b, kc=kc, q=q, pb=pb: e.transpose(
                        PS[pb][:, q * 128:(q + 1) * 128], xn[b][:, kc * 128:(kc + 1) * 128], ident[:]),
                        reads=[xnb[b], cb], writes=[PSB[pb]], inc=(q == 3), pe_acc=True)
                for q in range(4):
                    kc = half * 4 + q
                    if q % 2 == 0:
                        fw.op("act", lambda e, kc=kc, q=q, pb=pb, tt=tt, setA=setA: e.activation(
                            out=hT[:, kc, tt * 128:(tt + 1) * 128], in_=PS[pb][:, q * 128:(q + 1) * 128],
                            func=AF.Identity, scale=colv[:, setA, kc:kc + 1], bias=colv[:, setA + 1, kc:kc + 1]),
                            reads=[PSB[pb], mb], writes=[hTb[tt]])
                    else:
                        fw.op("dve", lambda e, kc=kc, q=q, pb=pb, tt=tt, setA=setA: e.tensor_scalar(
                            out=hT[:, kc, tt * 128:(tt + 1) * 128], in0=PS[pb][:, q * 128:(q + 1) * 128],
                            scalar1=colv[:, setA, kc:kc + 1], scalar2=colv[:, setA + 1, kc:kc + 1],
                            op0=ALU.mult, op1=ALU.add),
                            reads=[PSB[pb], mb], writes=[hTb[tt]])

    epsc = sb("epsc", [128, 1], F32)
    fw.op("dve", lambda e: e.memset(epsc[:], EPS), writes=[cb])

    def x_src0(tt):
        return I["ctx"][tt * 128:(tt + 1) * 128, :] if tt < 2 else I["x"][(tt - 2) * 128:(tt - 1) * 128, :]


    ARENA_ELEMS = 44200
    AR = sb("arena", [128, ARENA_ELEMS], BF16)
    aoff = [0]

    def carve(nelem_bf16):
        o = aoff[0]
        aoff[0] += nelem_bf16
        assert aoff[0] <= ARENA_ELEMS, aoff[0]
        return AR[:, o:o + nelem_bf16]

    QT = carve(4 * T).rearrange("p (a t) -> p a t", a=4)
    KT = carve(4 * T).rearrange("p (a t) -> p a t", a=4)
    VA = carve(NT * 520).rearrange("p (a t) -> p a t", a=NT)
    OTs1 = carve(4 * 512).rearrange("p (a t) -> p a t", a=4)
    ON = carve(4 * 512).rearrange("p (a t) -> p a t", a=4)
    OFr = carve(2048)
    OF = OFr.bitcast(F32).rearrange("p (m q d) -> p m q d", m=2, q=4)
    TB2r = AR[:, aoff[0]:aoff[0] + 8192]
    TB2 = TB2r.rearrange("p (i h q) -> p i h q", i=16, h=8)
    TM = [carve(1024).bitcast(F32) for i in range(4)]
    RS = [carve(1024).bitcast(F32) for i in range(2)]
    SQ = [carve(512) for i in range(2)]
    RAW = [carve(512) for i in range(2)]
    ETp = [carve(1024) for i in range(2)]
    ET = [ETp[0][:, 0:512], ETp[0][:, 512:1024], ETp[1][:, 0:512], ETp[1][:, 512:1024]]
    KRr = AR[:, 4 * T * 2 + NT * 520 + 2048: 4 * T * 2 + NT * 520 + 2048 + 4096]
    XXb = XX[:].rearrange("p a d -> p (a d)").bitcast(BF16)
    DQ = XXb[:, 0:2 * T].rearrange("p (a t) -> p a t", a=2)
    DKV = XXb[:, 2 * T:3 * T]
    QTb = [Buf() for _ in TBLK]
    KTb = [Buf() for _ in TBLK]
    VAb = [Buf() for _ in range(NT)]
    ETb = [Buf() for _ in range(4)]
    ecnt = [0]
    RAWb = [Buf(), Buf()]
    SQb = [Buf(), Buf()]
    RSb = [Buf(), Buf()]
    TMb = [Buf() for _ in range(4)]
    rcnt = [0]
    ONb = Buf()
    OFb = Buf()
    OTs = [OTs1]
    OTsb = [Buf()]
    otcnt = [0]
    OTd = nc.dram_tensor("OTd", [16, 128, T], BF16).ap()
    identb = sb("identb", [128, 128], BF16)
    onesb = sb("onesb", [128, 128], BF16)
    bd64 = sb("bd64", [128, 128], BF16)
    pm16 = sb("pm16", [128, 128], BF16)
    pm8 = sb("pm8", [128, 128], BF16)
    cos16 = sb("cos16", [128, S], BF16)
    sin16 = sb("sin16", [128, S], BF16)
    cos8 = sb("cos8", [128, S], BF16)
    sin8 = sb("sin8", [128, S], BF16)
    pcol = sb("pcol", [128, 16], F32)
    pcoli = sb("pcoli", [128, 8], I32)
    mcoli = XX[:, 0, 384:512].bitcast(I32)
    mcolf = XX[:, 0, 256:384]
    mtmp = XX[:, 0, 0:128]
    mtmp2 = XX[:, 0, 128:256]
    brv = sb("brv", [128, 16], F32)
    brb = Buf()
    lamrow = TM[0][:, 0:256].rearrange("p (a d) -> p a d", a=4)
    psA = [0]
    psX = [0]

    def ps_main():
        i = psA[0] % 4
        psA[0] += 1
        return i

    def ps_aux():
        i = 4 + psX[0] % 3
        psX[0] += 1
        return i

    fw.op("dve", lambda e: e.tensor_copy(out=identb[:], in_=ident[:]), reads=[cb], writes=[cb])
    fw.op("dve", lambda e: e.memset(onesb[:], 1.0), writes=[cb])
    fw.op("dve", lambda e: e.memset(bd64[:], 0.0), writes=[cb])
    fw.op("dve", lambda e: e.memset(bd64[0:64, 0:64], 1.0), writes=[cb])
    fw.op("dve", lambda e: e.memset(bd64[64:128, 64:128], 1.0), writes=[cb])
    fw.op("pool", lambda e: e.iota(mcoli[:], [[1, 128]], base=0, channel_multiplier=0), writes=[cb])
    fw.op("pool", lambda e: e.iota(pcoli[:, 0:1], [[0, 1]], base=0, channel_multiplier=1), writes=[cb])

    def build_pm(pm, h):
        fw.op("dve", lambda e: e.tensor_single_scalar(out=mcoli[:], in_=mcoli[:], scalar=h, op=ALU.bitwise_and),
              reads=[cb], writes=[cb])
        fw.op("dve", lambda e: e.tensor_copy(out=mcolf[:], in_=mcoli[:]), reads=[cb], writes=[cb])
        fw.op("dve", lambda e: e.tensor_single_scalar(out=mcolf[:], in_=mcolf[:], scalar=0.5, op=ALU.is_gt),
              reads=[cb], writes=[cb])
        fw.op("dve", lambda e: e.tensor_single_scalar(out=mtmp[:], in_=iot[:], scalar=float(h), op=ALU.is_equal),
              reads=[cb], writes=[cb])
        fw.op("dve", lambda e: e.tensor_tensor(out=mtmp[:], in0=mtmp[:], in1=mcolf[:], op=ALU.mult),
              reads=[cb], writes=[cb])
        fw.op("dve", lambda e: e.tensor_single_scalar(out=mtmp2[:], in_=iot[:], scalar=float(-h), op=ALU.is_equal),
              reads=[cb], writes=[cb])
        fw.op("dve", lambda e: e.tensor_scalar(out=mcolf[:], in0=mcolf[:], scalar1=-1.0, scalar2=1.0,
                                               op0=ALU.mult, op1=ALU.add), reads=[cb], writes=[cb])
        fw.op("dve", lambda e: e.tensor_tensor(out=mtmp2[:], in0=mtmp2[:], in1=mcolf[:], op=ALU.mult),
              reads=[cb], writes=[cb])
        fw.op("dve", lambda e: e.tensor_tensor(out=pm[:], in0=mtmp[:], in1=mtmp2[:], op=ALU.subtract),
              reads=[cb], writes=[cb])
        fw.op("pool", lambda e: e.iota(mcoli[:], [[1, 128]], base=0, channel_multiplier=0), reads=[cb], writes=[cb])

    fw.mark("cmat")
    build_pm(pm16, 16)
    build_pm(pm8, 8)
    fw.mark("pm")

    def build_rope(cosT, sinT, h):
        f32v = AR[:, 0:4 * T].bitcast(F32)
        f32w = AR[:, 4 * T:8 * T].bitcast(F32)
        Rr, Cc = f32v[:, 0:S], f32v[:, S:2 * S]
        U, Fr = f32w[:, 0:S], f32w[:, S:2 * S]
        Ui = AR[:, 8 * T:8 * T + 2 * S].bitcast(I32)
        fw.op("dve", lambda e: e.tensor_single_scalar(out=pcoli[:, 1:2], in_=pcoli[:, 0:1], scalar=h - 1,
                                                      op=ALU.bitwise_and), reads=[cb], writes=[cb])
        fw.op("dve", lambda e: e.tensor_single_scalar(out=pcoli[:, 2:3], in_=pcoli[:, 0:1], scalar=2 * h,
                                                      op=ALU.bitwise_and), reads=[cb], writes=[cb])
        fw.op("dve", lambda e: e.tensor_single_scalar(out=pcoli[:, 3:4], in_=pcoli[:, 0:1], scalar=h,
                                                      op=ALU.bitwise_and), reads=[cb], writes=[cb])
        fw.op("dve", lambda e: e.tensor_copy(out=pcol[:, 1:4], in_=pcoli[:, 1:4]), reads=[cb], writes=[cb])
        fw.op("act", lambda e: e.activation(out=pcol[:, 4:5], in_=pcol[:, 1:2], func=AF.Exp,
                                            scale=-math.log(10000.0) / h), reads=[cb], writes=[cb])
        fw.op("dve", lambda e: e.tensor_single_scalar(out=pcol[:, 5:6], in_=pcol[:, 2:3], scalar=0.5, op=ALU.is_gt),
              reads=[cb], writes=[cb])
        fw.op("dve", lambda e: e.tensor_scalar(out=pcol[:, 6:7], in0=pcol[:, 3:4], scalar1=0.5, scalar2=2.0,
                                               op0=ALU.is_gt, op1=ALU.mult), reads=[cb], writes=[cb])
        fw.op("dve", lambda e: e.tensor_scalar(out=pcol[:, 6:7], in0=pcol[:, 6:7], scalar1=-1.0, scalar2=None,
                                               op0=ALU.add), reads=[cb], writes=[cb])
        fw.op("pool", lambda e: e.iota(Rr, [[1, 32], [0, 64]], base=0, channel_multiplier=0,
                                      allow_small_or_imprecise_dtypes=True), reads=[cb], writes=[cb])
        fw.op("pool", lambda e: e.iota(Cc, [[0, 32], [1, 64]], base=0, channel_multiplier=0,
                                      allow_small_or_imprecise_dtypes=True), reads=[cb], writes=[cb])
        fw.op("dve", lambda e: e.tensor_tensor(out=Cc, in0=Cc, in1=Rr, op=ALU.subtract), reads=[cb], writes=[cb])
        fw.op("dve", lambda e: e.scalar_tensor_tensor(out=Rr, in0=Cc, scalar=pcol[:, 5:6], in1=Rr,
                                                      op0=ALU.mult, op1=ALU.add), reads=[cb], writes=[cb])
        for which, dst, off in ((0, sinT, 0.5), (1, cosT, 0.75)):
            fw.op("dve", lambda e: e.tensor_scalar(out=U, in0=Rr, scalar1=pcol[:, 4:5], scalar2=1.0 / (2 * math.pi),
                                                   op0=ALU.mult, op1=ALU.mult), reads=[cb], writes=[cb])
            fw.op("dve", lambda e, off=off: e.tensor_scalar(out=U, in0=U, scalar1=off, scalar2=None, op0=ALU.add),
                  reads=[cb], writes=[cb])
            fw.op("dve", lambda e: e.tensor_copy(out=Ui, in_=U), reads=[cb], writes=[cb])
            fw.op("dve", lambda e: e.tensor_copy(out=Fr, in_=Ui), reads=[cb], writes=[cb])
            fw.op("dve", lambda e: e.tensor_tensor(out=Fr, in0=U, in1=Fr, op=ALU.subtract), reads=[cb], writes=[cb])
            fw.op("dve", lambda e: e.tensor_single_scalar(out=U, in_=Fr, scalar=0.0, op=ALU.is_lt),
                  reads=[cb], writes=[cb])
            fw.op("dve", lambda e: e.tensor_tensor(out=Fr, in0=Fr, in1=U, op=ALU.add), reads=[cb], writes=[cb])
            fw.op("dve", lambda e: e.tensor_single_scalar(out=U, in_=Fr, scalar=1.0, op=ALU.is_ge),
                  reads=[cb], writes=[cb])
            fw.op("dve", lambda e: e.tensor_tensor(out=Fr, in0=Fr, in1=U, op=ALU.subtract), reads=[cb], writes=[cb])
            fw.op("dve", lambda e: e.tensor_scalar(out=Fr, in0=Fr, scalar1=-0.5, scalar2=2 * math.pi * (1 - 1e-6),
                                                   op0=ALU.add, op1=ALU.mult), reads=[cb], writes=[cb])
            fw.op("act", lambda e, dst=dst: e.activation(out=dst[:], in_=Fr, func=AF.Sin),
                  reads=[cb], writes=[cb])

    build_rope(cos16, sin16, 16)
    build_rope(cos8, sin8, 8)
    fw.barrier()
    fw.mark("rope")

    def evac_copy(n, dst, src, reads, writes):
        if n % 2 == 0:
            fw.op("act", lambda e: e.activation(out=dst, in_=src, func=AF.Copy), reads=reads, writes=writes)
        else:
            fw.op("dve", lambda e: e.tensor_copy(out=dst, in_=src), reads=reads, writes=writes)

    def blocks_for(li):
        return list(range(5))

    def proj_fm(li, col0, nchunks, post, tbs, wsrc=None, krows=1024, rhs_fn=None, rbufs_fn=None, group=1):
        c = 0
        while c < nchunks:
            nload = min(4, nchunks - c)
            src = (wsrc if wsrc is not None else I["w_in"][li])[:, col0 + c * 128: col0 + (c + nload) * 128]
            w, wb = load_w(src, nload * 128, krows)
            nk = krows // 128
            for g0 in range(0, nload, group):
                for tb in tbs:
                    t0, tn = TBLK[tb]
                    pis = []
                    for gi in range(group):
                        cc = g0 + gi
                        pi = ps_main()
                        pis.append(pi)
                        for kc in range(nk):
                            rhs = rhs_fn(kc, t0, tn) if rhs_fn else hT[:, kc, t0:t0 + tn]
                            rb = rbufs_fn(tb) if rbufs_fn else [hTb[t0 // 128 + q] for q in range(tn // 128)]
                            fw.op("pe", lambda e, pi=pi, cc=cc, kc=kc, rhs=rhs, tn=tn, w=w, nk=nk: e.matmul(
                                PS[pi][:, 0:tn], w[:, kc, cc * 128:(cc + 1) * 128], rhs,
                                start=(kc == 0), stop=(kc == nk - 1)),
                                reads=[wb] + rb, writes=[PSB[pi]], inc=(kc == nk - 1), pe_acc=True)
                    post(c + g0, tb, pis)
            c += nload

    def post_plain(dst, dstb):
        cnt = [0]

        def f(ci, tb, pis):
            t0, tn = TBLK[tb]
            cnt[0] += 1
            evac_copy(cnt[0], dst[:, ci, t0:t0 + tn], PS[pis[0]][:, 0:tn], [PSB[pis[0]]], [dstb[tb]])
        return f

    def rope_apply(src_ps, src_psb, raw_i, dst, dstb, t0, tn, cosT, sinT, pm, prange=(0, 128)):
        p0, p1 = prange
        s0 = t0 - C
        pa = ps_aux()
        fw.op("pe", lambda e: e.matmul(PS[pa][p0:p1, 0:tn], pm[p0:p1, p0:p1], RAW[raw_i][p0:p1, 0:tn],
                                       start=True, stop=True),
              reads=[RAWb[raw_i], cb], writes=[PSB[pa]])
        a, b = rcnt[0] % 4, (rcnt[0] + 1) % 4
        rcnt[0] += 2
        if src_ps is not None:
            fw.op("dve", lambda e: e.tensor_tensor(out=TM[a][p0:p1, 0:tn], in0=src_ps[p0:p1, 0:tn],
                                                   in1=cosT[p0:p1, s0:s0 + tn], op=ALU.mult),
                  reads=[src_psb, cb], writes=[TMb[a]])
        else:
            fw.op("pool", lambda e: e.tensor_tensor(out=TM[a][p0:p1, 0:tn], in0=RAW[raw_i][p0:p1, 0:tn],
                                                    in1=cosT[p0:p1, s0:s0 + tn], op=ALU.mult),
                  reads=[RAWb[raw_i], cb], writes=[TMb[a]])
        fw.op("dve", lambda e: e.tensor_tensor(out=TM[b][p0:p1, 0:tn], in0=PS[pa][p0:p1, 0:tn],
                                               in1=sinT[p0:p1, s0:s0 + tn], op=ALU.mult),
              reads=[PSB[pa], cb], writes=[TMb[b]])
        fw.op("pool", lambda e: e.tensor_tensor(out=dst[p0:p1], in0=TM[a][p0:p1, 0:tn], in1=TM[b][p0:p1, 0:tn],
                                                op=ALU.add),
              reads=[TMb[a], TMb[b]], writes=[dstb])

    def post_rope(dst, dstb, cosT, sinT, pm):
        cnt = [0]

        def f(ci, tb, pis):
            t0, tn = TBLK[tb]
            pi = pis[0]
            if tb == 0:
                cnt[0] += 1
                evac_copy(cnt[0], dst[:, ci, t0:t0 + tn], PS[pi][:, 0:tn], [PSB[pi]], [dstb[tb]])
                return
            r = cnt[0] % 2
            cnt[0] += 1
            fw.op("act", lambda e: e.activation(out=RAW[r][:, 0:tn], in_=PS[pi][:, 0:tn], func=AF.Copy),
                  reads=[PSB[pi]], writes=[RAWb[r]])
            rope_apply(None, None, r, dst[:, ci, t0:t0 + tn], dstb[tb], t0, tn, cosT, sinT, pm)
        return f

    def post_norm(dst, dstb, redmat, cnt_feat, gcol_fn, rope=None, dst_idx=None):
        cnt = [0]

        def f(ci, tb, pis):
            t0, tn = TBLK[tb]
            pa = ps_aux()
            sqs = []
            for n, pi in enumerate(pis):
                r = cnt[0] % 2
                cnt[0] += 1
                sqs.append(r)
                fw.op("act", lambda e, r=r, pi=pi: e.activation(out=SQ[r][:, 0:tn], in_=PS[pi][:, 0:tn],
                                                                func=AF.Square),
                      reads=[PSB[pi]], writes=[SQb[r]])
            for n, r in enumerate(sqs):
                fw.op("pe", lambda e, r=r, n=n: e.matmul(PS[pa][:, 0:tn], redmat[:, :], SQ[r][:, 0:tn],
                                                         start=(n == 0), stop=(n == len(sqs) - 1)),
                      reads=[SQb[r], cb], writes=[PSB[pa]], inc=(n == len(sqs) - 1), pe_acc=True)
            rs = cnt[0] % 2
            fw.op("act", lambda e: e.activation(out=RS[rs][:, 0:tn], in_=PS[pa][:, 0:tn], func=AF.Ln,
                                                scale=1.0 / cnt_feat, bias=epsc[:, 0:1]),
                  reads=[PSB[pa], cb], writes=[RSb[rs]])
            fw.op("act", lambda e: e.activation(out=RS[rs][:, 0:tn], in_=RS[rs][:, 0:tn], func=AF.Exp, scale=-0.5),
                  reads=[RSb[rs]], writes=[RSb[rs]])
            for n, pi in enumerate(pis):
                cidx = (ci + n) if dst_idx is None else dst_idx(ci + n)
                g = gcol_fn(ci + n)
                if rope is None or tb == 0:
                    fw.op("dve", lambda e, pi=pi, cidx=cidx, g=g: e.scalar_tensor_tensor(
                        out=dst[:, cidx, t0:t0 + tn], in0=PS[pi][:, 0:tn], scalar=g, in1=RS[rs][:, 0:tn],
                        op0=ALU.mult, op1=ALU.mult), reads=[PSB[pi], RSb[rs], brb], writes=[dstb[tb]])
                else:
                    r = cnt[0] % 2
                    cnt[0] += 1
                    fw.op("dve", lambda e, pi=pi, r=r, g=g: e.scalar_tensor_tensor(
                        out=RAW[r][:, 0:tn], in0=PS[pi][:, 0:tn], scalar=g, in1=RS[rs][:, 0:tn],
                        op0=ALU.mult, op1=ALU.mult), reads=[PSB[pi], RSb[rs], brb], writes=[RAWb[r]])
                    cosT, sinT, pm = rope
                    rope_apply(None, None, r, dst[:, cidx, t0:t0 + tn], dstb[tb], t0, tn, cosT, sinT, pm)
        return f

    def proj_v(li, col0, nheads, dv, wsrc=None):
        ncols = nheads * dv
        src = (wsrc if wsrc is not None else I["w_in"][li])[:, col0:col0 + ncols]
        w, wb = load_w(src, ncols)
        for tt in range(NT):
            pi = ps_main()
            for kc in range(8):
                fw.op("pe", lambda e, pi=pi, kc=kc, tt=tt: e.matmul(
                    PS[pi][:, 0:ncols], hT[:, kc, tt * 128:(tt + 1) * 128], w[:, kc, 0:ncols],
                    start=(kc == 0), stop=(kc == 7)),
                    reads=[wb, hTb[tt]], writes=[PSB[pi]], inc=(kc == 7), pe_acc=True)
            va = VA[:, tt, 0:nheads * (dv + 1)].rearrange("p (h d) -> p h d", d=dv + 1)
            evac_copy(tt, va[:, :, 0:dv], PS[pi][:, 0:ncols].rearrange("p (h d) -> p h d", d=dv),
                      [PSB[pi]], [VAb[tt]])
            fw.op("pool", lambda e, va=va: e.memset(va[:, :, dv:dv + 1], 1.0), writes=[VAb[tt]])

    pvset = [0]

    def attn_head(steps, qap_fn, kap_fn, vcol, dv, scale, q0, nq, kcs, finish, vap_fn=None):
        nqt = nq // 128
        pset = pvset[0] % 2
        pvset[0] += 1
        pvb = [4 + 2 * pset, 5 + 2 * pset]
        W = dv + 1
        for n, kc in enumerate(kcs):
            steps.append(dict(k=kap_fn(kc), q=qap_fn(q0, nq), nq=nq, scale=scale, nqt=nqt, pvb=pvb, W=W,
                              v=(vap_fn(kc) if vap_fn else VA[:, kc, vcol:vcol + W]), first=(n == 0),
                              last=(n == len(kcs) - 1), finish=finish, mask=None))

    def run_steps(steps, look=3):
        def qk(st):
            si = ecnt[0] % 4
            ecnt[0] += 1
            st["si"] = si
            fw.op("pe", lambda e: e.matmul(PS[si][:, 0:st["nq"]], st["k"], st["q"], start=True, stop=True),
                  reads=[], writes=[PSB[si]])

        def rest(st):
            si = st["si"]
            nq, nqt, W, pvb = st["nq"], st["nqt"], st["W"], st["pvb"]
            fw.op("act", lambda e: e.activation(out=ET[si][:, 0:nq], in_=PS[si][:, 0:nq], func=AF.Exp,
                                                scale=st["scale"]),
                  reads=[PSB[si]], writes=[ETb[si]])
            for qt in range(nqt):
                bk = pvb[qt // 2]
                col = (qt % 2) * W
                stf = st["first"] and (qt % 2 == 0)
                last = st["last"]
                fw.op("pe", lambda e, bk=bk, col=col, qt=qt, stf=stf, last=last: e.matmul(
                    PS[bk][:, col:col + W], ET[si][:, qt * 128:(qt + 1) * 128], st["v"],
                    start=stf, stop=last, skip_group_check=True),
                    reads=[ETb[si]], writes=[PSB[bk]], inc=(last and (qt == nqt - 1 or qt % 2 == 1)), pe_acc=True)
            if st["last"]:
                st["finish"](pvb, nqt, W)

        n = len(steps)
        look = 4
        for i in range(min(look, n)):
            qk(steps[i])
        for i in range(0, n, 2):
            rest(steps[i])
            if i + 1 < n:
                rest(steps[i + 1])
            for j in (i + look, i + look + 1):
                if j < n:
                    qk(steps[j])

    def interleave(a_, b_):
        o_ = []
        for x_, y_ in zip(a_, b_):
            o_ += [x_, y_]
        return o_

    def finish_plain(h, dv):
        def f(pvb, nqt, W):
            for half in range((nqt + 1) // 2):
                bk = pvb[half]
                nq2 = min(2, nqt - half * 2)
                acc = PS[bk][:, 0:nq2 * W].rearrange("p (q w) -> p q w", w=W)
                fw.op("dve", lambda e, acc=acc, nq2=nq2: e.reciprocal(out=stat[:, 8:8 + nq2], in_=acc[:, :, dv]),
                      reads=[PSB[bk]], writes=[statb])
                fw.op("dve", lambda e, acc=acc, nq2=nq2, half=half: e.tensor_tensor(
                    out=ON[:, half * 2:half * 2 + nq2, h * dv:(h + 1) * dv], in0=acc[:, :, 0:dv],
                    in1=stat[:, 8:8 + nq2].unsqueeze(2).broadcast_to([128, nq2, dv]), op=ALU.mult),
                    reads=[PSB[bk], statb], writes=[ONb])
        return f

    def flush_o(branch, q0, nq, scale_col=None, ecs=(0, 1, 2, 3)):
        nqt = nq // 128
        si = 0
        for ec in ecs:
            pb = 0
            psb16 = PS[pb][:].bitcast(BF16)
            for qt in range(nqt):
                fw.op("pe", lambda e, qt=qt, ec=ec: e.transpose(psb16[:, qt * 128:(qt + 1) * 128],
                                                               ON[:, qt, ec * 128:(ec + 1) * 128], identb[:]),
                      reads=[ONb, cb], writes=[PSB[pb]], inc=(qt == nqt - 1), pe_acc=True)
            if scale_col is None:
                evac_copy(ec, OTs[si][:, ec, 0:nq], psb16[:, 0:nq], [PSB[pb]], [OTsb[si]])
            else:
                fw.op("dve", lambda e, ec=ec: e.tensor_scalar(out=OTs[si][:, ec, 0:nq], in0=psb16[:, 0:nq],
                                                             scalar1=scale_col, scalar2=None, op0=ALU.mult),
                      reads=[PSB[pb], brb], writes=[OTsb[si]])
        fw.dma("sp", OTd[branch * 4 + ecs[0]:branch * 4 + ecs[-1] + 1, :, q0:q0 + nq].rearrange("c p q -> p c q"),
               OTs[si][:, ecs[0]:ecs[-1] + 1, 0:nq], reads=[OTsb[si]])

    ALLK = list(range(NT))
    CTXK = [0, 1]

    def qblocks(li):
        return [0, 1, 2, 3, 4] if li == 0 else [1, 2, 3, 4]

    def branch_a(li):
        lam_init = 0.8 - 0.6 * math.exp(-0.3 * li)
        for n, nm in enumerate(("lam_q1", "lam_k1", "lam_q2", "lam_k2")):
            fw.dma("sp", lamrow[:, n, :], I[nm][li:li + 1, :].broadcast_to([128, 64]), writes=[brb])
        fw.dma("sp", brv[:, 5:6], I["g_diff_sub"][li:li + 1, :].rearrange("o d -> d o"), writes=[brb],
               allow_slow_non_contiguous=True)
        for n in range(2):
            fw.op("dve", lambda e, n=n: e.tensor_tensor(out=lamrow[:, 2 * n, :], in0=lamrow[:, 2 * n, :],
                                                       in1=lamrow[:, 2 * n + 1, :], op=ALU.mult),
                  reads=[brb], writes=[brb])
            fw.op("dve", lambda e, n=n: e.reduce_sum(out=stat[:, 20 + n:21 + n], in_=lamrow[:, 2 * n, :],
                                                    axis=AX.X), reads=[brb], writes=[brb])
        fw.op("act", lambda e: e.activation(out=stat[:, 20:22], in_=stat[:, 20:22], func=AF.Exp),
              reads=[brb], writes=[brb])
        fw.op("dve", lambda e: e.tensor_tensor(out=stat[:, 22:23], in0=stat[:, 21:22], in1=stat[:, 20:21],
                                               op=ALU.subtract), reads=[brb], writes=[brb])
        fw.op("dve", lambda e: e.tensor_scalar(out=brv[:, 6:7], in0=stat[:, 22:23], scalar1=-lam_init,
                                               scalar2=None, op0=ALU.add), reads=[brb], writes=[brb])
        fw.op("dve", lambda e: e.tensor_scalar(out=brv[:, 5:6], in0=brv[:, 5:6], scalar1=1.0 - lam_init,
                                               scalar2=None, op0=ALU.mult), reads=[brb], writes=[brb])
        proj_fm(li, O_AQ, 4, post_rope(QT, QTb, cos16, sin16, pm16), qblocks(li))
        proj_fm(li, O_AK, 4, post_rope(KT, KTb, cos16, sin16, pm16), range(5))
        proj_v(li, O_AV, 4, 128)
        fw.barrier()
        fw.mark("a_kv")

        def finish_a(h, m):
            def f(pvb, nqt, W):
                for half in range((nqt + 1) // 2):
                    bk = pvb[half]
                    nq2 = min(2, nqt - half * 2)
                    acc = PS[bk][:, 0:nq2 * W].rearrange("p (q w) -> p q w", w=W)
                    fw.op("dve", lambda e, acc=acc, nq2=nq2: e.reciprocal(out=stat[:, 8:8 + nq2], in_=acc[:, :, 128]),
                          reads=[PSB[bk]], writes=[statb])
                    fw.op("dve", lambda e, acc=acc, nq2=nq2, half=half: e.tensor_tensor(
                        out=OF[:, m, half * 2:half * 2 + nq2, :], in0=acc[:, :, 0:128],
                        in1=stat[:, 8:8 + nq2].unsqueeze(2).broadcast_to([128, nq2, 128]), op=ALU.mult),
                        reads=[PSB[bk], statb], writes=[OFb])
                if m == 1:
                    fw.op("dve", lambda e: e.scalar_tensor_tensor(
                        out=OF[:, 0, 0:nqt, :], in0=OF[:, 1, 0:nqt, :], scalar=brv[:, 6:7], in1=OF[:, 0, 0:nqt, :],
                        op0=ALU.mult, op1=ALU.add), reads=[OFb, brb], writes=[OFb])
                    fw.op("pool", lambda e: e.tensor_tensor(out=OF[:, 1, 0:nqt, :], in0=OF[:, 0, 0:nqt, :],
                                                            in1=OF[:, 0, 0:nqt, :], op=ALU.mult),
                          reads=[OFb], writes=[OFb])
                    fw.op("dve", lambda e: e.reduce_sum(out=stat[:, 16:16 + nqt], in_=OF[:, 1, 0:nqt, :], axis=AX.X),
                          reads=[OFb], writes=[statb])
                    fw.op("act", lambda e: e.activation(out=stat[:, 16:16 + nqt], in_=stat[:, 16:16 + nqt],
                                                        func=AF.Ln, scale=1.0 / 128, bias=epsc[:, 0:1]),
                          reads=[statb, cb], writes=[statb])
                    fw.op("act", lambda e: e.activation(out=stat[:, 16:16 + nqt], in_=stat[:, 16:16 + nqt],
                                                        func=AF.Exp, scale=-0.5), reads=[statb], writes=[statb])
                    fw.op("dve", lambda e: e.tensor_tensor(
                        out=ON[:, 0:nqt, h * 128:(h + 1) * 128], in0=OF[:, 0, 0:nqt, :],
                        in1=stat[:, 16:16 + nqt].unsqueeze(2).broadcast_to([128, nqt, 128]), op=ALU.mult),
                        reads=[OFb, statb], writes=[ONb])
            return f

        for tb in qblocks(li):
            q0, nq = TBLK[tb]
            kcs = CTXK if tb == 0 else ALLK
            steps = []
            for h in range(4):
                sm = [[], []]
                for m in range(2):
                    attn_head(sm[m], lambda a, n, h=h, m=m: QT[m * 64:(m + 1) * 64, h, a:a + n],
                              lambda kc, h=h, m=m: KT[m * 64:(m + 1) * 64, h, kc * 128:(kc + 1) * 128],
                              h * 129, 128, 0.125, q0, nq, kcs, finish_a(h, m))
                steps += interleave(sm[0], sm[1])
            run_steps(steps)
            flush_o(0, q0, nq, scale_col=brv[:, 5:6])
        fw.barrier()

    KRt = sb("KRt", [32, T], BF16)
    DQb = [Buf() for _ in TBLK]
    DKVb = [Buf() for _ in TBLK]
    KRb = Buf()

    def branch_d(li):
        fw.dma("sp", brv[:, 2:4], I["g_q_lora"][li].rearrange("(c d) -> d c", d=128), writes=[brb],
               allow_slow_non_contiguous=True)
        fw.dma("sp", brv[:, 4:5], I["g_kv_lora"][li:li + 1, :].rearrange("o d -> d o"), writes=[brb],
               allow_slow_non_contiguous=True)
        DKV3 = DKV.rearrange("p (a t) -> p a t", a=1)
        KR = KRt[:, :]
        proj_fm(li, O_DQA, 2, post_norm(DQ, DQb, onesb, 256, lambda c: brv[:, 2 + c:3 + c]), qblocks(li), group=2)
        proj_fm(li, O_DKVA, 1, post_norm(DKV3, DKVb, onesb, 128, lambda c: brv[:, 4:5]), range(5))
        w, wb = load_w(I["w_in"][li][:, O_DKR:O_DKR + 32], 32)
        for tb in range(5):
            t0, tn = TBLK[tb]
            pi = ps_main()
            for kc in range(8):
                fw.op("pe", lambda e, pi=pi, kc=kc, t0=t0, tn=tn, w=w: e.matmul(
                    PS[pi][0:32, 0:tn], w[:, kc, 0:32], hT[:, kc, t0:t0 + tn], start=(kc == 0), stop=(kc == 7)),
                    reads=[wb] + [hTb[t0 // 128 + q] for q in range(tn // 128)], writes=[PSB[pi]],
                    inc=(kc == 7), pe_acc=True)
            if tb == 0:
                fw.op("act", lambda e, pi=pi, t0=t0, tn=tn: e.activation(out=KR[0:32, t0:t0 + tn],
                                                                       in_=PS[pi][0:32, 0:tn], func=AF.Copy),
                      reads=[PSB[pi]], writes=[KRb])
            else:
                r = tb % 2
                fw.op("act", lambda e, pi=pi, r=r, tn=tn: e.activation(out=RAW[r][0:32, 0:tn], in_=PS[pi][0:32, 0:tn],
                                                                     func=AF.Copy), reads=[PSB[pi]], writes=[RAWb[r]])
                rope_apply(None, None, r, KR[:, t0:t0 + tn], KRb, t0, tn, cos8, sin8, pm8, prange=(0, 32))
        iq = wcnt[0] % 3
        wcnt[0] += 1
        wuq = wt[iq][:].rearrange("p k c -> p (k c)")[:, 0:1536].rearrange("p (k c) -> p k c", k=2)
        fw.dma("pool", wuq, I["w_uq"][li].rearrange("(k p) c -> p k c", p=128), writes=[wtb[iq]])
        ik = wcnt[0] % 3
        wcnt[0] += 1
        wukv = wt[ik][:].rearrange("p k c -> p (k c)")[:, 0:1024]
        fw.dma("pool", wukv, I["w_ukv"][li], writes=[wtb[ik]])
        wv = wukv.rearrange("p (h e) -> p h e", e=128)[:, :, 64:128]
        for tt in range(NT):
            pi = ps_main()
            fw.op("pe", lambda e, pi=pi, tt=tt: e.matmul(PS[pi][:, :], DKV[:, tt * 128:(tt + 1) * 128], wv,
                                                         start=True, stop=True),
                  reads=[wtb[ik], DKVb[0], DKVb[1], DKVb[2], DKVb[3], DKVb[4]], writes=[PSB[pi]])
            va = VA[:, tt, 0:520].rearrange("p (h d) -> p h d", d=65)
            evac_copy(tt, va[:, :, 0:64], PS[pi][:, :].rearrange("p (h d) -> p h d", d=64), [PSB[pi]], [VAb[tt]])
            fw.op("pool", lambda e, va=va: e.memset(va[:, :, 64:65], 1.0), writes=[VAb[tt]])
        sc_d = 96.0 ** -0.5
        for g in range(2):
            for hl in range(4):
                h = g * 4 + hl
                for tb in range(5):
                    t0, tn = TBLK[tb]
                    pi = ps_main()
                    fw.op("pe", lambda e, pi=pi, h=h, t0=t0, tn=tn: e.matmul(
                        PS[pi][0:64, 0:tn], wukv[:, h * 128:h * 128 + 64], DKV[:, t0:t0 + tn], start=True, stop=True),
                        reads=[wtb[ik], DKVb[tb]], writes=[PSB[pi]])
                    evac_copy(tb, KT[0:64, hl, t0:t0 + tn], PS[pi][0:64, 0:tn], [PSB[pi]], [KTb[tb]])
                    if tb not in qblocks(li):
                        continue
                    pq = ps_main()
                    for c in range(2):
                        fw.op("pe", lambda e, pq=pq, c=c, h=h, t0=t0, tn=tn: e.matmul(
                            PS[pq][0:96, 0:tn], wuq[:, c, h * 96:(h + 1) * 96], DQ[:, c, t0:t0 + tn],
                            start=(c == 0), stop=(c == 1)),
                            reads=[wtb[iq], DQb[tb]], writes=[PSB[pq]], inc=(c == 1), pe_acc=True)
                    if tb == 0:
                        evac_copy(hl, QT[0:96, hl, t0:t0 + tn], PS[pq][0:96, 0:tn], [PSB[pq]], [QTb[tb]])
                    else:
                        evac_copy(hl, QT[0:64, hl, t0:t0 + tn], PS[pq][0:64, 0:tn], [PSB[pq]], [QTb[tb]])
                        r = (hl + tb) % 2
                        fw.op("act", lambda e, pq=pq, r=r, tn=tn: e.activation(
                            out=RAW[r][64:96, 0:tn], in_=PS[pq][64:96, 0:tn], func=AF.Copy),
                            reads=[PSB[pq]], writes=[RAWb[r]])
                        pa = ps_aux()
                        fw.op("pe", lambda e, pa=pa, r=r, tn=tn: e.matmul(
                            PS[pa][64:96, 0:tn], pm8[64:96, 64:96], RAW[r][64:96, 0:tn], start=True, stop=True),
                            reads=[RAWb[r], cb], writes=[PSB[pa]])
                        a_, b_ = rcnt[0] % 4, (rcnt[0] + 1) % 4
                        rcnt[0] += 2
                        s0 = t0 - C
                        fw.op("pool", lambda e, r=r, a_=a_, s0=s0, tn=tn: e.tensor_tensor(
                            out=TM[a_][64:96, 0:tn], in0=RAW[r][64:96, 0:tn], in1=cos8[64:96, s0:s0 + tn],
                            op=ALU.mult), reads=[RAWb[r], cb], writes=[TMb[a_]])
                        fw.op("dve", lambda e, pa=pa, b_=b_, s0=s0, tn=tn: e.tensor_tensor(
                            out=TM[b_][64:96, 0:tn], in0=PS[pa][64:96, 0:tn], in1=sin8[64:96, s0:s0 + tn],
                            op=ALU.mult), reads=[PSB[pa], cb], writes=[TMb[b_]])
                        fw.op("pool", lambda e, a_=a_, b_=b_, hl=hl, t0=t0, tn=tn: e.tensor_tensor(
                            out=QT[64:96, hl, t0:t0 + tn], in0=TM[a_][64:96, 0:tn], in1=TM[b_][64:96, 0:tn],
                            op=ALU.add), reads=[TMb[a_], TMb[b_]], writes=[QTb[tb]])
            fw.barrier()
            for hl in range(4):
                fw.dma("sp", KT[64:96, hl, :], KR[0:32, :])
            fw.barrier()
            for tb in qblocks(li):
                q0, nq = TBLK[tb]
                kcs = CTXK if tb == 0 else ALLK
                steps = []
                for hl in range(4):
                    h = g * 4 + hl
                    attn_head(steps, lambda a, n, hl=hl: QT[0:96, hl, a:a + n],
                              lambda kc, hl=hl: KT[0:96, hl, kc * 128:(kc + 1) * 128],
                              h * 65, 64, sc_d, q0, nq, kcs, finish_plain(h, 64))
                run_steps(steps)
                flush_o(3, q0, nq, ecs=(2 * g, 2 * g + 1))
            fw.barrier()

    def build_tb2(li):
        rp32 = ET[0].bitcast(F32)
        rpb16 = ET[1]
        IE = ET[2]
        winf = OFr.bitcast(F32)
        fw.dma("sp", rp32[0:31, 0:120], I["na_rpb"][li].rearrange("h r c -> c (h r)"), writes=[ETb[0]],
               allow_slow_non_contiguous=True)
        fw.op("dve", lambda e: e.tensor_copy(out=rpb16[0:31, 0:120], in_=rp32[0:31, 0:120]),
              reads=[ETb[0]], writes=[ETb[1]])
        fw.op("dve", lambda e: e.tensor_single_scalar(out=IE[0:31, 0:128], in_=iot[0:31, :], scalar=48.0,
                                                      op=ALU.is_equal), reads=[cb], writes=[ETb[2]])
        fw.op("dve", lambda e: e.tensor_copy(out=pcol[:, 7:8], in_=pcoli[:, 0:1]), reads=[cb], writes=[cb])
        qc_ = winf[0:64, 0:64]
        fw.op("dve", lambda e: e.tensor_scalar(out=qc_, in0=iot[0:64, 0:64], scalar1=pcol[0:64, 7:8], scalar2=-8.0,
                                               op0=ALU.add, op1=ALU.add), reads=[cb], writes=[OFb])
        fw.op("dve", lambda e: e.tensor_scalar(out=qc_, in0=qc_, scalar1=0.0, scalar2=48.0,
                                               op0=ALU.max, op1=ALU.min), reads=[OFb], writes=[OFb])
        fw.op("dve", lambda e: e.tensor_scalar(out=qc_, in0=qc_, scalar1=pcol[0:64, 7:8], scalar2=None,
                                               op0=ALU.subtract), reads=[OFb, cb], writes=[OFb])
        m1 = winf[0:64, 64:128]
        fw.op("dve", lambda e: e.tensor_single_scalar(out=m1, in_=qc_, scalar=0.0, op=ALU.is_le),
              reads=[OFb], writes=[OFb])
        fw.op("dve", lambda e: e.tensor_single_scalar(out=qc_, in_=qc_, scalar=-16.0, op=ALU.is_gt),
              reads=[OFb], writes=[OFb])
        fw.op("dve", lambda e: e.tensor_tensor(out=m1, in0=m1, in1=qc_, op=ALU.mult), reads=[OFb], writes=[OFb])
        tbb = Buf()
        for q0 in range(0, 64, 4):
            pi = ps_main()
            for ql in range(4):
                qc = q0 + ql
                fw.op("pe", lambda e, pi=pi, ql=ql, qc=qc: e.matmul(
                    PS[pi][0:64, ql * 120:(ql + 1) * 120], IE[0:31, 63 - qc:127 - qc], rpb16[0:31, 0:120],
                    start=True, stop=True), reads=[ETb[1], ETb[2]], writes=[PSB[pi]], inc=(ql == 3), pe_acc=True)
            fw.op("act", lambda e, pi=pi, q0=q0: e.activation(
                out=TB2[0:64, 1:16, :, q0:q0 + 4].rearrange("p r h q -> p q h r"),
                in_=PS[pi][0:64, 0:480].rearrange("p (q h r) -> p q h r", q=4, h=8), func=AF.Exp),
                reads=[PSB[pi]], writes=[tbb])
        fw.op("dve", lambda e: e.tensor_tensor(
            out=TB2[0:64, 1:16, :, :].rearrange("p i h q -> p (i h) q"),
            in0=TB2[0:64, 1:16, :, :].rearrange("p i h q -> p (i h) q"),
            in1=m1.unsqueeze(1).broadcast_to([64, 120, 64]), op=ALU.mult), reads=[tbb, OFb], writes=[tbb])
        fw.op("dve", lambda e: e.memset(TB2[0:64, 0, :, :], 0.0), writes=[tbb])
        fw.op("dve", lambda e: e.memset(TB2[64:128, 15, :, :], 0.0), writes=[tbb])
        fw.dma("sp", TB2[64:128, 0:15, :, :], TB2[0:64, 1:16, :, :], reads=[tbb], writes=[tbb])

    def branch_b(li):
        proj_fm(li, O_BQ, 4, post_plain(QT, QTb), qblocks(li))
        proj_fm(li, O_BK, 4, post_plain(KT, KTb), range(5))
        proj_v(li, O_BV, 8, 64)
        fw.barrier()
        fw.mark("b_proj")
        build_tb2(li)
        fw.barrier()
        fw.mark("b_tb2")
        if li == 0:
            steps = []
            for hg in range(4):
                sm = [[], []]
                for hh in range(2):
                    h = 2 * hg + hh
                    pb = (h % 2) * 64
                    attn_head(sm[hh], lambda a, n, h=h, pb=pb: QT[pb:pb + 64, h // 2, a:a + n],
                              lambda kc, h=h, pb=pb: KT[pb:pb + 64, h // 2, kc * 128:(kc + 1) * 128],
                              h * 65, 64, 0.125, 0, 256, CTXK, finish_plain(h, 64))
                steps += interleave(sm[0], sm[1])
            run_steps(steps)
            flush_o(1, 0, 256)
            fw.barrier()
            fw.mark("b_ctx")
        items = []
        for a in range(16):
            pset = pvset[0] % 2
            pvset[0] += 1
            pvb = [4 + 2 * pset, 5 + 2 * pset]
            for rl in range(2):
                r = 2 * a + rl
                s_ = min(max(r - 4, 0), 24)
                chunks = [("c", 0), ("c", 1)] + [("w", ap) for ap in range(s_ // 2, (s_ + 7) // 2 + 1)]
                for n, (kind, idx) in enumerate(chunks):
                    items.append(dict(a=a, rl=rl, r=r, s=s_, n=n, kind=kind, idx=idx, pvb=pvb,
                                      last=(n == len(chunks) - 1)))

        def b_qk(it, i):
            bpair = ((0, 1), (2, 3))[i % 2]
            it["bpair"] = bpair
            kcol = it["idx"] * 128 if it["kind"] == "c" else C + it["idx"] * 128
            qcol = C + it["r"] * 64
            for h in range(8):
                hp = (h % 2) * 64
                bk_s = bpair[h % 2]
                g_ = h // 2
                fw.op("pe", lambda e, bk_s=bk_s, g_=g_, h=h, hp=hp, kcol=kcol, qcol=qcol: e.matmul(
                    PS[bk_s][:, g_ * 64:(g_ + 1) * 64], KT[hp:hp + 64, h // 2, kcol:kcol + 128],
                    QT[hp:hp + 64, h // 2, qcol:qcol + 64], start=True, stop=True),
                    reads=[], writes=[PSB[bk_s]], inc=(h >= 6), pe_acc=True)

        def b_rest(it, i):
            si = i % 3
            bpair = it["bpair"]
            r, s_, idx, kind, pvb = it["r"], it["s"], it["idx"], it["kind"], it["pvb"]
            po = it["rl"] * 64
            tt = idx if kind == "c" else 2 + idx
            for par in range(2):
                bk_s = bpair[par]
                fw.op("act", lambda e, si=si, bk_s=bk_s, par=par: e.activation(
                    out=ET[si][:, :].rearrange("p (g two q) -> p g two q", two=2, q=64)[:, :, par, :],
                    in_=PS[bk_s][:, 0:256].rearrange("p (g q) -> p g q", q=64), func=AF.Exp, scale=0.125),
                    reads=[PSB[bk_s]], writes=[ETb[si]])
            if kind == "w":
                dr0 = 2 * idx - r + 7
                fw.op("dve", lambda e, si=si, dr0=dr0: e.tensor_tensor(
                    out=ET[si][:, :], in0=ET[si][:, :],
                    in1=TB2[:, dr0 + 1, :, :].rearrange("p h q -> p (h q)"), op=ALU.mult),
                    reads=[ETb[si]], writes=[ETb[si]])
                if 2 * idx < s_:
                    fw.op("pool", lambda e, si=si: e.memset(ET[si][0:64, :], 0.0), writes=[ETb[si]])
                if 2 * idx + 1 >= s_ + 8:
                    fw.op("pool", lambda e, si=si: e.memset(ET[si][64:128, :], 0.0), writes=[ETb[si]])
            n, last = it["n"], it["last"]
            for h in range(8):
                bk = pvb[h // 4]
                col = (h % 4) * 65
                fw.op("pe", lambda e, si=si, h=h, bk=bk, col=col, tt=tt, n=n, last=last, po=po: e.matmul(
                    PS[bk][po:po + 64, col:col + 65], ET[si][:, h * 64:(h + 1) * 64],
                    VA[:, tt, h * 65:(h + 1) * 65], start=(n == 0 and h % 4 == 0), stop=last,
                    skip_group_check=True),
                    reads=[ETb[si]], writes=[PSB[bk]], inc=(last and h % 4 == 3), pe_acc=True)
            if last and it["rl"] == 1:
                a = it["a"]
                qt = a % 4
                for half in range(2):
                    bk = pvb[half]
                    acc = PS[bk][:, 0:260].rearrange("p (h w) -> p h w", w=65)
                    fw.op("dve", lambda e, acc=acc: e.reciprocal(out=stat[:, 8:12], in_=acc[:, :, 64]),
                          reads=[PSB[bk]], writes=[statb])
                    fw.op("dve", lambda e, acc=acc, half=half, qt=qt: e.tensor_tensor(
                        out=ON[:, qt, half * 256:(half + 1) * 256].rearrange("p (h d) -> p h d", d=64),
                        in0=acc[:, :, 0:64], in1=stat[:, 8:12].unsqueeze(2).broadcast_to([128, 4, 64]), op=ALU.mult),
                        reads=[PSB[bk], statb], writes=[ONb])
                if qt == 3:
                    flush_o(1, C + (a // 4) * 512, 512)

        b_qk(items[0], 0)
        for i, it in enumerate(items):
            will_flush = it["last"] and it["rl"] == 1 and it["a"] % 4 == 3
            if i + 1 < len(items) and not will_flush:
                b_qk(items[i + 1], i + 1)
            b_rest(it, i)
            if i + 1 < len(items) and will_flush:
                b_qk(items[i + 1], i + 1)
        fw.barrier()

    def branch_c(li):
        fw.dma("sp", brv[0:64, 0:1], I["g_qnorm"][li:li + 1, :].rearrange("o d -> d o"), writes=[brb],
               allow_slow_non_contiguous=True)
        fw.dma("sp", brv[64:128, 0:1], I["g_qnorm"][li:li + 1, :].rearrange("o d -> d o"), writes=[brb],
               allow_slow_non_contiguous=True)
        fw.dma("sp", brv[0:64, 1:2], I["g_knorm"][li:li + 1, :].rearrange("o d -> d o"), writes=[brb],
               allow_slow_non_contiguous=True)
        fw.dma("sp", brv[64:128, 1:2], I["g_knorm"][li:li + 1, :].rearrange("o d -> d o"), writes=[brb],
               allow_slow_non_contiguous=True)
        rope = (cos16, sin16, pm16)
        proj_fm(li, O_CQ, 4, post_norm(QT, QTb, bd64, 64, lambda c: brv[:, 0:1], rope), qblocks(li))
        proj_fm(li, O_CK, 1, post_norm(KT, KTb, bd64, 64, lambda c: brv[:, 1:2], rope), range(5))
        proj_v(li, O_CV, 2, 64)
        fw.barrier()
        fw.dma("sp", KT[64:128, 1, :], KT[0:64, 0, :])
        fw.dma("sp", KT[0:64, 1, :], KT[64:128, 0, :])
        fw.barrier()
        for tb in qblocks(li):
            q0, nq = TBLK[tb]
            kcs = CTXK if tb == 0 else ALLK
            steps = []
            for hg in range(4):
                sm = [[], []]
                for hh in range(2):
                    h = 2 * hg + hh
                    kvh = h // 4
                    pb = (h % 2) * 64
                    kch = 0 if kvh * 64 == pb else 1
                    attn_head(sm[hh], lambda a, n, h=h, pb=pb: QT[pb:pb + 64, h // 2, a:a + n],
                              lambda kc, kch=kch, pb=pb: KT[pb:pb + 64, kch, kc * 128:(kc + 1) * 128],
                              kvh * 65, 64, 0.125, q0, nq, kcs, finish_plain(h, 64))
                steps += interleave(sm[0], sm[1])
            run_steps(steps)
            flush_o(2, q0, nq)
        fw.barrier()

    mT = AR[:, 0:8 * T].rearrange("p (a t) -> p a t", a=8)
    mTb = [Buf() for _ in TBLK]
    OTt = [AR[:, 18432 + i * 8192:18432 + (i + 1) * 8192].rearrange("p (c q) -> p c q", c=16) for i in range(2)]
    OTtb = [Buf(), Buf()]
    wbr = [AR[:, 34816 + i * 2048:34816 + (i + 1) * 2048].rearrange("p (c q) -> p c q", c=16) for i in range(2)]
    wbrb = [Buf(), Buf()]
    SGm = [AR[:, 38912 + i * 512:38912 + (i + 1) * 512] for i in range(2)]
    SGmb = [Buf(), Buf()]
    ACCm = [AR[:, 39936 + i * 1024:39936 + (i + 1) * 1024].bitcast(F32) for i in range(2)]
    ACCmb = [Buf(), Buf()]
    TMPm = AR[:, 41984:43008].bitcast(F32)
    TMPmb = Buf()
    wout = AR[:, 18432:18432 + 8192].rearrange("p (k c) -> p k c", k=8)
    woutb = Buf()

    def merge_phase(li):
        tbs = qblocks(li)
        brn = ("w_br_a", "w_br_b", "w_br_c", "w_br_d")
        cnt = 0
        acn = 0
        for jp in range(4):
            wgs = []
            for jl in range(2):
                j = 2 * jp + jl
                i = wcnt[0] % 3
                wcnt[0] += 1
                wg, wgb = wt[i], wtb[i]
                wgs.append((wg, wgb))
                for br in range(4):
                    c0 = O_G + br * 1024 + j * 128
                    fw.dma("pool", wg[:, :, br * 128:(br + 1) * 128],
                           I["w_in"][li][:, c0:c0 + 128].rearrange("(k p) c -> p k c", p=128), writes=[wgb])
                for br in range(4):
                    fw.dma("pool", wbr[jl][:, br * 4:(br + 1) * 4, :],
                           I[brn[br]][li][:, j * 128:(j + 1) * 128].rearrange("(e p) c -> p e c", p=128),
                           writes=[wbrb[jl]])
            for tb in tbs:
                t0, tn = TBLK[tb]
                ob = cnt % 2
                cnt += 1
                fw.dma("sp", OTt[ob][:, :, 0:tn], OTd[:, :, t0:t0 + tn].rearrange("c p q -> p c q"), writes=[OTtb[ob]])
                for jl in range(2):
                    j = 2 * jp + jl
                    wg, wgb = wgs[jl]
                    ac = acn % 2
                    acn += 1
                    for br in range(4):
                        pg = ps_main()
                        for kc in range(8):
                            fw.op("pe", lambda e, pg=pg, kc=kc, br=br, t0=t0, tn=tn, wg=wg: e.matmul(
                                PS[pg][:, 0:tn], wg[:, kc, br * 128:(br + 1) * 128], hT[:, kc, t0:t0 + tn],
                                start=(kc == 0), stop=(kc == 7)),
                                reads=[wgb] + [hTb[t0 // 128 + q] for q in range(tn // 128)], writes=[PSB[pg]],
                                inc=(kc == 7), pe_acc=True)
                        sg = br % 2
                        fw.op("act", lambda e, pg=pg, sg=sg, tn=tn: e.activation(
                            out=SGm[sg][:, 0:tn], in_=PS[pg][:, 0:tn], func=AF.Sigmoid),
                            reads=[PSB[pg]], writes=[SGmb[sg]])
                        pb = ps_aux()
                        for ec in range(4):
                            fw.op("pe", lambda e, pb=pb, ec=ec, br=br, tn=tn, jl=jl, ob=ob: e.matmul(
                                PS[pb][:, 0:tn], wbr[jl][:, br * 4 + ec, :], OTt[ob][:, br * 4 + ec, 0:tn],
                                start=(ec == 0), stop=(ec == 3)),
                                reads=[wbrb[jl], OTtb[ob]], writes=[PSB[pb]], inc=(ec == 3), pe_acc=True)
                        if br == 0:
                            fw.op("dve", lambda e, pb=pb, sg=sg, ac=ac, tn=tn: e.tensor_tensor(
                                out=ACCm[ac][:, 0:tn], in0=PS[pb][:, 0:tn], in1=SGm[sg][:, 0:tn], op=ALU.mult),
                                reads=[PSB[pb], SGmb[sg]], writes=[ACCmb[ac]])
                        else:
                            fw.op("dve", lambda e, pb=pb, sg=sg, tn=tn: e.tensor_tensor(
                                out=TMPm[:, 0:tn], in0=PS[pb][:, 0:tn], in1=SGm[sg][:, 0:tn], op=ALU.mult),
                                reads=[PSB[pb], SGmb[sg]], writes=[TMPmb])
                            if br < 3:
                                fw.op("pool", lambda e, ac=ac, tn=tn: e.tensor_tensor(
                                    out=ACCm[ac][:, 0:tn], in0=ACCm[ac][:, 0:tn], in1=TMPm[:, 0:tn], op=ALU.add),
                                    reads=[TMPmb, ACCmb[ac]], writes=[ACCmb[ac]])
                            else:
                                fw.op("pool", lambda e, ac=ac, tn=tn, j=j, t0=t0: e.tensor_tensor(
                                    out=mT[:, j, t0:t0 + tn], in0=ACCm[ac][:, 0:tn], in1=TMPm[:, 0:tn], op=ALU.add),
                                    reads=[TMPmb, ACCmb[ac]], writes=[mTb[tb]])
        fw.barrier()

    def resid_tile(n, tt, ysrc, ybufs, gidx, xsrc, dsts):
        b = n % 2
        fw.dma("sp", xt[b][:], xsrc, writes=[xtb[b]])
        for half in range(2):
            fw.op("act", lambda e, half=half, b=b: e.activation(
                out=xn[b][:, half * 512:(half + 1) * 512], in_=ysrc[half], func=AF.Square,
                accum_out=stat[:, 24 + half:25 + half]), reads=[ybufs[half]], writes=[xnb[b], statb])
        fw.op("dve", lambda e: e.tensor_tensor(out=stat[:, 26:27], in0=stat[:, 24:25], in1=stat[:, 25:26], op=ALU.add),
              reads=[statb], writes=[statb])
        fw.op("act", lambda e: e.activation(out=stat[:, 27:28], in_=stat[:, 26:27], func=AF.Ln, scale=1.0 / D,
                                            bias=epsc[:, 0:1]), reads=[statb, cb], writes=[statb])
        fw.op("act", lambda e: e.activation(out=stat[:, 27:28], in_=stat[:, 27:28], func=AF.Exp, scale=-0.5),
              reads=[statb], writes=[statb])
        for half in range(2):
            fw.op("dve", lambda e, half=half, b=b: e.scalar_tensor_tensor(
                out=xn[b][:, half * 512:(half + 1) * 512], in0=ysrc[half], scalar=stat[:, 27:28],
                in1=gb[:, gidx, half * 512:(half + 1) * 512], op0=ALU.mult, op1=ALU.mult),
                reads=[ybufs[half], statb, mb], writes=[xnb[b]])
        fw.op("pool", lambda e, b=b: e.tensor_tensor(out=xt[b][:], in0=xt[b][:], in1=xn[b][:], op=ALU.add),
              reads=[xnb[b], xtb[b]], writes=[xtb[b]])
        for d in dsts:
            fw.dma("sp", d, xt[b][:], reads=[xtb[b]])

    def wout_phase(li, xsrc_fn):
        tiles = list(range(NT)) if li == 0 else list(range(2, NT))
        fw.dma("pool", wout[:, :, :], I["w_out"][li].rearrange("(k p) c -> p k c", p=128), writes=[woutb])
        for n, tt in enumerate(tiles):
            pis = []
            for half in range(2):
                pi = ps_main()
                pis.append(pi)
                for kc in range(8):
                    fw.op("pe", lambda e, pi=pi, kc=kc, tt=tt, half=half: e.matmul(
                        PS[pi][:, :], mT[:, kc, tt * 128:(tt + 1) * 128], wout[:, kc, half * 512:(half + 1) * 512],
                        start=(kc == 0), stop=(kc == 7)),
                        reads=[woutb], writes=[PSB[pi]], inc=(kc == 7), pe_acc=True)
            gidx = 1 if tt < 2 else 0
            resid_tile(n, tt, [PS[pis[0]][:, :], PS[pis[1]][:, :]], [PSB[pis[0]], PSB[pis[1]]], gidx,
                       xsrc_fn(tt), [Xd[tt * 128:(tt + 1) * 128, :]])
        fw.barrier()

    NFC = FFN_DENSE // 128
    gTd = AR[:, 0:NFC * 768].rearrange("p (f t) -> p f t", f=NFC)
    gTdb = Buf()
    W2d = AR[:, NFC * 768:NFC * 768 + NFC * 1024].rearrange("p (f c) -> p f c", f=NFC)
    W2db = Buf()
    SLU = [AR[:, NFC * 1792 + i * 512:NFC * 1792 + (i + 1) * 512] for i in range(2)]
    SLUb = [Buf(), Buf()]

    def ffn_dense(li):
        w1, w3, w2 = I["w1_dense"][0], I["w3_dense"][0], I["w2_dense"][0]
        for g0 in range(0, NFC, 8):
            ng = min(8, NFC - g0)
            fw.dma("pool", W2d[:, g0:g0 + ng, :], w2[g0 * 128:(g0 + ng) * 128, :].rearrange("(f p) c -> p f c", p=128),
                   writes=[W2db])
        cnt = 0
        for third in range(3):
            tok0 = third * 768
            for g0 in range(0, NFC, 4):
                ng = min(4, NFC - g0)
                wa, wab = load_w(w1[:, g0 * 128:(g0 + ng) * 128], ng * 128)
                wc, wcb = load_w(w3[:, g0 * 128:(g0 + ng) * 128], ng * 128)
                for c in range(ng):
                    fc = g0 + c
                    for (s0, sn) in ((0, 512), (512, 256)):
                        t0 = tok0 + s0
                        rb = [hTb[t0 // 128 + q] for q in range(sn // 128)]
                        pa_, pb_ = ps_main(), ps_main()
                        for (pp, ww, wwb) in ((pa_, wa, wab), (pb_, wc, wcb)):
                            for kc in range(8):
                                fw.op("pe", lambda e, pp=pp, ww=ww, kc=kc, c=c, t0=t0, sn=sn: e.matmul(
                                    PS[pp][:, 0:sn], ww[:, kc, c * 128:(c + 1) * 128], hT[:, kc, t0:t0 + sn],
                                    start=(kc == 0), stop=(kc == 7)),
                                    reads=[wwb] + rb, writes=[PSB[pp]], inc=(kc == 7), pe_acc=True)
                        sl = cnt % 2
                        cnt += 1
                        fw.op("act", lambda e, pa_=pa_, sl=sl, sn=sn: e.activation(out=SLU[sl][:, 0:sn], in_=PS[pa_][:, 0:sn],
                                                                                 func=AF.Silu),
                              reads=[PSB[pa_]], writes=[SLUb[sl]])
                        fw.op("dve", lambda e, pb_=pb_, sl=sl, sn=sn, fc=fc, s0=s0: e.tensor_tensor(
                            out=gTd[:, fc, s0:s0 + sn], in0=PS[pb_][:, 0:sn], in1=SLU[sl][:, 0:sn], op=ALU.mult),
                            reads=[PSB[pb_], SLUb[sl]], writes=[gTdb])
            for q in range(6):
                tt = third * 6 + q
                pis = []
                for half in range(2):
                    pi = ps_main()
                    pis.append(pi)
                    for fc in range(NFC):
                        fw.op("pe", lambda e, pi=pi, fc=fc, q=q, half=half: e.matmul(
                            PS[pi][:, :], gTd[:, fc, q * 128:(q + 1) * 128], W2d[:, fc, half * 512:(half + 1) * 512],
                            start=(fc == 0), stop=(fc == NFC - 1)),
                            reads=[gTdb, W2db], writes=[PSB[pi]], inc=(fc == NFC - 1), pe_acc=True)
                gidx = 3 if tt < 2 else 2
                resid_tile(q, tt, [PS[pis[0]][:, :], PS[pis[1]][:, :]], [PSB[pis[0]], PSB[pis[1]]], gidx,
                           Xd[tt * 128:(tt + 1) * 128, :], [Xd[tt * 128:(tt + 1) * 128, :]])
        fw.barrier()

    NFE = FFN_EXPERT // 128
    oacc = AR[:, 0:32768].bitcast(F32).rearrange("p (t c) -> p t c", t=16)
    oaccb = [Buf() for _ in range(16)]
    W2m = [AR[:, 32768 + i * 4096:32768 + (i + 1) * 4096].rearrange("p (f c) -> p f c", f=4) for i in range(2)]
    W2mb = [Buf(), Buf()]
    SLm = [AR[:, 40960 + i * 512:40960 + (i + 1) * 512] for i in range(2)]
    SLmb = [Buf(), Buf()]
    comb = AR[:, 41984:41984 + 256].bitcast(F32).rearrange("p (t e) -> p t e", t=16)
    combb = Buf()
    gTm = [cos16, sin16, cos8, sin8]
    gTmb = [Buf() for _ in range(4)]
    LT = [(C + i * 512, 512) for i in range(4)]

    def moe_router():
        wr, wrb = load_w(I["w_router"][0], 8)
        A_, B_ = stat[:, 28:36], stat[:, 36:44]
        for t in range(16):
            tok = C + t * 128
            pi = ps_main()
            for kc in range(8):
                fw.op("pe", lambda e, pi=pi, kc=kc, tok=tok: e.matmul(
                    PS[pi][:, 0:8], hT[:, kc, tok:tok + 128], wr[:, kc, 0:8], start=(kc == 0), stop=(kc == 7)),
                    reads=[wrb, hTb[2 + t]], writes=[PSB[pi]], inc=(kc == 7), pe_acc=True)
            fw.op("dve", lambda e, pi=pi: e.tensor_copy(out=A_, in_=PS[pi][:, 0:8]), reads=[PSB[pi]], writes=[statb])
            fw.op("dve", lambda e: e.reduce_max(out=stat[:, 44:45], in_=A_, axis=AX.X), reads=[statb], writes=[statb])
            fw.op("dve", lambda e: e.tensor_scalar(out=B_, in0=A_, scalar1=stat[:, 44:45], scalar2=None,
                                                   op0=ALU.is_equal), reads=[statb], writes=[statb])
            fw.op("dve", lambda e: e.scalar_tensor_tensor(out=A_, in0=B_, scalar=-1e30, in1=A_, op0=ALU.mult,
                                                          op1=ALU.add), reads=[statb], writes=[statb])
            fw.op("dve", lambda e: e.reduce_max(out=stat[:, 45:46], in_=A_, axis=AX.X), reads=[statb], writes=[statb])
            fw.op("dve", lambda e: e.tensor_scalar(out=A_, in0=A_, scalar1=stat[:, 45:46], scalar2=None,
                                                   op0=ALU.is_equal), reads=[statb], writes=[statb])
            fw.op("dve", lambda e: e.tensor_tensor(out=stat[:, 46:47], in0=stat[:, 45:46], in1=stat[:, 44:45],
                                                   op=ALU.subtract), reads=[statb], writes=[statb])
            fw.op("act", lambda e: e.activation(out=stat[:, 47:48], in_=stat[:, 46:47], func=AF.Exp),
                  reads=[statb], writes=[statb])
            fw.op("dve", lambda e: e.tensor_scalar(out=stat[:, 48:49], in0=stat[:, 47:48], scalar1=1.0, scalar2=None,
                                                   op0=ALU.add), reads=[statb], writes=[statb])
            fw.op("dve", lambda e: e.reciprocal(out=stat[:, 48:49], in_=stat[:, 48:49]), reads=[statb], writes=[statb])
            fw.op("dve", lambda e: e.tensor_tensor(out=stat[:, 49:50], in0=stat[:, 47:48], in1=stat[:, 48:49],
                                                   op=ALU.mult), reads=[statb], writes=[statb])
            fw.op("dve", lambda e: e.tensor_scalar(out=B_, in0=B_, scalar1=stat[:, 48:49], scalar2=None,
                                                   op0=ALU.mult), reads=[statb], writes=[statb])
            fw.op("dve", lambda e, t=t: e.scalar_tensor_tensor(out=comb[:, t, :], in0=A_, scalar=stat[:, 49:50],
                                                              in1=B_, op0=ALU.mult, op1=ALU.add),
                  reads=[statb], writes=[combb])

    def moe_phase():
        moe_router()
        w1, w3, w2 = I["w1_moe"][0], I["w3_moe"][0], I["w2_moe"][0]
        cnt = 0
        first = True
        gi = 0
        for ex in range(N_EXPERTS):
            for g0 in range(0, NFE, 4):
                wa, wab = load_w(w1[ex][:, g0 * 128:(g0 + 4) * 128], 512)
                wc, wcb = load_w(w3[ex][:, g0 * 128:(g0 + 4) * 128], 512)
                wi = gi % 2
                gi += 1
                fw.dma("pool", W2m[wi][:, :, :], w2[ex][g0 * 128:(g0 + 4) * 128, :].rearrange("(f p) c -> p f c", p=128),
                       writes=[W2mb[wi]])
                for c in range(4):
                    for (t0, tn) in LT:
                        rb = [hTb[t0 // 128 + q] for q in range(4)]
                        pa_, pb_ = ps_main(), ps_main()
                        for (pp, ww, wwb) in ((pa_, wa, wab), (pb_, wc, wcb)):
                            for kc in range(8):
                                fw.op("pe", lambda e, pp=pp, ww=ww, kc=kc, c=c, t0=t0: e.matmul(
                                    PS[pp][:, :], ww[:, kc, c * 128:(c + 1) * 128], hT[:, kc, t0:t0 + 512],
                                    start=(kc == 0), stop=(kc == 7)),
                                    reads=[wwb] + rb, writes=[PSB[pp]], inc=(kc == 7), pe_acc=True)
                        sl = cnt % 2
                        cnt += 1
                        fw.op("act", lambda e, pa_=pa_, sl=sl: e.activation(out=SLm[sl][:, :], in_=PS[pa_][:, :],
                                                                          func=AF.Silu),
                              reads=[PSB[pa_]], writes=[SLmb[sl]])
                        fw.op("dve", lambda e, pb_=pb_, sl=sl, c=c, t0=t0: e.tensor_tensor(
                            out=gTm[c][:, t0 - C:t0 - C + 512], in0=PS[pb_][:, :], in1=SLm[sl][:, :], op=ALU.mult),
                            reads=[PSB[pb_], SLmb[sl]], writes=[gTmb[c]])
                for t in range(16):
                    for half in range(2):
                        pi = ps_aux()
                        for c in range(4):
                            fw.op("pe", lambda e, pi=pi, c=c, t=t, half=half, wi=wi: e.matmul(
                                PS[pi][:, :], gTm[c][:, t * 128:(t + 1) * 128], W2m[wi][:, c, half * 512:(half + 1) * 512],
                                start=(c == 0), stop=(c == 3)),
                                reads=[gTmb[c], W2mb[wi]], writes=[PSB[pi]], inc=(c == 3), pe_acc=True)
                        if first:
                            fw.op("dve", lambda e, pi=pi, t=t, half=half, ex=ex: e.tensor_scalar(
                                out=oacc[:, t, half * 512:(half + 1) * 512], in0=PS[pi][:, :],
                                scalar1=comb[:, t, ex:ex + 1], scalar2=None, op0=ALU.mult),
                                reads=[PSB[pi], combb], writes=[oaccb[t]])
                        else:
                            fw.op("dve", lambda e, pi=pi, t=t, half=half, ex=ex: e.scalar_tensor_tensor(
                                out=oacc[:, t, half * 512:(half + 1) * 512], in0=PS[pi][:, :],
                                scalar=comb[:, t, ex:ex + 1], in1=oacc[:, t, half * 512:(half + 1) * 512],
                                op0=ALU.mult, op1=ALU.add),
                                reads=[PSB[pi], combb, oaccb[t]], writes=[oaccb[t]])
                first = False
        for t in range(16):
            tt = 2 + t
            resid_tile(t, tt, [oacc[:, t, 0:512], oacc[:, t, 512:1024]], [oaccb[t], oaccb[t]], 2,
                       Xd[tt * 128:(tt + 1) * 128, :], [out[t * 128:(t + 1) * 128, :]])
        fw.barrier()

    def layer1():
        xs = lambda tt: Xd[tt * 128:(tt + 1) * 128, :]
        layer_vectors(1)
        fw.barrier()
        norm_phase(1, 0, xs, list(range(NT)))
        fw.barrier()
        branch_a(1)
        branch_b(1)
        branch_c(1)
        branch_d(1)
        fw.mark("t_l1attn")
        merge_phase(1)
        wout_phase(1, xs)
        fw.mark("t_l1mix")
        norm_phase(1, 1, xs, list(range(2, NT)))
        fw.barrier()
        moe_phase()

    def layer0():
        branch_a(0)
        fw.mark("t_a")
        branch_b(0)
        fw.mark("t_b")
        branch_c(0)
        fw.mark("t_c")
        branch_d(0)
        fw.mark("t_l0attn")
        merge_phase(0)
        wout_phase(0, x_src0)
        fw.mark("t_l0mix")
        norm_phase(0, 1, lambda tt: Xd[tt * 128:(tt + 1) * 128, :], list(range(NT)))
        fw.barrier()
        ffn_dense(0)
        fw.mark("t_l0")

    layer_vectors(0)
    fw.barrier()
    fw.mark("lv")
    norm_phase(0, 0, x_src0, list(range(NT)))
    fw.barrier()
    fw.mark("norm")
    if dbg is None or dbg["what"] == "full" or (dbg.get("stop") or "")[:2] in ("t_", "a_"):
        fw.mark("t_pre")
        layer0()
        layer1()
    if dbg is not None and dbg["what"] in ("x1", "x2"):
        branch_a(0)
        branch_b(0)
        branch_c(0)
        branch_d(0)
        merge_phase(0)
        wout_phase(0, x_src0)
        if dbg["what"] == "x2":
            norm_phase(0, 1, lambda tt: Xd[tt * 128:(tt + 1) * 128, :], list(range(NT)))
            fw.barrier()
            ffn_dense(0)
    if dbg is not None and dbg["what"] in ("oc", "qkc"):
        branch_c(0)
    if dbg is not None and dbg["what"] == "oa":
        branch_a(0)
    if dbg is not None and (dbg["what"] == "ob" or (dbg.get("stop") or "").startswith("b")):
        branch_b(0)
    if dbg is not None and dbg["what"] == "od":
        branch_d(0)

    fw.frozen = False
    fw.barrier()
    dcnt = [0]

    def dump(src, dst, rb=()):
        p, n = src.shape[0], src.shape[1]
        for c0 in range(0, n, 1024):
            w = min(1024, n - c0)
            b = dcnt[0] % 2
            dcnt[0] += 1
            fw.op("dve", lambda e, b=b, c0=c0, w=w: e.tensor_copy(out=xn[b][0:p, 0:w], in_=src[:, c0:c0 + w]),
                  reads=list(rb), writes=[xnb[b]])
            fw.dma("sp", dst[:, c0:c0 + w], xn[b][0:p, 0:w], reads=[xnb[b]])

    if dbg is not None and dbg["what"] in ("x1", "x2"):
        for tt in range(NT):
            b = tt % 2
            fw.dma("sp", xt[b][:], Xd[tt * 128:(tt + 1) * 128, :], writes=[xtb[b]])
            fw.dma("sp", dbg_out[tt * 128:(tt + 1) * 128, :], xt[b][:], reads=[xtb[b]])
    if dbg is not None and dbg["what"] == "bpv":
        for i in (3, 4):
            dump(PS[i][:, 0:260], dbg_out[i * 128:(i + 1) * 128, 0:260])
    if dbg is not None and dbg["what"] == "stage":
        dump(ident[:, :], dbg_out[:, :])
    if dbg is not None and dbg["what"] == "qkc":
        for c in range(4):
            dump(QT[:, c, :], dbg_out[c * 128:(c + 1) * 128, :])
        dump(KT[:, 0, :], dbg_out[512:640, :])
        dump(KT[:, 1, :], dbg_out[640:768, :])
    if dbg is not None and dbg["what"] == "rope":
        dump(cos16[:, :], dbg_out[0:128, :])
        dump(sin16[:, :], dbg_out[128:256, :])
        dump(pm16[:, :], dbg_out[256:384, 0:128])
        dump(cos8[:, :], dbg_out[384:512, :])
        dump(sin8[:, :], dbg_out[512:640, :])
        dump(pm8[:, :], dbg_out[640:768, 0:128])
    if dbg is not None and dbg["what"] == "hT":
        for kc in range(8):
            dump(hT[:, kc, :], dbg_out[kc * 128:(kc + 1) * 128, :])
    if dbg is not None and dbg["what"] in ("oa", "ob", "oc", "od"):
        br = "abcd".index(dbg["what"][1])
        for ec in range(4):
            for c0 in range(0, T, 512):
                w = min(512, T - c0)
                fw.dma("sp", OTs[0][:, 0, 0:w], OTd[br * 4 + ec, :, c0:c0 + w], writes=[OTsb[0]])
                dump(OTs[0][:, 0, 0:w], dbg_out[ec * 128:(ec + 1) * 128, c0:c0 + w], rb=[OTsb[0]])
    fw.barrier(only=["sp"])
    fw.emit()


def make_in_maps(inputs, cores):
    maps = []
    shared = {}
    for k, v in inputs.items():
        if k in ("x", "c", "ctx", "c_ctx"):
            continue
        a = np.ascontiguousarray(np.asarray(v, dtype=np.float32))
        shared[k] = a
    for b in cores:
        m = dict(shared)
        m["x"] = np.ascontiguousarray(np.asarray(inputs["x"][b], dtype=np.float32))
        m["ctx"] = np.ascontiguousarray(np.asarray(inputs["ctx"][b], dtype=np.float32))
        m["cvec"] = np.ascontiguousarray(
            np.stack([np.asarray(inputs["c"][b]), np.asarray(inputs["c_ctx"])]).astype(np.float32))
        maps.append(m)
    return maps


_NC_CACHE = {}


def kernel(**inputs):
    if "nc" not in _NC_CACHE:
        _NC_CACHE["nc"] = build_program()
    nc = _NC_CACHE["nc"]
    maps = make_in_maps(inputs, list(range(8)))
    res = run_bass_kernel_spmd(nc, maps, core_ids=list(range(8)))
    return np.stack([np.asarray(r["out"], dtype=np.float32) for r in res.results], axis=0)
```

```python
import contextlib
import math
import numpy as np
import concourse.bass as bass
import concourse.mybir as mybir
from concourse.bass_utils import run_bass_kernel_spmd

F32 = mybir.dt.float32
BF16 = mybir.dt.bfloat16
I32 = mybir.dt.int32
AF = mybir.ActivationFunctionType
ALU = mybir.AluOpType
AX = mybir.AxisListType

D = 1024
S = 2048
C = 256
T = S + C
NT = T // 128
DEPTH = 2
D_IN = 8352
EPS = 1e-6
FFN_DENSE = 2816
N_EXPERTS = 8
FFN_EXPERT = 3584
O_AQ, O_AK, O_AV = 0, 512, 1024
O_BQ, O_BK, O_BV = 1536, 2048, 2560
O_CQ, O_CK, O_CV = 3072, 3584, 3712
O_DQA, O_DKVA, O_DKR = 3840, 4096, 4224
O_G = 4256
TBLK = [(0, 256), (256, 512), (768, 512), (1280, 512), (1792, 512)]


class Buf:
    __slots__ = ("w", "r")

    def __init__(self):
        self.w = None
        self.r = {}


class Stream:
    def __init__(self, name, sem):
        self.name = name
        self.sem = sem
        self.count = 0
        self.waited = {}
        self.ops = []


class FW:
    def __init__(self, nc, es):
        self.nc = nc
        self.es = es
        self.st = {}
        for n in ("pe", "act", "dve", "pool", "sp"):
            self.st[n] = Stream(n, es.enter_context(nc.semaphore("c_" + n)))
        self.dpool = {}
        for q, n in (("sp", 12), ("pool", 8), ("act", 4)):
            self.dpool[q] = [[es.enter_context(nc.semaphore(f"d_{q}{i}")), 0] for i in range(n)]
        self.dnext = {"sp": 0, "pool": 0, "act": 0}
        self.nbuf = 0
        self.frozen = False
        self.stop = None

    def mark(self, name):
        if self.stop is not None and name == self.stop:
            self.frozen = True

    def _wait(self, s, ev):
        sem, val = ev
        k = id(sem)
        if s.waited.get(k, 0) < val:
            s.waited[k] = val
            s.ops.append(("w", sem, val))

    def _deps(self, s, reads, writes, pe_acc=False):
        for b in reads:
            if b.w is not None:
                self._wait(s, b.w)
        for b in writes:
            if b.w is not None and not (pe_acc and b.w[0] is s.sem):
                self._wait(s, b.w)
            for ev in b.r.values():
                self._wait(s, ev)

    def op(self, eng, fn, reads=(), writes=(), inc=True, pe_acc=False):
        if self.frozen:
            return
        s = self.st[eng]
        self._deps(s, reads, writes, pe_acc)
        ev = (s.sem, s.count + 1)
        if inc:
            s.count += 1
        s.ops.append(("i", fn, inc))
        for b in writes:
            b.w = ev
            b.r = {}
        for b in reads:
            b.r[id(s.sem)] = ev

    def dma(self, q, out, in_, reads=(), writes=(), **kw):
        if self.frozen:
            return
        s = self.st[q]
        pool = self.dpool[q]
        i = self.dnext[q]
        self.dnext[q] = (i + 1) % len(pool)
        ent = pool[i]
        sem = ent[0]
        if ent[1] > 0:
            self._wait(s, (sem, ent[1]))
        self._deps(s, reads, writes)
        ent[1] += 16
        ev = (sem, ent[1])
        s.ops.append(("d", out, in_, sem, kw))
        for b in writes:
            b.w = ev
            b.r = {}
        for b in reads:
            b.r[id(sem)] = ev
        return ev

    def all_events(self):
        evs = []
        for n, s in self.st.items():
            if s.count > 0:
                evs.append((s.sem, s.count))
        for q, pool in self.dpool.items():
            for sem, val in pool:
                if val > 0:
                    evs.append((sem, val))
        return evs

    def barrier(self, only=None):
        if self.frozen:
            return
        evs = self.all_events()
        for n, s in self.st.items():
            if only is not None and n not in only:
                continue
            for ev in evs:
                if ev[0] is s.sem:
                    continue
                self._wait(s, ev)

    def emit(self):
        nc = self.nc
        hmap = {"pe": "tensor", "act": "scalar", "dve": "vector", "pool": "gpsimd", "sp": "sync"}
        with nc.Block() as block:
            for n, s in self.st.items():
                def body(e, s=s):
                    for o in s.ops:
                        if o[0] == "w":
                            e.wait_ge(o[1], o[2])
                        elif o[0] == "i":
                            ins = o[1](e)
                            if o[2]:
                                ins.then_inc(s.sem, 1)
                        else:
                            e.dma_start(out=o[1], in_=o[2], **o[4]).then_inc(o[3], 16)
                getattr(block, hmap[n])(body)


class K:
    pass


def build_program(dbg=None):
    nc = bass.Bass("TRN2", target_bir_lowering=False)
    es = contextlib.ExitStack()
    with es:
        _build(nc, es, dbg)
    return nc


def _dram_inputs(nc):
    L = DEPTH
    specs = {
        "x": [S, D], "ctx": [C, D], "cvec": [2, D],
        "w_ada": [L, D, 6 * D], "b_ada": [L, 6 * D],
        "g_mix_pre": [L, D], "g_mix_post": [L, D], "g_ffn_pre": [L, D], "g_ffn_post": [L, D],
        "w_in": [L, D, D_IN],
        "lam_q1": [L, 64], "lam_k1": [L, 64], "lam_q2": [L, 64], "lam_k2": [L, 64],
        "g_diff_sub": [L, 128], "na_rpb": [L, 8, 15, 31],
        "g_qnorm": [L, 64], "g_knorm": [L, 64], "g_q_lora": [L, 256],
        "w_uq": [L, 256, 768], "g_kv_lora": [L, 128], "w_ukv": [L, 128, 1024],
        "w_br_a": [L, 512, D], "w_br_b": [L, 512, D], "w_br_c": [L, 512, D], "w_br_d": [L, 512, D],
        "w_out": [L, D, D],
        "w1_dense": [1, D, FFN_DENSE], "w3_dense": [1, D, FFN_DENSE], "w2_dense": [1, FFN_DENSE, D],
        "w_router": [1, D, N_EXPERTS],
        "w1_moe": [1, N_EXPERTS, D, FFN_EXPERT], "w3_moe": [1, N_EXPERTS, D, FFN_EXPERT],
        "w2_moe": [1, N_EXPERTS, FFN_EXPERT, D],
    }
    return {k: nc.dram_tensor(k, v, F32, kind="ExternalInput").ap() for k, v in specs.items()}


def _build(nc, es, dbg):
    fw = FW(nc, es)
    if dbg is not None:
        fw.stop = dbg.get("stop")
    I = _dram_inputs(nc)
    out = nc.dram_tensor("out", [S, D], F32, kind="ExternalOutput").ap()
    dbg_out = None
    if dbg is not None:
        dbg_out = nc.dram_tensor("dbg", list(dbg["shape"]), F32, kind="ExternalOutput").ap()
    Xd = nc.dram_tensor("Xres", [T, D], F32).ap()

    def sb(name, shape, dt):
        return es.enter_context(nc.sbuf_tensor(name, list(shape), dt))

    PSall = es.enter_context(nc.psum_tensor("psall", [128, 4096], F32))
    PS = [PSall[:, i * 512:(i + 1) * 512] for i in range(8)]
    PSB = [Buf() for _ in range(8)]

    ident = sb("ident", [128, 128], F32)
    ones_f = sb("ones_f", [128, 128], F32)
    cb = Buf()
    iot = sb("iot", [128, 128], F32)
    fw.op("pool", lambda e: e.iota(iot[:], [[1, 128]], base=0, channel_multiplier=-1,
                                  allow_small_or_imprecise_dtypes=True), writes=[cb])
    fw.op("dve", lambda e: e.tensor_single_scalar(out=ident[:], in_=iot[:], scalar=0.0, op=ALU.is_equal),
          reads=[cb], writes=[cb])
    fw.op("dve", lambda e: e.memset(ones_f[:], 1.0), writes=[cb])
    fw.mark("const0")

    colv = sb("colv", [128, 8, 8], F32)
    gb = sb("gb", [128, 4, D], F32)
    sTb = sb("sTb", [128, 8, 2, 128], BF16)
    sTf = sb("sTf", [128, 16], F32)
    cv_row = sb("cv_row", [48, 128], F32)
    modc = sb("modc", [128, 48, 2], F32)
    badc = sb("badc", [128, 48], F32)
    gcol = sb("gcol", [128, 4, 8], F32)
    XX = sb("XX", [128, 4, D], F32)
    xt = [XX[:, i, :] for i in range(2)]
    xtb = [Buf(), Buf()]
    xn = [XX[:, 2 + i, :] for i in range(2)]
    xnb = [Buf(), Buf()]
    stat = sb("stat", [128, 64], F32)
    statb = Buf()
    hT = sb("hT", [128, 8, T], BF16)
    hTb = [Buf() for _ in range(NT)]
    wt = [sb(f"wt{i}", [128, 8, 512], BF16) for i in range(3)]
    wtb = [Buf() for _ in range(3)]
    wcnt = [0]

    mb = Buf()

    def load_w(src, ncols, krows=1024):
        i = wcnt[0] % 3
        wcnt[0] += 1
        nk = krows // 128
        fw.dma("pool", wt[i][:, 0:nk, 0:ncols], src.rearrange("(k p) c -> p k c", p=128), writes=[wtb[i]])
        return wt[i], wtb[i]

    def to_cols(src_rows_ap, nrows, dst, ps_i=0):
        fw.dma("sp", cv_row[0:nrows, :], src_rows_ap, writes=[mb])
        fw.op("pe", lambda e: e.transpose(PS[ps_i][:, 0:nrows], cv_row[0:nrows, :], ident[0:nrows, 0:nrows]),
              reads=[mb, cb], writes=[PSB[ps_i]])
        fw.op("dve", lambda e: e.tensor_copy(out=dst, in_=PS[ps_i][:, 0:nrows]), reads=[PSB[ps_i]], writes=[mb])

    to_cols(I["cvec"].rearrange("j (k d) -> (j k) d", d=128), 16, sTf[:, 0:16])
    fw.op("act", lambda e: e.activation(out=sTf[:, 0:16], in_=sTf[:, 0:16], func=AF.Silu), reads=[mb], writes=[mb])
    for j in range(2):
        for kc in range(8):
            fw.op("dve", lambda e, j=j, kc=kc: e.tensor_scalar(
                out=sTb[:, kc, j, :], in0=ones_f[:, :], scalar1=sTf[:, j * 8 + kc:j * 8 + kc + 1], scalar2=None,
                op0=ALU.mult), reads=[mb, cb], writes=[mb])

    fw.mark("stb")

    def layer_vectors(li):
        to_cols(I["b_ada"][li].rearrange("(r d) -> r d", d=128), 48, badc[:, :])
        for gi, nm in enumerate(("g_mix_pre", "g_mix_post", "g_ffn_pre", "g_ffn_post")):
            to_cols(I[nm][li].rearrange("(r d) -> r d", d=128), 8, gcol[:, gi, :])
        for piece in range(12):
            w, wb = load_w(I["w_ada"][li, :, piece * 512:(piece + 1) * 512], 512)
            for q in range(4):
                ech = piece * 4 + q
                for kc in range(8):
                    fw.op("pe", lambda e, q=q, kc=kc, ech=ech, w=w: e.matmul(
                        PS[1][:, ech * 2:ech * 2 + 2], w[:, kc, q * 128:(q + 1) * 128], sTb[:, kc, :, 0],
                        start=(kc == 0), stop=(kc == 7)),
                        reads=[wb, mb], writes=[PSB[1]], inc=(kc == 7 and q == 3), pe_acc=True)
            part = piece // 2
            if part in (2, 5):
                half = piece % 2
                for j in range(2):
                    pj = 2 + j
                    for kc in range(8):
                        fw.op("pe", lambda e, kc=kc, j=j, pj=pj, w=w: e.matmul(
                            PS[pj][:, :], sTb[:, kc, j, :], w[:, kc, :], start=(kc == 0), stop=(kc == 7)),
                            reads=[wb, mb], writes=[PSB[pj]], inc=(kc == 7), pe_acc=True)
                    idx = (0 if part == 2 else 2) + j
                    bsel = j
                    fw.dma("sp", xt[bsel][:, 0:512],
                           I["b_ada"][li:li + 1, piece * 512:(piece + 1) * 512].broadcast_to([128, 512]),
                           writes=[xtb[bsel]])
                    gname = "g_mix_post" if part == 2 else "g_ffn_post"
                    fw.dma("sp", xt[bsel][:, 512:1024],
                           I[gname][li:li + 1, half * 512:(half + 1) * 512].broadcast_to([128, 512]),
                           writes=[xtb[bsel]])
                    fw.op("dve", lambda e, pj=pj, bsel=bsel: e.tensor_tensor(
                        out=xn[bsel][:, 0:512], in0=PS[pj][:, :], in1=xt[bsel][:, 0:512], op=ALU.add),
                        reads=[PSB[pj], xtb[bsel]], writes=[xnb[bsel]])
                    fw.op("dve", lambda e, idx=idx, half=half, bsel=bsel: e.tensor_tensor(
                        out=gb[:, idx, half * 512:(half + 1) * 512], in0=xn[bsel][:, 0:512],
                        in1=xt[bsel][:, 512:1024], op=ALU.mult),
                        reads=[xnb[bsel], xtb[bsel]], writes=[mb])
        fw.op("dve", lambda e: e.tensor_copy(out=modc[:].rearrange("p a j -> p (a j)"), in_=PS[1][:, 0:96]),
              reads=[PSB[1]], writes=[mb])
        for j in range(2):
            for h, (gi, sci, shi) in enumerate(((0, 1, 0), (2, 4, 3))):
                setA = h * 4 + j * 2
                fw.op("dve", lambda e, j=j, sci=sci, setA=setA: e.scalar_tensor_tensor(
                    out=colv[:, setA, :], in0=modc[:, sci * 8:(sci + 1) * 8, j], scalar=1.0,
                    in1=badc[:, sci * 8:(sci + 1) * 8], op0=ALU.add, op1=ALU.add), reads=[mb], writes=[mb])
                fw.op("dve", lambda e, gi=gi, setA=setA: e.tensor_tensor(
                    out=colv[:, setA, :], in0=colv[:, setA, :], in1=gcol[:, gi, :], op=ALU.mult),
                    reads=[mb], writes=[mb])
                fw.op("dve", lambda e, j=j, shi=shi, setA=setA: e.tensor_tensor(
                    out=colv[:, setA + 1, :], in0=modc[:, shi * 8:(shi + 1) * 8, j],
                    in1=badc[:, shi * 8:(shi + 1) * 8], op=ALU.add), reads=[mb], writes=[mb])

    def norm_phase(li, which, src_ap_fn, tiles):
        for n, tt in enumerate(tiles):
            b = n % 2
            isctx = tt < 2
            fw.dma("sp", xt[b][:], src_ap_fn(tt), writes=[xtb[b]])
            fw.op("act", lambda e, b=b, tt=tt: e.activation(out=xn[b][:], in_=xt[b][:], func=AF.Square,
                                                          accum_out=stat[:, 0:1]),
                  reads=[xtb[b]], writes=[xnb[b], statb])
            fw.op("act", lambda e: e.activation(out=stat[:, 1:2], in_=stat[:, 0:1], func=AF.Ln,
                                                scale=1.0 / D, bias=epsc[:, 0:1]), reads=[statb, cb], writes=[statb])
            fw.op("act", lambda e: e.activation(out=stat[:, 2:3], in_=stat[:, 1:2], func=AF.Exp, scale=-0.5),
                  reads=[statb], writes=[statb])
            fw.op("dve", lambda e, b=b: e.tensor_scalar(out=xn[b][:], in0=xt[b][:], scalar1=stat[:, 2:3],
                                                       scalar2=None, op0=ALU.mult),
                  reads=[xtb[b], statb], writes=[xnb[b]])
            setA = which * 4 + (2 if isctx else 0)
            for half in range(2):
                pb = 6 + half
                for q in range(4):
                    kc = half * 4 + q
                    fw.op("pe", lambda e, b=b, kc=kc, q=q, pb=pb: e.transpose(
                        PS[pb][:, q * 128:(q + 1) * 128], xn[b][:, kc * 128:(kc + 1) * 128], ident[:]),
                        reads=[xnb[b], cb], writes=[PSB[pb]], inc=(q == 3), pe_acc=True)
                for q in range(4):
                    kc = half * 4 + q
                    if q % 2 == 0:
                        fw.op("act", lambda e, kc=kc, q=q, pb=pb, tt=tt, setA=setA: e.activation(
                            out=hT[:, kc, tt * 128:(tt + 1) * 128], in_=PS[pb][:, q * 128:(q + 1) * 128],
                            func=AF.Identity, scale=colv[:, setA, kc:kc + 1], bias=colv[:, setA + 1, kc:kc + 1]),
                            reads=[PSB[pb], mb], writes=[hTb[tt]])
                    else:
                        fw.op("dve", lambda e, kc=kc, q=q, pb=pb, tt=tt, setA=setA: e.tensor_scalar(
                            out=hT[:, kc, tt * 128:(tt + 1) * 128], in0=PS[pb][:, q * 128:(q + 1) * 128],
                            scalar1=colv[:, setA, kc:kc + 1], scalar2=colv[:, setA + 1, kc:kc + 1],
                            op0=ALU.mult, op1=ALU.add),
                            reads=[PSB[pb], mb], writes=[hTb[tt]])

    epsc = sb("epsc", [128, 1], F32)
    fw.op("dve", lambda e: e.memset(epsc[:], EPS), writes=[cb])

    def x_src0(tt):
        return I["ctx"][tt * 128:(tt + 1) * 128, :] if tt < 2 else I["x"][(tt - 2) * 128:(tt - 1) * 128, :]


    ARENA_ELEMS = 44200
    AR = sb("arena", [128, ARENA_ELEMS], BF16)
    aoff = [0]

    def carve(nelem_bf16):
        o = aoff[0]
        aoff[0] += nelem_bf16
        assert aoff[0] <= ARENA_ELEMS, aoff[0]
        return AR[:, o:o + nelem_bf16]

    QT = carve(4 * T).rearrange("p (a t) -> p a t", a=4)
    KT = carve(4 * T).rearrange("p (a t) -> p a t", a=4)
    VA = carve(NT * 520).rearrange("p (a t) -> p a t", a=NT)
    OTs1 = carve(4 * 512).rearrange("p (a t) -> p a t", a=4)
    ON = carve(4 * 512).rearrange("p (a t) -> p a t", a=4)
    OFr = carve(2048)
    OF = OFr.bitcast(F32).rearrange("p (m q d) -> p m q d", m=2, q=4)
    TB2r = AR[:, aoff[0]:aoff[0] + 8192]
    TB2 = TB2r.rearrange("p (i h q) -> p i h q", i=16, h=8)
    TM = [carve(1024).bitcast(F32) for i in range(4)]
    RS = [carve(1024).bitcast(F32) for i in range(2)]
    SQ = [carve(512) for i in range(2)]
    RAW = [carve(512) for i in range(2)]
    ETp = [carve(1024) for i in range(2)]
    ET = [ETp[0][:, 0:512], ETp[0][:, 512:1024], ETp[1][:, 0:512], ETp[1][:, 512:1024]]
    KRr = AR[:, 4 * T * 2 + NT * 520 + 2048: 4 * T * 2 + NT * 520 + 2048 + 4096]
    XXb = XX[:].rearrange("p a d -> p (a d)").bitcast(BF16)
    DQ = XXb[:, 0:2 * T].rearrange("p (a t) -> p a t", a=2)
    DKV = XXb[:, 2 * T:3 * T]
    QTb = [Buf() for _ in TBLK]
    KTb = [Buf() for _ in TBLK]
    VAb = [Buf() for _ in range(NT)]
    ETb = [Buf() for _ in range(4)]
    ecnt = [0]
    RAWb = [Buf(), Buf()]
    SQb = [Buf(), Buf()]
    RSb = [Buf(), Buf()]
    TMb = [Buf() for _ in range(4)]
    rcnt = [0]
    ONb = Buf()
    OFb = Buf()
    OTs = [OTs1]
    OTsb = [Buf()]
    otcnt = [0]
    OTd = nc.dram_tensor("OTd", [16, 128, T], BF16).ap()
    identb = sb("identb", [128, 128], BF16)
    onesb = sb("onesb", [128, 128], BF16)
    bd64 = sb("bd64", [128, 128], BF16)
    pm16 = sb("pm16", [128, 128], BF16)
    pm8 = sb("pm8", [128, 128], BF16)
    cos16 = sb("cos16", [128, S], BF16)
    sin16 = sb("sin16", [128, S], BF16)
    cos8 = sb("cos8", [128, S], BF16)
    sin8 = sb("sin8", [128, S], BF16)
    pcol = sb("pcol", [128, 16], F32)
    pcoli = sb("pcoli", [128, 8], I32)
    mcoli = XX[:, 0, 384:512].bitcast(I32)
    mcolf = XX[:, 0, 256:384]
    mtmp = XX[:, 0, 0:128]
    mtmp2 = XX[:, 0, 128:256]
    brv = sb("brv", [128, 16], F32)
    brb = Buf()
    lamrow = TM[0][:, 0:256].rearrange("p (a d) -> p a d", a=4)
    psA = [0]
    psX = [0]

    def ps_main():
        i = psA[0] % 4
        psA[0] += 1
        return i

    def ps_aux():
        i = 4 + psX[0] % 3
        psX[0] += 1
        return i

    fw.op("dve", lambda e: e.tensor_copy(out=identb[:], in_=ident[:]), reads=[cb], writes=[cb])
    fw.op("dve", lambda e: e.memset(onesb[:], 1.0), writes=[cb])
    fw.op("dve", lambda e: e.memset(bd64[:], 0.0), writes=[cb])
    fw.op("dve", lambda e: e.memset(bd64[0:64, 0:64], 1.0), writes=[cb])
    fw.op("dve", lambda e: e.memset(bd64[64:128, 64:128], 1.0), writes=[cb])
    fw.op("pool", lambda e: e.iota(mcoli[:], [[1, 128]], base=0, channel_multiplier=0), writes=[cb])
    fw.op("pool", lambda e: e.iota(pcoli[:, 0:1], [[0, 1]], base=0, channel_multiplier=1), writes=[cb])

    def build_pm(pm, h):
        fw.op("dve", lambda e: e.tensor_single_scalar(out=mcoli[:], in_=mcoli[:], scalar=h, op=ALU.bitwise_and),
              reads=[cb], writes=[cb])
        fw.op("dve", lambda e: e.tensor_copy(out=mcolf[:], in_=mcoli[:]), reads=[cb], writes=[cb])
        fw.op("dve", lambda e: e.tensor_single_scalar(out=mcolf[:], in_=mcolf[:], scalar=0.5, op=ALU.is_gt),
              reads=[cb], writes=[cb])
        fw.op("dve", lambda e: e.tensor_single_scalar(out=mtmp[:], in_=iot[:], scalar=float(h), op=ALU.is_equal),
              reads=[cb], writes=[cb])
        fw.op("dve", lambda e: e.tensor_tensor(out=mtmp[:], in0=mtmp[:], in1=mcolf[:], op=ALU.mult),
              reads=[cb], writes=[cb])
        fw.op("dve", lambda e: e.tensor_single_scalar(out=mtmp2[:], in_=iot[:], scalar=float(-h), op=ALU.is_equal),
              reads=[cb], writes=[cb])
        fw.op("dve", lambda e: e.tensor_scalar(out=mcolf[:], in0=mcolf[:], scalar1=-1.0, scalar2=1.0,
                                               op0=ALU.mult, op1=ALU.add), reads=[cb], writes=[cb])
        fw.op("dve", lambda e: e.tensor_tensor(out=mtmp2[:], in0=mtmp2[:], in1=mcolf[:], op=ALU.mult),
              reads=[cb], writes=[cb])
        fw.op("dve", lambda e: e.tensor_tensor(out=pm[:], in0=mtmp[:], in1=mtmp2[:], op=ALU.subtract),
              reads=[cb], writes=[cb])
        fw.op("pool", lambda e: e.iota(mcoli[:], [[1, 128]], base=0, channel_multiplier=0), reads=[cb], writes=[cb])

    fw.mark("cmat")
    build_pm(pm16, 16)
    build_pm(pm8, 8)
    fw.barrier()
    fw.mark("pm")

    def build_rope(cosT, sinT, h):
        f32v = AR[:, 0:4 * T].bitcast(F32)
        f32w = AR[:, 4 * T:8 * T].bitcast(F32)
        Rr, Cc = f32v[:, 0:S], f32v[:, S:2 * S]
        U, Fr = f32w[:, 0:S], f32w[:, S:2 * S]
        Ui = AR[:, 8 * T:8 * T + 2 * S].bitcast(I32)
        fw.op("dve", lambda e: e.tensor_single_scalar(out=pcoli[:, 1:2], in_=pcoli[:, 0:1], scalar=h - 1,
                                                      op=ALU.bitwise_and), reads=[cb], writes=[cb])
        fw.op("dve", lambda e: e.tensor_single_scalar(out=pcoli[:, 2:3], in_=pcoli[:, 0:1], scalar=2 * h,
                                                      op=ALU.bitwise_and), reads=[cb], writes=[cb])
        fw.op("dve", lambda e: e.tensor_single_scalar(out=pcoli[:, 3:4], in_=pcoli[:, 0:1], scalar=h,
                                                      op=ALU.bitwise_and), reads=[cb], writes=[cb])
        fw.op("dve", lambda e: e.tensor_copy(out=pcol[:, 1:4], in_=pcoli[:, 1:4]), reads=[cb], writes=[cb])
        fw.op("act", lambda e: e.activation(out=pcol[:, 4:5], in_=pcol[:, 1:2], func=AF.Exp,
                                            scale=-math.log(10000.0) / h), reads=[cb], writes=[cb])
        fw.op("dve", lambda e: e.tensor_single_scalar(out=pcol[:, 5:6], in_=pcol[:, 2:3], scalar=0.5, op=ALU.is_gt),
              reads=[cb], writes=[cb])
        fw.op("dve", lambda e: e.tensor_scalar(out=pcol[:, 6:7], in0=pcol[:, 3:4], scalar1=0.5, scalar2=2.0,
                                               op0=ALU.is_gt, op1=ALU.mult), reads=[cb], writes=[cb])
        fw.op("dve", lambda e: e.tensor_scalar(out=pcol[:, 6:7], in0=pcol[:, 6:7], scalar1=-1.0, scalar2=None,
                                               op0=ALU.add), reads=[cb], writes=[cb])
        fw.op("pool", lambda e: e.iota(Rr, [[1, 32], [0, 64]], base=0, channel_multiplier=0,
                                      allow_small_or_imprecise_dtypes=True), reads=[cb], writes=[cb])
        fw.op("pool", lambda e: e.iota(Cc, [[0, 32], [1, 64]], base=0, channel_multiplier=0,
                                      allow_small_or_imprecise_dtypes=True), reads=[cb], writes=[cb])
        fw.op("dve", lambda e: e.tensor_tensor(out=Cc, in0=Cc, in1=Rr, op=ALU.subtract), reads=[cb], writes=[cb])
        fw.op("dve", lambda e: e.scalar_tensor_tensor(out=Rr, in0=Cc, scalar=pcol[:, 5:6], in1=Rr,
                                                      op0=ALU.mult, op1=ALU.add), reads=[cb], writes=[cb])
        for which, dst, off in ((0, sinT, 0.5), (1, cosT, 0.75)):
            fw.op("dve", lambda e: e.tensor_scalar(out=U, in0=Rr, scalar1=pcol[:, 4:5], scalar2=1.0 / (2 * math.pi),
                                                   op0=ALU.mult, op1=ALU.mult), reads=[cb], writes=[cb])
            fw.op("dve", lambda e, off=off: e.tensor_scalar(out=U, in0=U, scalar1=off, scalar2=None, op0=ALU.add),
                  reads=[cb], writes=[cb])
            fw.op("dve", lambda e: e.tensor_copy(out=Ui, in_=U), reads=[cb], writes=[cb])
            fw.op("dve", lambda e: e.tensor_copy(out=Fr, in_=Ui), reads=[cb], writes=[cb])
            fw.op("dve", lambda e: e.tensor_tensor(out=Fr, in0=U, in1=Fr, op=ALU.subtract), reads=[cb], writes=[cb])
            fw.op("dve", lambda e: e.tensor_single_scalar(out=U, in_=Fr, scalar=0.0, op=ALU.is_lt),
                  reads=[cb], writes=[cb])
            fw.op("dve", lambda e: e.tensor_tensor(out=Fr, in0=Fr, in1=U, op=ALU.add), reads=[cb], writes=[cb])
            fw.op("dve", lambda e: e.tensor_single_scalar(out=U, in_=Fr, scalar=1.0, op=ALU.is_ge),
                  reads=[cb], writes=[cb])
            fw.op("dve", lambda e: e.tensor_tensor(out=Fr, in0=Fr, in1=U, op=ALU.subtract), reads=[cb], writes=[cb])
            fw.op("dve", lambda e: e.tensor_scalar(out=Fr, in0=Fr, scalar1=-0.5, scalar2=2 * math.pi * (1 - 1e-6),
                                                   op0=ALU.add, op1=ALU.mult), reads=[cb], writes=[cb])
            fw.op("act", lambda e, dst=dst: e.activation(out=dst[:], in_=Fr, func=AF.Sin),
                  reads=[cb], writes=[cb])


    def evac_copy(n, dst, src, reads, writes):
        if n % 2 == 0:
            fw.op("act", lambda e: e.activation(out=dst, in_=src, func=AF.Copy), reads=reads, writes=writes)
        else:
            fw.op("dve", lambda e: e.tensor_copy(out=dst, in_=src), reads=reads, writes=writes)

    def blocks_for(li):
        return list(range(5))

    def proj_fm(li, col0, nchunks, post, tbs, wsrc=None, krows=1024, rhs_fn=None, rbufs_fn=None, group=1):
        c = 0
        while c < nchunks:
            nload = min(4, nchunks - c)
            src = (wsrc if wsrc is not None else I["w_in"][li])[:, col0 + c * 128: col0 + (c + nload) * 128]
            w, wb = load_w(src, nload * 128, krows)
            nk = krows // 128
            for g0 in range(0, nload, group):
                for tb in tbs:
                    t0, tn = TBLK[tb]
                    pis = []
                    for gi in range(group):
                        cc = g0 + gi
                        pi = ps_main()
                        pis.append(pi)
                        for kc in range(nk):
                            rhs = rhs_fn(kc, t0, tn) if rhs_fn else hT[:, kc, t0:t0 + tn]
                            rb = rbufs_fn(tb) if rbufs_fn else [hTb[t0 // 128 + q] for q in range(tn // 128)]
                            fw.op("pe", lambda e, pi=pi, cc=cc, kc=kc, rhs=rhs, tn=tn, w=w, nk=nk: e.matmul(
                                PS[pi][:, 0:tn], w[:, kc, cc * 128:(cc + 1) * 128], rhs,
                                start=(kc == 0), stop=(kc == nk - 1)),
                                reads=[wb] + rb, writes=[PSB[pi]], inc=(kc == nk - 1), pe_acc=True)
                    post(c + g0, tb, pis)
            c += nload

    def post_plain(dst, dstb):
        cnt = [0]

        def f(ci, tb, pis):
            t0, tn = TBLK[tb]
            cnt[0] += 1
            evac_copy(cnt[0], dst[:, ci, t0:t0 + tn], PS[pis[0]][:, 0:tn], [PSB[pis[0]]], [dstb[tb]])
        return f

    def rope_apply(src_ps, src_psb, raw_i, dst, dstb, t0, tn, cosT, sinT, pm, prange=(0, 128)):
        p0, p1 = prange
        s0 = t0 - C
        pa = ps_aux()
        fw.op("pe", lambda e: e.matmul(PS[pa][p0:p1, 0:tn], pm[p0:p1, p0:p1], RAW[raw_i][p0:p1, 0:tn],
                                       start=True, stop=True),
              reads=[RAWb[raw_i], cb], writes=[PSB[pa]])
        a, b = rcnt[0] % 4, (rcnt[0] + 1) % 4
        rcnt[0] += 2
        if src_ps is not None:
            fw.op("dve", lambda e: e.tensor_tensor(out=TM[a][p0:p1, 0:tn], in0=src_ps[p0:p1, 0:tn],
                                                   in1=cosT[p0:p1, s0:s0 + tn], op=ALU.mult),
                  reads=[src_psb, cb], writes=[TMb[a]])
        else:
            fw.op("pool", lambda e: e.tensor_tensor(out=TM[a][p0:p1, 0:tn], in0=RAW[raw_i][p0:p1, 0:tn],
                                                    in1=cosT[p0:p1, s0:s0 + tn], op=ALU.mult),
                  reads=[RAWb[raw_i], cb], writes=[TMb[a]])
        fw.op("dve", lambda e: e.tensor_tensor(out=TM[b][p0:p1, 0:tn], in0=PS[pa][p0:p1, 0:tn],
                                               in1=sinT[p0:p1, s0:s0 + tn], op=ALU.mult),
              reads=[PSB[pa], cb], writes=[TMb[b]])
        fw.op("pool", lambda e: e.tensor_tensor(out=dst[p0:p1], in0=TM[a][p0:p1, 0:tn], in1=TM[b][p0:p1, 0:tn],
                                                op=ALU.add),
              reads=[TMb[a], TMb[b]], writes=[dstb])

    def post_rope(dst, dstb, cosT, sinT, pm):
        cnt = [0]

        def f(ci, tb, pis):
            t0, tn = TBLK[tb]
            pi = pis[0]
            if tb == 0:
                cnt[0] += 1
                evac_copy(cnt[0], dst[:, ci, t0:t0 + tn], PS[pi][:, 0:tn], [PSB[pi]], [dstb[tb]])
                return
            r = cnt[0] % 2
            cnt[0] += 1
            fw.op("act", lambda e: e.activation(out=RAW[r][:, 0:tn], in_=PS[pi][:, 0:tn], func=AF.Copy),
                  reads=[PSB[pi]], writes=[RAWb[r]])
            rope_apply(None, None, r, dst[:, ci, t0:t0 + tn], dstb[tb], t0, tn, cosT, sinT, pm)
        return f

    def post_norm(dst, dstb, redmat, cnt_feat, gcol_fn, rope=None, dst_idx=None):
        cnt = [0]

        def f(ci, tb, pis):
            t0, tn = TBLK[tb]
            pa = ps_aux()
            sqs = []
            for n, pi in enumerate(pis):
                r = cnt[0] % 2
                cnt[0] += 1
                sqs.append(r)
                fw.op("act", lambda e, r=r, pi=pi: e.activation(out=SQ[r][:, 0:tn], in_=PS[pi][:, 0:tn],
                                                                func=AF.Square),
                      reads=[PSB[pi]], writes=[SQb[r]])
            for n, r in enumerate(sqs):
                fw.op("pe", lambda e, r=r, n=n: e.matmul(PS[pa][:, 0:tn], redmat[:, :], SQ[r][:, 0:tn],
                                                         start=(n == 0), stop=(n == len(sqs) - 1)),
                      reads=[SQb[r], cb], writes=[PSB[pa]], inc=(n == len(sqs) - 1), pe_acc=True)
            rs = cnt[0] % 2
            fw.op("act", lambda e: e.activation(out=RS[rs][:, 0:tn], in_=PS[pa][:, 0:tn], func=AF.Ln,
                                                scale=1.0 / cnt_feat, bias=epsc[:, 0:1]),
                  reads=[PSB[pa], cb], writes=[RSb[rs]])
            fw.op("act", lambda e: e.activation(out=RS[rs][:, 0:tn], in_=RS[rs][:, 0:tn], func=AF.Exp, scale=-0.5),
                  reads=[RSb[rs]], writes=[RSb[rs]])
            for n, pi in enumerate(pis):
                cidx = (ci + n) if dst_idx is None else dst_idx(ci + n)
                g = gcol_fn(ci + n)
                if rope is None or tb == 0:
                    fw.op("dve", lambda e, pi=pi, cidx=cidx, g=g: e.scalar_tensor_tensor(
                        out=dst[:, cidx, t0:t0 + tn], in0=PS[pi][:, 0:tn], scalar=g, in1=RS[rs][:, 0:tn],
                        op0=ALU.mult, op1=ALU.mult), reads=[PSB[pi], RSb[rs], brb], writes=[dstb[tb]])
                else:
                    r = cnt[0] % 2
                    cnt[0] += 1
                    fw.op("dve", lambda e, pi=pi, r=r, g=g: e.scalar_tensor_tensor(
                        out=RAW[r][:, 0:tn], in0=PS[pi][:, 0:tn], scalar=g, in1=RS[rs][:, 0:tn],
                        op0=ALU.mult, op1=ALU.mult), reads=[PSB[pi], RSb[rs], brb], writes=[RAWb[r]])
                    cosT, sinT, pm = rope
                    rope_apply(None, None, r, dst[:, cidx, t0:t0 + tn], dstb[tb], t0, tn, cosT, sinT, pm)
        return f

    def proj_v(li, col0, nheads, dv, wsrc=None):
        ncols = nheads * dv
        src = (wsrc if wsrc is not None else I["w_in"][li])[:, col0:col0 + ncols]
        w, wb = load_w(src, ncols)
        for tt in range(NT):
            pi = ps_main()
            for kc in range(8):
                fw.op("pe", lambda e, pi=pi, kc=kc, tt=tt: e.matmul(
                    PS[pi][:, 0:ncols], hT[:, kc, tt * 128:(tt + 1) * 128], w[:, kc, 0:ncols],
                    start=(kc == 0), stop=(kc == 7)),
                    reads=[wb, hTb[tt]], writes=[PSB[pi]], inc=(kc == 7), pe_acc=True)
            va = VA[:, tt, 0:nheads * (dv + 1)].rearrange("p (h d) -> p h d", d=dv + 1)
            evac_copy(tt, va[:, :, 0:dv], PS[pi][:, 0:ncols].rearrange("p (h d) -> p h d", d=dv),
                      [PSB[pi]], [VAb[tt]])
            fw.op("pool", lambda e, va=va: e.memset(va[:, :, dv:dv + 1], 1.0), writes=[VAb[tt]])

    pvset = [0]

    def attn_head(steps, qap_fn, kap_fn, vcol, dv, scale, q0, nq, kcs, finish, vap_fn=None):
        nqt = nq // 128
        pset = pvset[0] % 2
        pvset[0] += 1
        pvb = [4 + 2 * pset, 5 + 2 * pset]
        W = dv + 1
        for n, kc in enumerate(kcs):
            steps.append(dict(k=kap_fn(kc), q=qap_fn(q0, nq), nq=nq, scale=scale, nqt=nqt, pvb=pvb, W=W,
                              v=(vap_fn(kc) if vap_fn else VA[:, kc, vcol:vcol + W]), first=(n == 0),
                              last=(n == len(kcs) - 1), finish=finish, mask=None))

    def run_steps(steps, look=3):
        def qk(st):
            si = ecnt[0] % 4
            ecnt[0] += 1
            st["si"] = si
            fw.op("pe", lambda e: e.matmul(PS[si][:, 0:st["nq"]], st["k"], st["q"], start=True, stop=True),
                  reads=[], writes=[PSB[si]])

        def rest(st):
            si = st["si"]
            nq, nqt, W, pvb = st["nq"], st["nqt"], st["W"], st["pvb"]
            fw.op("act", lambda e: e.activation(out=ET[si][:, 0:nq], in_=PS[si][:, 0:nq], func=AF.Exp,
                                                scale=st["scale"]),
                  reads=[PSB[si]], writes=[ETb[si]])
            for qt in range(nqt):
                bk = pvb[qt // 2]
                col = (qt % 2) * W
                stf = st["first"] and (qt % 2 == 0)
                last = st["last"]
                fw.op("pe", lambda e, bk=bk, col=col, qt=qt, stf=stf, last=last: e.matmul(
                    PS[bk][:, col:col + W], ET[si][:, qt * 128:(qt + 1) * 128], st["v"],
                    start=stf, stop=last, skip_group_check=True),
                    reads=[ETb[si]], writes=[PSB[bk]], inc=(last and (qt == nqt - 1 or qt % 2 == 1)), pe_acc=True)
            if st["last"]:
                st["finish"](pvb, nqt, W)

        n = len(steps)
        look = 4
        for i in range(min(look, n)):
            qk(steps[i])
        for i in range(0, n, 2):
            rest(steps[i])
            if i + 1 < n:
                rest(steps[i + 1])
            for j in (i + look, i + look + 1):
                if j < n:
                    qk(steps[j])

    def interleave(a_, b_):
        o_ = []
        for x_, y_ in zip(a_, b_):
            o_ += [x_, y_]
        return o_

    def finish_plain(h, dv):
        def f(pvb, nqt, W):
            for half in range((nqt + 1) // 2):
                bk = pvb[half]
                nq2 = min(2, nqt - half * 2)
                acc = PS[bk][:, 0:nq2 * W].rearrange("p (q w) -> p q w", w=W)
                fw.op("dve", lambda e, acc=acc, nq2=nq2: e.reciprocal(out=stat[:, 8:8 + nq2], in_=acc[:, :, dv]),
                      reads=[PSB[bk]], writes=[statb])
                fw.op("dve", lambda e, acc=acc, nq2=nq2, half=half: e.tensor_tensor(
                    out=ON[:, half * 2:half * 2 + nq2, h * dv:(h + 1) * dv], in0=acc[:, :, 0:dv],
                    in1=stat[:, 8:8 + nq2].unsqueeze(2).broadcast_to([128, nq2, dv]), op=ALU.mult),
                    reads=[PSB[bk], statb], writes=[ONb])
        return f

    def flush_o(branch, q0, nq, scale_col=None, ecs=(0, 1, 2, 3)):
        nqt = nq // 128
        si = 0
        for ec in ecs:
            pb = 0
            psb16 = PS[pb][:].bitcast(BF16)
            for qt in range(nqt):
                fw.op("pe", lambda e, qt=qt, ec=ec: e.transpose(psb16[:, qt * 128:(qt + 1) * 128],
                                                               ON[:, qt, ec * 128:(ec + 1) * 128], identb[:]),
                      reads=[ONb, cb], writes=[PSB[pb]], inc=(qt == nqt - 1), pe_acc=True)
            if scale_col is None:
                evac_copy(ec, OTs[si][:, ec, 0:nq], psb16[:, 0:nq], [PSB[pb]], [OTsb[si]])
            else:
                fw.op("dve", lambda e, ec=ec: e.tensor_scalar(out=OTs[si][:, ec, 0:nq], in0=psb16[:, 0:nq],
                                                             scalar1=scale_col, scalar2=None, op0=ALU.mult),
                      reads=[PSB[pb], brb], writes=[OTsb[si]])
        fw.dma("sp", OTd[branch * 4 + ecs[0]:branch * 4 + ecs[-1] + 1, :, q0:q0 + nq].rearrange("c p q -> p c q"),
               OTs[si][:, ecs[0]:ecs[-1] + 1, 0:nq], reads=[OTsb[si]])

    ALLK = list(range(NT))
    CTXK = [0, 1]

    def qblocks(li):
        return [0, 1, 2, 3, 4] if li == 0 else [1, 2, 3, 4]

    def branch_a(li):
        lam_init = 0.8 - 0.6 * math.exp(-0.3 * li)
        for n, nm in enumerate(("lam_q1", "lam_k1", "lam_q2", "lam_k2")):
            fw.dma("sp", lamrow[:, n, :], I[nm][li:li + 1, :].broadcast_to([128, 64]), writes=[brb])
        fw.dma("sp", brv[:, 5:6], I["g_diff_sub"][li:li + 1, :].rearrange("o d -> d o"), writes=[brb],
               allow_slow_non_contiguous=True)
        for n in range(2):
            fw.op("dve", lambda e, n=n: e.tensor_tensor(out=lamrow[:, 2 * n, :], in0=lamrow[:, 2 * n, :],
                                                       in1=lamrow[:, 2 * n + 1, :], op=ALU.mult),
                  reads=[brb], writes=[brb])
            fw.op("dve", lambda e, n=n: e.reduce_sum(out=stat[:, 20 + n:21 + n], in_=lamrow[:, 2 * n, :],
                                                    axis=AX.X), reads=[brb], writes=[brb])
        fw.op("act", lambda e: e.activation(out=stat[:, 20:22], in_=stat[:, 20:22], func=AF.Exp),
              reads=[brb], writes=[brb])
        fw.op("dve", lambda e: e.tensor_tensor(out=stat[:, 22:23], in0=stat[:, 21:22], in1=stat[:, 20:21],
                                               op=ALU.subtract), reads=[brb], writes=[brb])
        fw.op("dve", lambda e: e.tensor_scalar(out=brv[:, 6:7], in0=stat[:, 22:23], scalar1=-lam_init,
                                               scalar2=None, op0=ALU.add), reads=[brb], writes=[brb])
        fw.op("dve", lambda e: e.tensor_scalar(out=brv[:, 5:6], in0=brv[:, 5:6], scalar1=1.0 - lam_init,
                                               scalar2=None, op0=ALU.mult), reads=[brb], writes=[brb])
        proj_fm(li, O_AQ, 4, post_rope(QT, QTb, cos16, sin16, pm16), qblocks(li))
        proj_fm(li, O_AK, 4, post_rope(KT, KTb, cos16, sin16, pm16), range(5))
        proj_v(li, O_AV, 4, 128)
        fw.barrier()
        fw.mark("a_kv")

        def finish_a(h, m):
            def f(pvb, nqt, W):
                for half in range((nqt + 1) // 2):
                    bk = pvb[half]
                    nq2 = min(2, nqt - half * 2)
                    acc = PS[bk][:, 0:nq2 * W].rearrange("p (q w) -> p q w", w=W)
                    fw.op("dve", lambda e, acc=acc, nq2=nq2: e.reciprocal(out=stat[:, 8:8 + nq2], in_=acc[:, :, 128]),
                          reads=[PSB[bk]], writes=[statb])
                    fw.op("dve", lambda e, acc=acc, nq2=nq2, half=half: e.tensor_tensor(
                        out=OF[:, m, half * 2:half * 2 + nq2, :], in0=acc[:, :, 0:128],
                        in1=stat[:, 8:8 + nq2].unsqueeze(2).broadcast_to([128, nq2, 128]), op=ALU.mult),
                        reads=[PSB[bk], statb], writes=[OFb])
                if m == 1:
                    fw.op("dve", lambda e: e.scalar_tensor_tensor(
                        out=OF[:, 0, 0:nqt, :], in0=OF[:, 1, 0:nqt, :], scalar=brv[:, 6:7], in1=OF[:, 0, 0:nqt, :],
                        op0=ALU.mult, op1=ALU.add), reads=[OFb, brb], writes=[OFb])
                    fw.op("pool", lambda e: e.tensor_tensor(out=OF[:, 1, 0:nqt, :], in0=OF[:, 0, 0:nqt, :],
                                                            in1=OF[:, 0, 0:nqt, :], op=ALU.mult),
                          reads=[OFb], writes=[OFb])
                    fw.op("dve", lambda e: e.reduce_sum(out=stat[:, 16:16 + nqt], in_=OF[:, 1, 0:nqt, :], axis=AX.X),
                          reads=[OFb], writes=[statb])
                    fw.op("act", lambda e: e.activation(out=stat[:, 16:16 + nqt], in_=stat[:, 16:16 + nqt],
                                                        func=AF.Ln, scale=1.0 / 128, bias=epsc[:, 0:1]),
                          reads=[statb, cb], writes=[statb])
                    fw.op("act", lambda e: e.activation(out=stat[:, 16:16 + nqt], in_=stat[:, 16:16 + nqt],
                                                        func=AF.Exp, scale=-0.5), reads=[statb], writes=[statb])
                    fw.op("dve", lambda e: e.tensor_tensor(
                        out=ON[:, 0:nqt, h * 128:(h + 1) * 128], in0=OF[:, 0, 0:nqt, :],
                        in1=stat[:, 16:16 + nqt].unsqueeze(2).broadcast_to([128, nqt, 128]), op=ALU.mult),
                        reads=[OFb, statb], writes=[ONb])
            return f

        for tb in qblocks(li):
            q0, nq = TBLK[tb]
            kcs = CTXK if tb == 0 else ALLK
            steps = []
            for h in range(4):
                sm = [[], []]
                for m in range(2):
                    attn_head(sm[m], lambda a, n, h=h, m=m: QT[m * 64:(m + 1) * 64, h, a:a + n],
                              lambda kc, h=h, m=m: KT[m * 64:(m + 1) * 64, h, kc * 128:(kc + 1) * 128],
                              h * 129, 128, 0.125, q0, nq, kcs, finish_a(h, m))
                steps += interleave(sm[0], sm[1])
            run_steps(steps)
            flush_o(0, q0, nq, scale_col=brv[:, 5:6])
        fw.barrier()

    KRt = sb("KRt", [32, T], BF16)
    DQb = [Buf() for _ in TBLK]
    DKVb = [Buf() for _ in TBLK]
    KRb = Buf()

    def branch_d(li):
        fw.dma("sp", brv[:, 2:4], I["g_q_lora"][li].rearrange("(c d) -> d c", d=128), writes=[brb],
               allow_slow_non_contiguous=True)
        fw.dma("sp", brv[:, 4:5], I["g_kv_lora"][li:li + 1, :].rearrange("o d -> d o"), writes=[brb],
               allow_slow_non_contiguous=True)
        DKV3 = DKV.rearrange("p (a t) -> p a t", a=1)
        KR = KRt[:, :]
        proj_fm(li, O_DQA, 2, post_norm(DQ, DQb, onesb, 256, lambda c: brv[:, 2 + c:3 + c]), qblocks(li), group=2)
        proj_fm(li, O_DKVA, 1, post_norm(DKV3, DKVb, onesb, 128, lambda c: brv[:, 4:5]), range(5))
        w, wb = load_w(I["w_in"][li][:, O_DKR:O_DKR + 32], 32)
        for tb in range(5):
            t0, tn = TBLK[tb]
            pi = ps_main()
            for kc in range(8):
                fw.op("pe", lambda e, pi=pi, kc=kc, t0=t0, tn=tn, w=w: e.matmul(
                    PS[pi][0:32, 0:tn], w[:, kc, 0:32], hT[:, kc, t0:t0 + tn], start=(kc == 0), stop=(kc == 7)),
                    reads=[wb] + [hTb[t0 // 128 + q] for q in range(tn // 128)], writes=[PSB[pi]],
                    inc=(kc == 7), pe_acc=True)
            if tb == 0:
                fw.op("act", lambda e, pi=pi, t0=t0, tn=tn: e.activation(out=KR[0:32, t0:t0 + tn],
                                                                       in_=PS[pi][0:32, 0:tn], func=AF.Copy),
                      reads=[PSB[pi]], writes=[KRb])
            else:
                r = tb % 2
                fw.op("act", lambda e, pi=pi, r=r, tn=tn: e.activation(out=RAW[r][0:32, 0:tn], in_=PS[pi][0:32, 0:tn],
                                                                     func=AF.Copy), reads=[PSB[pi]], writes=[RAWb[r]])
                rope_apply(None, None, r, KR[:, t0:t0 + tn], KRb, t0, tn, cos8, sin8, pm8, prange=(0, 32))
        iq = wcnt[0] % 3
        wcnt[0] += 1
        wuq = wt[iq][:].rearrange("p k c -> p (k c)")[:, 0:1536].rearrange("p (k c) -> p k c", k=2)
        fw.dma("pool", wuq, I["w_uq"][li].rearrange("(k p) c -> p k c", p=128), writes=[wtb[iq]])
        ik = wcnt[0] % 3
        wcnt[0] += 1
        wukv = wt[ik][:].rearrange("p k c -> p (k c)")[:, 0:1024]
        fw.dma("pool", wukv, I["w_ukv"][li], writes=[wtb[ik]])
        wv = wukv.rearrange("p (h e) -> p h e", e=128)[:, :, 64:128]
        for tt in range(NT):
            pi = ps_main()
            fw.op("pe", lambda e, pi=pi, tt=tt: e.matmul(PS[pi][:, :], DKV[:, tt * 128:(tt + 1) * 128], wv,
                                                         start=True, stop=True),
                  reads=[wtb[ik], DKVb[0], DKVb[1], DKVb[2], DKVb[3], DKVb[4]], writes=[PSB[pi]])
            va = VA[:, tt, 0:520].rearrange("p (h d) -> p h d", d=65)
            evac_copy(tt, va[:, :, 0:64], PS[pi][:, :].rearrange("p (h d) -> p h d", d=64), [PSB[pi]], [VAb[tt]])
            fw.op("pool", lambda e, va=va: e.memset(va[:, :, 64:65], 1.0), writes=[VAb[tt]])
        sc_d = 96.0 ** -0.5
        for g in range(2):
            for hl in range(4):
                h = g * 4 + hl
                for tb in range(5):
                    t0, tn = TBLK[tb]
                    pi = ps_main()
                    fw.op("pe", lambda e, pi=pi, h=h, t0=t0, tn=tn: e.matmul(
                        PS[pi][0:64, 0:tn], wukv[:, h * 128:h * 128 + 64], DKV[:, t0:t0 + tn], start=True, stop=True),
                        reads=[wtb[ik], DKVb[tb]], writes=[PSB[pi]])
                    evac_copy(tb, KT[0:64, hl, t0:t0 + tn], PS[pi][0:64, 0:tn], [PSB[pi]], [KTb[tb]])
                    if tb not in qblocks(li):
                        continue
                    pq = ps_main()
                    for c in range(2):
                        fw.op("pe", lambda e, pq=pq, c=c, h=h, t0=t0, tn=tn: e.matmul(
                            PS[pq][0:96, 0:tn], wuq[:, c, h * 96:(h + 1) * 96], DQ[:, c, t0:t0 + tn],
                            start=(c == 0), stop=(c == 1)),
                            reads=[wtb[iq], DQb[tb]], writes=[PSB[pq]], inc=(c == 1), pe_acc=True)
                    if tb == 0:
                        evac_copy(hl, QT[0:96, hl, t0:t0 + tn], PS[pq][0:96, 0:tn], [PSB[pq]], [QTb[tb]])
                    else:
                        evac_copy(hl, QT[0:64, hl, t0:t0 + tn], PS[pq][0:64, 0:tn], [PSB[pq]], [QTb[tb]])
                        r = (hl + tb) % 2
                        fw.op("act", lambda e, pq=pq, r=r, tn=tn: e.activation(
                            out=RAW[r][64:96, 0:tn], in_=PS[pq][64:96, 0:tn], func=AF.Copy),
                            reads=[PSB[pq]], writes=[RAWb[r]])
                        pa = ps_aux()
                        fw.op("pe", lambda e, pa=pa, r=r, tn=tn: e.matmul(
                            PS[pa][64:96, 0:tn], pm8[64:96, 64:96], RAW[r][64:96, 0:tn], start=True, stop=True),
                            reads=[RAWb[r], cb], writes=[PSB[pa]])
                        a_, b_ = rcnt[0] % 4, (rcnt[0] + 1) % 4
                        rcnt[0] += 2
                        s0 = t0 - C
                        fw.op("pool", lambda e, r=r, a_=a_, s0=s0, tn=tn: e.tensor_tensor(
                            out=TM[a_][64:96, 0:tn], in0=RAW[r][64:96, 0:tn], in1=cos8[64:96, s0:s0 + tn],
                            op=ALU.mult), reads=[RAWb[r], cb], writes=[TMb[a_]])
                        fw.op("dve", lambda e, pa=pa, b_=b_, s0=s0, tn=tn: e.tensor_tensor(
                            out=TM[b_][64:96, 0:tn], in0=PS[pa][64:96, 0:tn], in1=sin8[64:96, s0:s0 + tn],
                            op=ALU.mult), reads=[PSB[pa], cb], writes=[TMb[b_]])
                        fw.op("pool", lambda e, a_=a_, b_=b_, hl=hl, t0=t0, tn=tn: e.tensor_tensor(
                            out=QT[64:96, hl, t0:t0 + tn], in0=TM[a_][64:96, 0:tn], in1=TM[b_][64:96, 0:tn],
                            op=ALU.add), reads=[TMb[a_], TMb[b_]], writes=[QTb[tb]])
            fw.barrier()
            for hl in range(4):
                fw.dma("sp", KT[64:96, hl, :], KR[0:32, :])
            fw.barrier()
            for tb in qblocks(li):
                q0, nq = TBLK[tb]
                kcs = CTXK if tb == 0 else ALLK
                steps = []
                for hl in range(4):
                    h = g * 4 + hl
                    attn_head(steps, lambda a, n, hl=hl: QT[0:96, hl, a:a + n],
                              lambda kc, hl=hl: KT[0:96, hl, kc * 128:(kc + 1) * 128],
                              h * 65, 64, sc_d, q0, nq, kcs, finish_plain(h, 64))
                run_steps(steps)
                flush_o(3, q0, nq, ecs=(2 * g, 2 * g + 1))
            fw.barrier()

    def build_tb2(li):
        rp32 = ET[0].bitcast(F32)
        rpb16 = ET[1]
        IE = ET[2]
        winf = OFr.bitcast(F32)
        fw.dma("sp", rp32[0:31, 0:120], I["na_rpb"][li].rearrange("h r c -> c (h r)"), writes=[ETb[0]],
               allow_slow_non_contiguous=True)
        fw.op("dve", lambda e: e.tensor_copy(out=rpb16[0:31, 0:120], in_=rp32[0:31, 0:120]),
              reads=[ETb[0]], writes=[ETb[1]])
        fw.op("dve", lambda e: e.tensor_single_scalar(out=IE[0:31, 0:128], in_=iot[0:31, :], scalar=48.0,
                                                      op=ALU.is_equal), reads=[cb], writes=[ETb[2]])
        fw.op("dve", lambda e: e.tensor_copy(out=pcol[:, 7:8], in_=pcoli[:, 0:1]), reads=[cb], writes=[cb])
        qc_ = winf[0:64, 0:64]
        fw.op("dve", lambda e: e.tensor_scalar(out=qc_, in0=iot[0:64, 0:64], scalar1=pcol[0:64, 7:8], scalar2=-8.0,
                                               op0=ALU.add, op1=ALU.add), reads=[cb], writes=[OFb])
        fw.op("dve", lambda e: e.tensor_scalar(out=qc_, in0=qc_, scalar1=0.0, scalar2=48.0,
                                               op0=ALU.max, op1=ALU.min), reads=[OFb], writes=[OFb])
        fw.op("dve", lambda e: e.tensor_scalar(out=qc_, in0=qc_, scalar1=pcol[0:64, 7:8], scalar2=None,
                                               op0=ALU.subtract), reads=[OFb, cb], writes=[OFb])
        m1 = winf[0:64, 64:128]
        fw.op("dve", lambda e: e.tensor_single_scalar(out=m1, in_=qc_, scalar=0.0, op=ALU.is_le),
              reads=[OFb], writes=[OFb])
        fw.op("dve", lambda e: e.tensor_single_scalar(out=qc_, in_=qc_, scalar=-16.0, op=ALU.is_gt),
              reads=[OFb], writes=[OFb])
        fw.op("dve", lambda e: e.tensor_tensor(out=m1, in0=m1, in1=qc_, op=ALU.mult), reads=[OFb], writes=[OFb])
        tbb = Buf()
        for q0 in range(0, 64, 4):
            pi = ps_main()
            for ql in range(4):
                qc = q0 + ql
                fw.op("pe", lambda e, pi=pi, ql=ql, qc=qc: e.matmul(
                    PS[pi][0:64, ql * 120:(ql + 1) * 120], IE[0:31, 63 - qc:127 - qc], rpb16[0:31, 0:120],
                    start=True, stop=True), reads=[ETb[1], ETb[2]], writes=[PSB[pi]], inc=(ql == 3), pe_acc=True)
            fw.op("act", lambda e, pi=pi, q0=q0: e.activation(
                out=TB2[0:64, 1:16, :, q0:q0 + 4].rearrange("p r h q -> p q h r"),
                in_=PS[pi][0:64, 0:480].rearrange("p (q h r) -> p q h r", q=4, h=8), func=AF.Exp),
                reads=[PSB[pi]], writes=[tbb])
        fw.op("dve", lambda e: e.tensor_tensor(
            out=TB2[0:64, 1:16, :, :].rearrange("p i h q -> p (i h) q"),
            in0=TB2[0:64, 1:16, :, :].rearrange("p i h q -> p (i h) q"),
            in1=m1.unsqueeze(1).broadcast_to([64, 120, 64]), op=ALU.mult), reads=[tbb, OFb], writes=[tbb])
        fw.op("dve", lambda e: e.memset(TB2[0:64, 0, :, :], 0.0), writes=[tbb])
        fw.op("dve", lambda e: e.memset(TB2[64:128, 15, :, :], 0.0), writes=[tbb])
        fw.dma("sp", TB2[64:128, 0:15, :, :], TB2[0:64, 1:16, :, :], reads=[tbb], writes=[tbb])

    def branch_b(li):
        proj_fm(li, O_BQ, 4, post_plain(QT, QTb), qblocks(li))
        proj_fm(li, O_BK, 4, post_plain(KT, KTb), range(5))
        proj_v(li, O_BV, 8, 64)
        fw.barrier()
        fw.mark("b_proj")
        build_tb2(li)
        fw.barrier()
        fw.mark("b_tb2")
        if li == 0:
            steps = []
            for hg in range(4):
                sm = [[], []]
                for hh in range(2):
                    h = 2 * hg + hh
                    pb = (h % 2) * 64
                    attn_head(sm[hh], lambda a, n, h=h, pb=pb: QT[pb:pb + 64, h // 2, a:a + n],
                              lambda kc, h=h, pb=pb: KT[pb:pb + 64, h // 2, kc * 128:(kc + 1) * 128],
                              h * 65, 64, 0.125, 0, 256, CTXK, finish_plain(h, 64))
                steps += interleave(sm[0], sm[1])
            run_steps(steps)
            flush_o(1, 0, 256)
            fw.barrier()
            fw.mark("b_ctx")
        items = []
        for a in range(16):
            pset = pvset[0] % 2
            pvset[0] += 1
            pvb = [4 + 2 * pset, 5 + 2 * pset]
            for rl in range(2):
                r = 2 * a + rl
                s_ = min(max(r - 4, 0), 24)
                chunks = [("c", 0), ("c", 1)] + [("w", ap) for ap in range(s_ // 2, (s_ + 7) // 2 + 1)]
                for n, (kind, idx) in enumerate(chunks):
                    items.append(dict(a=a, rl=rl, r=r, s=s_, n=n, kind=kind, idx=idx, pvb=pvb,
                                      last=(n == len(chunks) - 1)))

        def b_qk(it, i):
            bpair = ((0, 1), (2, 3))[i % 2]
            it["bpair"] = bpair
            kcol = it["idx"] * 128 if it["kind"] == "c" else C + it["idx"] * 128
            qcol = C + it["r"] * 64
            for h in range(8):
                hp = (h % 2) * 64
                bk_s = bpair[h % 2]
                g_ = h // 2
                fw.op("pe", lambda e, bk_s=bk_s, g_=g_, h=h, hp=hp, kcol=kcol, qcol=qcol: e.matmul(
                    PS[bk_s][:, g_ * 64:(g_ + 1) * 64], KT[hp:hp + 64, h // 2, kcol:kcol + 128],
                    QT[hp:hp + 64, h // 2, qcol:qcol + 64], start=True, stop=True),
                    reads=[], writes=[PSB[bk_s]], inc=(h >= 6), pe_acc=True)

        def b_rest(it, i):
            si = i % 3
            bpair = it["bpair"]
            r, s_, idx, kind, pvb = it["r"], it["s"], it["idx"], it["kind"], it["pvb"]
            po = it["rl"] * 64
            tt = idx if kind == "c" else 2 + idx
            for par in range(2):
                bk_s = bpair[par]
                fw.op("act", lambda e, si=si, bk_s=bk_s, par=par: e.activation(
                    out=ET[si][:, :].rearrange("p (g two q) -> p g two q", two=2, q=64)[:, :, par, :],
                    in_=PS[bk_s][:, 0:256].rearrange("p (g q) -> p g q", q=64), func=AF.Exp, scale=0.125),
                    reads=[PSB[bk_s]], writes=[ETb[si]])
            if kind == "w":
                dr0 = 2 * idx - r + 7
                fw.op("dve", lambda e, si=si, dr0=dr0: e.tensor_tensor(
                    out=ET[si][:, :], in0=ET[si][:, :],
                    in1=TB2[:, dr0 + 1, :, :].rearrange("p h q -> p (h q)"), op=ALU.mult),
                    reads=[ETb[si]], writes=[ETb[si]])
                if 2 * idx < s_:
                    fw.op("pool", lambda e, si=si: e.memset(ET[si][0:64, :], 0.0), writes=[ETb[si]])
                if 2 * idx + 1 >= s_ + 8:
                    fw.op("pool", lambda e, si=si: e.memset(ET[si][64:128, :], 0.0), writes=[ETb[si]])
            n, last = it["n"], it["last"]
            for h in range(8):
                bk = pvb[h // 4]
                col = (h % 4) * 65
                fw.op("pe", lambda e, si=si, h=h, bk=bk, col=col, tt=tt, n=n, last=last, po=po: e.matmul(
                    PS[bk][po:po + 64, col:col + 65], ET[si][:, h * 64:(h + 1) * 64],
                    VA[:, tt, h * 65:(h + 1) * 65], start=(n == 0 and h % 4 == 0), stop=last,
                    skip_group_check=True),
                    reads=[ETb[si]], writes=[PSB[bk]], inc=(last and h % 4 == 3), pe_acc=True)
            if last and it["rl"] == 1:
                a = it["a"]
                qt = a % 4
                for half in range(2):
                    bk = pvb[half]
                    acc = PS[bk][:, 0:260].rearrange("p (h w) -> p h w", w=65)
                    fw.op("dve", lambda e, acc=acc: e.reciprocal(out=stat[:, 8:12], in_=acc[:, :, 64]),
                          reads=[PSB[bk]], writes=[statb])
                    fw.op("dve", lambda e, acc=acc, half=half, qt=qt: e.tensor_tensor(
                        out=ON[:, qt, half * 256:(half + 1) * 256].rearrange("p (h d) -> p h d", d=64),
                        in0=acc[:, :, 0:64], in1=stat[:, 8:12].unsqueeze(2).broadcast_to([128, 4, 64]), op=ALU.mult),
                        reads=[PSB[bk], statb], writes=[ONb])
                if qt == 3:
                    flush_o(1, C + (a // 4) * 512, 512)

        b_qk(items[0], 0)
        for i, it in enumerate(items):
            will_flush = it["last"] and it["rl"] == 1 and it["a"] % 4 == 3
            if i + 1 < len(items) and not will_flush:
                b_qk(items[i + 1], i + 1)
            b_rest(it, i)
            if i + 1 < len(items) and will_flush:
                b_qk(items[i + 1], i + 1)
        fw.barrier()

    def branch_c(li):
        fw.dma("sp", brv[0:64, 0:1], I["g_qnorm"][li:li + 1, :].rearrange("o d -> d o"), writes=[brb],
               allow_slow_non_contiguous=True)
        fw.dma("sp", brv[64:128, 0:1], I["g_qnorm"][li:li + 1, :].rearrange("o d -> d o"), writes=[brb],
               allow_slow_non_contiguous=True)
        fw.dma("sp", brv[0:64, 1:2], I["g_knorm"][li:li + 1, :].rearrange("o d -> d o"), writes=[brb],
               allow_slow_non_contiguous=True)
        fw.dma("sp", brv[64:128, 1:2], I["g_knorm"][li:li + 1, :].rearrange("o d -> d o"), writes=[brb],
               allow_slow_non_contiguous=True)
        rope = (cos16, sin16, pm16)
        proj_fm(li, O_CQ, 4, post_norm(QT, QTb, bd64, 64, lambda c: brv[:, 0:1], rope), qblocks(li))
        proj_fm(li, O_CK, 1, post_norm(KT, KTb, bd64, 64, lambda c: brv[:, 1:2], rope), range(5))
        proj_v(li, O_CV, 2, 64)
        fw.barrier()
        fw.dma("sp", KT[64:128, 1, :], KT[0:64, 0, :])
        fw.dma("sp", KT[0:64, 1, :], KT[64:128, 0, :])
        fw.barrier()
        for tb in qblocks(li):
            q0, nq = TBLK[tb]
            kcs = CTXK if tb == 0 else ALLK
            steps = []
            for hg in range(4):
                sm = [[], []]
                for hh in range(2):
                    h = 2 * hg + hh
                    kvh = h // 4
                    pb = (h % 2) * 64
                    kch = 0 if kvh * 64 == pb else 1
                    attn_head(sm[hh], lambda a, n, h=h, pb=pb: QT[pb:pb + 64, h // 2, a:a + n],
                              lambda kc, kch=kch, pb=pb: KT[pb:pb + 64, kch, kc * 128:(kc + 1) * 128],
                              kvh * 65, 64, 0.125, q0, nq, kcs, finish_plain(h, 64))
                steps += interleave(sm[0], sm[1])
            run_steps(steps)
            flush_o(2, q0, nq)
        fw.barrier()

    mT = AR[:, 0:8 * T].rearrange("p (a t) -> p a t", a=8)
    mTb = [Buf() for _ in TBLK]
    OTt = [AR[:, 18432 + i * 8192:18432 + (i + 1) * 8192].rearrange("p (c q) -> p c q", c=16) for i in range(2)]
    OTtb = [Buf(), Buf()]
    wbr = [AR[:, 34816 + i * 2048:34816 + (i + 1) * 2048].rearrange("p (c q) -> p c q", c=16) for i in range(2)]
    wbrb = [Buf(), Buf()]
    SGm = [AR[:, 38912 + i * 512:38912 + (i + 1) * 512] for i in range(2)]
    SGmb = [Buf(), Buf()]
    ACCm = [AR[:, 39936 + i * 1024:39936 + (i + 1) * 1024].bitcast(F32) for i in range(2)]
    ACCmb = [Buf(), Buf()]
    TMPm = AR[:, 41984:43008].bitcast(F32)
    TMPmb = Buf()
    wout = AR[:, 18432:18432 + 8192].rearrange("p (k c) -> p k c", k=8)
    woutb = Buf()

    def merge_phase(li):
        tbs = qblocks(li)
        brn = ("w_br_a", "w_br_b", "w_br_c", "w_br_d")
        cnt = 0
        acn = 0
        for jp in range(4):
            wgs = []
            for jl in range(2):
                j = 2 * jp + jl
                i = wcnt[0] % 3
                wcnt[0] += 1
                wg, wgb = wt[i], wtb[i]
                wgs.append((wg, wgb))
                for br in range(4):
                    c0 = O_G + br * 1024 + j * 128
                    fw.dma("pool", wg[:, :, br * 128:(br + 1) * 128],
                           I["w_in"][li][:, c0:c0 + 128].rearrange("(k p) c -> p k c", p=128), writes=[wgb])
                for br in range(4):
                    fw.dma("pool", wbr[jl][:, br * 4:(br + 1) * 4, :],
                           I[brn[br]][li][:, j * 128:(j + 1) * 128].rearrange("(e p) c -> p e c", p=128),
                           writes=[wbrb[jl]])
            for tb in tbs:
                t0, tn = TBLK[tb]
                ob = cnt % 2
                cnt += 1
                fw.dma("sp", OTt[ob][:, :, 0:tn], OTd[:, :, t0:t0 + tn].rearrange("c p q -> p c q"), writes=[OTtb[ob]])
                for jl in range(2):
                    j = 2 * jp + jl
                    wg, wgb = wgs[jl]
                    ac = acn % 2
                    acn += 1
                    for br in range(4):
                        pg = ps_main()
                        for kc in range(8):
                            fw.op("pe", lambda e, pg=pg, kc=kc, br=br, t0=t0, tn=tn, wg=wg: e.matmul(
                                PS[pg][:, 0:tn], wg[:, kc, br * 128:(br + 1) * 128], hT[:, kc, t0:t0 + tn],
                                start=(kc == 0), stop=(kc == 7)),
                                reads=[wgb] + [hTb[t0 // 128 + q] for q in range(tn // 128)], writes=[PSB[pg]],
                                inc=(kc == 7), pe_acc=True)
                        sg = br % 2
                        fw.op("act", lambda e, pg=pg, sg=sg, tn=tn: e.activation(
                            out=SGm[sg][:, 0:tn], in_=PS[pg][:, 0:tn], func=AF.Sigmoid),
                            reads=[PSB[pg]], writes=[SGmb[sg]])
                        pb = ps_aux()
                        for ec in range(4):
                            fw.op("pe", lambda e, pb=pb, ec=ec, br=br, tn=tn, jl=jl, ob=ob: e.matmul(
                                PS[pb][:, 0:tn], wbr[jl][:, br * 4 + ec, :], OTt[ob][:, br * 4 + ec, 0:tn],
                                start=(ec == 0), stop=(ec == 3)),
                                reads=[wbrb[jl], OTtb[ob]], writes=[PSB[pb]], inc=(ec == 3), pe_acc=True)
                        if br == 0:
                            fw.op("dve", lambda e, pb=pb, sg=sg, ac=ac, tn=tn: e.tensor_tensor(
                                out=ACCm[ac][:, 0:tn], in0=PS[pb][:, 0:tn], in1=SGm[sg][:, 0:tn], op=ALU.mult),
                                reads=[PSB[pb], SGmb[sg]], writes=[ACCmb[ac]])
                        else:
                            fw.op("dve", lambda e, pb=pb, sg=sg, tn=tn: e.tensor_tensor(
                                out=TMPm[:, 0:tn], in0=PS[pb][:, 0:tn], in1=SGm[sg][:, 0:tn], op=ALU.mult),
                                reads=[PSB[pb], SGmb[sg]], writes=[TMPmb])
                            if br < 3:
                                fw.op("pool", lambda e, ac=ac, tn=tn: e.tensor_tensor(
                                    out=ACCm[ac][:, 0:tn], in0=ACCm[ac][:, 0:tn], in1=TMPm[:, 0:tn], op=ALU.add),
                                    reads=[TMPmb, ACCmb[ac]], writes=[ACCmb[ac]])
                            else:
                                fw.op("pool", lambda e, ac=ac, tn=tn, j=j, t0=t0: e.tensor_tensor(
                                    out=mT[:, j, t0:t0 + tn], in0=ACCm[ac][:, 0:tn], in1=TMPm[:, 0:tn], op=ALU.add),
                                    reads=[TMPmb, ACCmb[ac]], writes=[mTb[tb]])
        fw.barrier()

    def resid_tile(n, tt, ysrc, ybufs, gidx, xsrc, dsts):
        b = n % 2
        fw.dma("sp", xt[b][:], xsrc, writes=[xtb[b]])
        for half in range(2):
            fw.op("act", lambda e, half=half, b=b: e.activation(
                out=xn[b][:, half * 512:(half + 1) * 512], in_=ysrc[half], func=AF.Square,
                accum_out=stat[:, 24 + half:25 + half]), reads=[ybufs[half]], writes=[xnb[b], statb])
        fw.op("dve", lambda e: e.tensor_tensor(out=stat[:, 26:27], in0=stat[:, 24:25], in1=stat[:, 25:26], op=ALU.add),
              reads=[statb], writes=[statb])
        fw.op("act", lambda e: e.activation(out=stat[:, 27:28], in_=stat[:, 26:27], func=AF.Ln, scale=1.0 / D,
                                            bias=epsc[:, 0:1]), reads=[statb, cb], writes=[statb])
        fw.op("act", lambda e: e.activation(out=stat[:, 27:28], in_=stat[:, 27:28], func=AF.Exp, scale=-0.5),
              reads=[statb], writes=[statb])
        for half in range(2):
            fw.op("dve", lambda e, half=half, b=b: e.scalar_tensor_tensor(
                out=xn[b][:, half * 512:(half + 1) * 512], in0=ysrc[half], scalar=stat[:, 27:28],
                in1=gb[:, gidx, half * 512:(half + 1) * 512], op0=ALU.mult, op1=ALU.mult),
                reads=[ybufs[half], statb, mb], writes=[xnb[b]])
        fw.op("pool", lambda e, b=b: e.tensor_tensor(out=xt[b][:], in0=xt[b][:], in1=xn[b][:], op=ALU.add),
              reads=[xnb[b], xtb[b]], writes=[xtb[b]])
        for d in dsts:
            fw.dma("sp", d, xt[b][:], reads=[xtb[b]])

    def wout_phase(li, xsrc_fn):
        tiles = list(range(NT)) if li == 0 else list(range(2, NT))
        fw.dma("pool", wout[:, :, :], I["w_out"][li].rearrange("(k p) c -> p k c", p=128), writes=[woutb])
        for n, tt in enumerate(tiles):
            pis = []
            for half in range(2):
                pi = ps_main()
                pis.append(pi)
                for kc in range(8):
                    fw.op("pe", lambda e, pi=pi, kc=kc, tt=tt, half=half: e.matmul(
                        PS[pi][:, :], mT[:, kc, tt * 128:(tt + 1) * 128], wout[:, kc, half * 512:(half + 1) * 512],
                        start=(kc == 0), stop=(kc == 7)),
                        reads=[woutb], writes=[PSB[pi]], inc=(kc == 7), pe_acc=True)
            gidx = 1 if tt < 2 else 0
            resid_tile(n, tt, [PS[pis[0]][:, :], PS[pis[1]][:, :]], [PSB[pis[0]], PSB[pis[1]]], gidx,
                       xsrc_fn(tt), [Xd[tt * 128:(tt + 1) * 128, :]])
        fw.barrier()

    NFC = FFN_DENSE // 128
    gTd = AR[:, 0:NFC * 768].rearrange("p (f t) -> p f t", f=NFC)
    gTdb = Buf()
    W2d = AR[:, NFC * 768:NFC * 768 + NFC * 1024].rearrange("p (f c) -> p f c", f=NFC)
    W2db = Buf()
    SLU = [AR[:, NFC * 1792 + i * 512:NFC * 1792 + (i + 1) * 512] for i in range(2)]
    SLUb = [Buf(), Buf()]

    def ffn_dense(li):
        w1, w3, w2 = I["w1_dense"][0], I["w3_dense"][0], I["w2_dense"][0]
        for g0 in range(0, NFC, 8):
            ng = min(8, NFC - g0)
            fw.dma("pool", W2d[:, g0:g0 + ng, :], w2[g0 * 128:(g0 + ng) * 128, :].rearrange("(f p) c -> p f c", p=128),
                   writes=[W2db])
        cnt = 0
        for third in range(3):
            tok0 = third * 768
            for g0 in range(0, NFC, 4):
                ng = min(4, NFC - g0)
                wa, wab = load_w(w1[:, g0 * 128:(g0 + ng) * 128], ng * 128)
                wc, wcb = load_w(w3[:, g0 * 128:(g0 + ng) * 128], ng * 128)
                for c in range(ng):
                    fc = g0 + c
                    for (s0, sn) in ((0, 512), (512, 256)):
                        t0 = tok0 + s0
                        rb = [hTb[t0 // 128 + q] for q in range(sn // 128)]
                        pa_, pb_ = ps_main(), ps_main()
                        for (pp, ww, wwb) in ((pa_, wa, wab), (pb_, wc, wcb)):
                            for kc in range(8):
                                fw.op("pe", lambda e, pp=pp, ww=ww, kc=kc, c=c, t0=t0, sn=sn: e.matmul(
                                    PS[pp][:, 0:sn], ww[:, kc, c * 128:(c + 1) * 128], hT[:, kc, t0:t0 + sn],
                                    start=(kc == 0), stop=(kc == 7)),
                                    reads=[wwb] + rb, writes=[PSB[pp]], inc=(kc == 7), pe_acc=True)
                        sl = cnt % 2
                        cnt += 1
                        fw.op("act", lambda e, pa_=pa_, sl=sl, sn=sn: e.activation(out=SLU[sl][:, 0:sn], in_=PS[pa_][:, 0:sn],
                                                                                 func=AF.Silu),
                              reads=[PSB[pa_]], writes=[SLUb[sl]])
                        fw.op("dve", lambda e, pb_=pb_, sl=sl, sn=sn, fc=fc, s0=s0: e.tensor_tensor(
                            out=gTd[:, fc, s0:s0 + sn], in0=PS[pb_][:, 0:sn], in1=SLU[sl][:, 0:sn], op=ALU.mult),
                            reads=[PSB[pb_], SLUb[sl]], writes=[gTdb])
            for q in range(6):
                tt = third * 6 + q
                pis = []
                for half in range(2):
                    pi = ps_main()
                    pis.append(pi)
                    for fc in range(NFC):
                        fw.op("pe", lambda e, pi=pi, fc=fc, q=q, half=half: e.matmul(
                            PS[pi][:, :], gTd[:, fc, q * 128:(q + 1) * 128], W2d[:, fc, half * 512:(half + 1) * 512],
                            start=(fc == 0), stop=(fc == NFC - 1)),
                            reads=[gTdb, W2db], writes=[PSB[pi]], inc=(fc == NFC - 1), pe_acc=True)
                gidx = 3 if tt < 2 else 2
                resid_tile(q, tt, [PS[pis[0]][:, :], PS[pis[1]][:, :]], [PSB[pis[0]], PSB[pis[1]]], gidx,
                           Xd[tt * 128:(tt + 1) * 128, :], [Xd[tt * 128:(tt + 1) * 128, :]])
        fw.barrier()

    NFE = FFN_EXPERT // 128
    oacc = AR[:, 0:32768].bitcast(F32).rearrange("p (t c) -> p t c", t=16)
    oaccb = [Buf() for _ in range(16)]
    W2m = [AR[:, 32768 + i * 4096:32768 + (i + 1) * 4096].rearrange("p (f c) -> p f c", f=4) for i in range(2)]
    W2mb = [Buf(), Buf()]
    SLm = [AR[:, 40960 + i * 512:40960 + (i + 1) * 512] for i in range(2)]
    SLmb = [Buf(), Buf()]
    comb = AR[:, 41984:41984 + 256].bitcast(F32).rearrange("p (t e) -> p t e", t=16)
    combb = Buf()
    gTm = [cos16, sin16, cos8, sin8]
    gTmb = [Buf() for _ in range(4)]
    LT = [(C + i * 512, 512) for i in range(4)]

    def moe_router():
        wr, wrb = load_w(I["w_router"][0], 8)
        A_, B_ = stat[:, 28:36], stat[:, 36:44]
        for t in range(16):
            tok = C + t * 128
            pi = ps_main()
            for kc in range(8):
                fw.op("pe", lambda e, pi=pi, kc=kc, tok=tok: e.matmul(
                    PS[pi][:, 0:8], hT[:, kc, tok:tok + 128], wr[:, kc, 0:8], start=(kc == 0), stop=(kc == 7)),
                    reads=[wrb, hTb[2 + t]], writes=[PSB[pi]], inc=(kc == 7), pe_acc=True)
            fw.op("dve", lambda e, pi=pi: e.tensor_copy(out=A_, in_=PS[pi][:, 0:8]), reads=[PSB[pi]], writes=[statb])
            fw.op("dve", lambda e: e.reduce_max(out=stat[:, 44:45], in_=A_, axis=AX.X), reads=[statb], writes=[statb])
            fw.op("dve", lambda e: e.tensor_scalar(out=B_, in0=A_, scalar1=stat[:, 44:45], scalar2=None,
                                                   op0=ALU.is_equal), reads=[statb], writes=[statb])
            fw.op("dve", lambda e: e.scalar_tensor_tensor(out=A_, in0=B_, scalar=-1e30, in1=A_, op0=ALU.mult,
                                                          op1=ALU.add), reads=[statb], writes=[statb])
            fw.op("dve", lambda e: e.reduce_max(out=stat[:, 45:46], in_=A_, axis=AX.X), reads=[statb], writes=[statb])
            fw.op("dve", lambda e: e.tensor_scalar(out=A_, in0=A_, scalar1=stat[:, 45:46], scalar2=None,
                                                   op0=ALU.is_equal), reads=[statb], writes=[statb])
            fw.op("dve", lambda e: e.tensor_tensor(out=stat[:, 46:47], in0=stat[:, 45:46], in1=stat[:, 44:45],
                                                   op=ALU.subtract), reads=[statb], writes=[statb])
            fw.op("act", lambda e: e.activation(out=stat[:, 47:48], in_=stat[:, 46:47], func=AF.Exp),
                  reads=[statb], writes=[statb])
            fw.op("dve", lambda e: e.tensor_scalar(out=stat[:, 48:49], in0=stat[:, 47:48], scalar1=1.0, scalar2=None,
                                                   op0=ALU.add), reads=[statb], writes=[statb])
            fw.op("dve", lambda e: e.reciprocal(out=stat[:, 48:49], in_=stat[:, 48:49]), reads=[statb], writes=[statb])
            fw.op("dve", lambda e: e.tensor_tensor(out=stat[:, 49:50], in0=stat[:, 47:48], in1=stat[:, 48:49],
                                                   op=ALU.mult), reads=[statb], writes=[statb])
            fw.op("dve", lambda e: e.tensor_scalar(out=B_, in0=B_, scalar1=stat[:, 48:49], scalar2=None,
                                                   op0=ALU.mult), reads=[statb], writes=[statb])
            fw.op("dve", lambda e, t=t: e.scalar_tensor_tensor(out=comb[:, t, :], in0=A_, scalar=stat[:, 49:50],
                                                              in1=B_, op0=ALU.mult, op1=ALU.add),
                  reads=[statb], writes=[combb])

    def moe_phase():
        moe_router()
        w1, w3, w2 = I["w1_moe"][0], I["w3_moe"][0], I["w2_moe"][0]
        cnt = 0
        first = True
        gi = 0
        for ex in range(N_EXPERTS):
            for g0 in range(0, NFE, 4):
                wa, wab = load_w(w1[ex][:, g0 * 128:(g0 + 4) * 128], 512)
                wc, wcb = load_w(w3[ex][:, g0 * 128:(g0 + 4) * 128], 512)
                wi = gi % 2
                gi += 1
                fw.dma("pool", W2m[wi][:, :, :], w2[ex][g0 * 128:(g0 + 4) * 128, :].rearrange("(f p) c -> p f c", p=128),
                       writes=[W2mb[wi]])
                for c in range(4):
                    for (t0, tn) in LT:
                        rb = [hTb[t0 // 128 + q] for q in range(4)]
                        pa_, pb_ = ps_main(), ps_main()
                        for (pp, ww, wwb) in ((pa_, wa, wab), (pb_, wc, wcb)):
                            for kc in range(8):
                                fw.op("pe", lambda e, pp=pp, ww=ww, kc=kc, c=c, t0=t0: e.matmul(
                                    PS[pp][:, :], ww[:, kc, c * 128:(c + 1) * 128], hT[:, kc, t0:t0 + 512],
                                    start=(kc == 0), stop=(kc == 7)),
                                    reads=[wwb] + rb, writes=[PSB[pp]], inc=(kc == 7), pe_acc=True)
                        sl = cnt % 2
                        cnt += 1
                        fw.op("act", lambda e, pa_=pa_, sl=sl: e.activation(out=SLm[sl][:, :], in_=PS[pa_][:, :],
                                                                          func=AF.Silu),
                              reads=[PSB[pa_]], writes=[SLmb[sl]])
                        fw.op("dve", lambda e, pb_=pb_, sl=sl, c=c, t0=t0: e.tensor_tensor(
                            out=gTm[c][:, t0 - C:t0 - C + 512], in0=PS[pb_][:, :], in1=SLm[sl][:, :], op=ALU.mult),
                            reads=[PSB[pb_], SLmb[sl]], writes=[gTmb[c]])
                for t in range(16):
                    for half in range(2):
                        pi = ps_aux()
                        for c in range(4):
                            fw.op("pe", lambda e, pi=pi, c=c, t=t, half=half, wi=wi: e.matmul(
                                PS[pi][:, :], gTm[c][:, t * 128:(t + 1) * 128], W2m[wi][:, c, half * 512:(half + 1) * 512],
                                start=(c == 0), stop=(c == 3)),
                                reads=[gTmb[c], W2mb[wi]], writes=[PSB[pi]], inc=(c == 3), pe_acc=True)
                        if first:
                            fw.op("dve", lambda e, pi=pi, t=t, half=half, ex=ex: e.tensor_scalar(
                                out=oacc[:, t, half * 512:(half + 1) * 512], in0=PS[pi][:, :],
                                scalar1=comb[:, t, ex:ex + 1], scalar2=None, op0=ALU.mult),
                                reads=[PSB[pi], combb], writes=[oaccb[t]])
                        else:
                            fw.op("dve", lambda e, pi=pi, t=t, half=half, ex=ex: e.scalar_tensor_tensor(
                                out=oacc[:, t, half * 512:(half + 1) * 512], in0=PS[pi][:, :],
                                scalar=comb[:, t, ex:ex + 1], in1=oacc[:, t, half * 512:(half + 1) * 512],
                                op0=ALU.mult, op1=ALU.add),
                                reads=[PSB[pi], combb, oaccb[t]], writes=[oaccb[t]])
                first = False
        for t in range(16):
            tt = 2 + t
            resid_tile(t, tt, [oacc[:, t, 0:512], oacc[:, t, 512:1024]], [oaccb[t], oaccb[t]], 2,
                       Xd[tt * 128:(tt + 1) * 128, :], [out[t * 128:(t + 1) * 128, :]])
        fw.barrier()

    def layer1():
        xs = lambda tt: Xd[tt * 128:(tt + 1) * 128, :]
        layer_vectors(1)
        fw.barrier()
        norm_phase(1, 0, xs, list(range(NT)))
        fw.barrier()
        branch_a(1)
        branch_b(1)
        branch_c(1)
        branch_d(1)
        fw.mark("t_l1attn")
        merge_phase(1)
        wout_phase(1, xs)
        fw.mark("t_l1mix")
        norm_phase(1, 1, xs, list(range(2, NT)))
        fw.barrier()
        moe_phase()

    def layer0():
        branch_a(0)
        fw.mark("t_a")
        branch_b(0)
        fw.mark("t_b")
        branch_c(0)
        fw.mark("t_c")
        branch_d(0)
        fw.mark("t_l0attn")
        merge_phase(0)
        wout_phase(0, x_src0)
        fw.mark("t_l0mix")
        norm_phase(0, 1, lambda tt: Xd[tt * 128:(tt + 1) * 128, :], list(range(NT)))
        fw.barrier()
        ffn_dense(0)
        fw.mark("t_l0")

    layer_vectors(0)
    build_rope(cos16, sin16, 16)
    build_rope(cos8, sin8, 8)
    fw.barrier()
    fw.mark("lv")
    norm_phase(0, 0, x_src0, list(range(NT)))
    fw.barrier()
    fw.mark("norm")
    if dbg is None or dbg["what"] == "full" or (dbg.get("stop") or "")[:2] in ("t_", "a_"):
        fw.mark("t_pre")
        layer0()
        layer1()
    if dbg is not None and dbg["what"] in ("x1", "x2"):
        branch_a(0)
        branch_b(0)
        branch_c(0)
        branch_d(0)
        merge_phase(0)
        wout_phase(0, x_src0)
        if dbg["what"] == "x2":
            norm_phase(0, 1, lambda tt: Xd[tt * 128:(tt + 1) * 128, :], list(range(NT)))
            fw.barrier()
            ffn_dense(0)
    if dbg is not None and dbg["what"] in ("oc", "qkc"):
        branch_c(0)
    if dbg is not None and dbg["what"] == "oa":
        branch_a(0)
    if dbg is not None and (dbg["what"] == "ob" or (dbg.get("stop") or "").startswith("b")):
        branch_b(0)
    if dbg is not None and dbg["what"] == "od":
        branch_d(0)

    fw.frozen = False
    fw.barrier()
    dcnt = [0]

    def dump(src, dst, rb=()):
        p, n = src.shape[0], src.shape[1]
        for c0 in range(0, n, 1024):
            w = min(1024, n - c0)
            b = dcnt[0] % 2
            dcnt[0] += 1
            fw.op("dve", lambda e, b=b, c0=c0, w=w: e.tensor_copy(out=xn[b][0:p, 0:w], in_=src[:, c0:c0 + w]),
                  reads=list(rb), writes=[xnb[b]])
            fw.dma("sp", dst[:, c0:c0 + w], xn[b][0:p, 0:w], reads=[xnb[b]])

    if dbg is not None and dbg["what"] in ("x1", "x2"):
        for tt in range(NT):
            b = tt % 2
            fw.dma("sp", xt[b][:], Xd[tt * 128:(tt + 1) * 128, :], writes=[xtb[b]])
            fw.dma("sp", dbg_out[tt * 128:(tt + 1) * 128, :], xt[b][:], reads=[xtb[b]])
    if dbg is not None and dbg["what"] == "bpv":
        for i in (3, 4):
            dump(PS[i][:, 0:260], dbg_out[i * 128:(i + 1) * 128, 0:260])
    if dbg is not None and dbg["what"] == "stage":
        dump(ident[:, :], dbg_out[:, :])
    if dbg is not None and dbg["what"] == "qkc":
        for c in range(4):
            dump(QT[:, c, :], dbg_out[c * 128:(c + 1) * 128, :])
        dump(KT[:, 0, :], dbg_out[512:640, :])
        dump(KT[:, 1, :], dbg_out[640:768, :])
    if dbg is not None and dbg["what"] == "rope":
        dump(cos16[:, :], dbg_out[0:128, :])
        dump(sin16[:, :], dbg_out[128:256, :])
        dump(pm16[:, :], dbg_out[256:384, 0:128])
        dump(cos8[:, :], dbg_out[384:512, :])
        dump(sin8[:, :], dbg_out[512:640, :])
        dump(pm8[:, :], dbg_out[640:768, 0:128])
    if dbg is not None and dbg["what"] == "hT":
        for kc in range(8):
            dump(hT[:, kc, :], dbg_out[kc * 128:(kc + 1) * 128, :])
    if dbg is not None and dbg["what"] in ("oa", "ob", "oc", "od"):
        br = "abcd".index(dbg["what"][1])
        for ec in range(4):
            for c0 in range(0, T, 512):
                w = min(512, T - c0)
                fw.dma("sp", OTs[0][:, 0, 0:w], OTd[br * 4 + ec, :, c0:c0 + w], writes=[OTsb[0]])
                dump(OTs[0][:, 0, 0:w], dbg_out[ec * 128:(ec + 1) * 128, c0:c0 + w], rb=[OTsb[0]])
    fw.barrier(only=["sp"])
    fw.emit()


def make_in_maps(inputs, cores):
    maps = []
    shared = {}
    for k, v in inputs.items():
        if k in ("x", "c", "ctx", "c_ctx"):
            continue
        a = np.ascontiguousarray(np.asarray(v, dtype=np.float32))
        shared[k] = a
    for b in cores:
        m = dict(shared)
        m["x"] = np.ascontiguousarray(np.asarray(inputs["x"][b], dtype=np.float32))
        m["ctx"] = np.ascontiguousarray(np.asarray(inputs["ctx"][b], dtype=np.float32))
        m["cvec"] = np.ascontiguousarray(
            np.stack([np.asarray(inputs["c"][b]), np.asarray(inputs["c_ctx"])]).astype(np.float32))
        maps.append(m)
    return maps


_NC_CACHE = {}


def kernel(**inputs):
    if "nc" not in _NC_CACHE:
        _NC_CACHE["nc"] = build_program()
    nc = _NC_CACHE["nc"]
    maps = make_in_maps(inputs, list(range(8)))
    res = run_bass_kernel_spmd(nc, maps, core_ids=list(range(8)))
    return np.stack([np.asarray(r["out"], dtype=np.float32) for r in res.results], axis=0)
```

```python
import contextlib
import math
import numpy as np
import concourse.bass as bass
import concourse.mybir as mybir
from concourse.bass_utils import run_bass_kernel_spmd

F32 = mybir.dt.float32
BF16 = mybir.dt.bfloat16
I32 = mybir.dt.int32
AF = mybir.ActivationFunctionType
ALU = mybir.AluOpType
AX = mybir.AxisListType

D = 1024
S = 2048
C = 256
T = S + C
NT = T // 128
DEPTH = 2
D_IN = 8352
EPS = 1e-6
FFN_DENSE = 2816
N_EXPERTS = 8
FFN_EXPERT = 3584
O_AQ, O_AK, O_AV = 0, 512, 1024
O_BQ, O_BK, O_BV = 1536, 2048, 2560
O_CQ, O_CK, O_CV = 3072, 3584, 3712
O_DQA, O_DKVA, O_DKR = 3840, 4096, 4224
O_G = 4256
TBLK = [(0, 256), (256, 512), (768, 512), (1280, 512), (1792, 512)]


class Buf:
    __slots__ = ("w", "r")

    def __init__(self):
        self.w = None
        self.r = {}


class Stream:
    def __init__(self, name, sem):
        self.name = name
        self.sem = sem
        self.count = 0
        self.waited = {}
        self.ops = []


class FW:
    def __init__(self, nc, es):
        self.nc = nc
        self.es = es
        self.st = {}
        for n in ("pe", "act", "dve", "pool", "sp"):
            self.st[n] = Stream(n, es.enter_context(nc.semaphore("c_" + n)))
        self.dpool = {}
        for q, n in (("sp", 12), ("pool", 8), ("act", 4)):
            self.dpool[q] = [[es.enter_context(nc.semaphore(f"d_{q}{i}")), 0] for i in range(n)]
        self.dnext = {"sp": 0, "pool": 0, "act": 0}
        self.nbuf = 0
        self.frozen = False
        self.stop = None

    def mark(self, name):
        if self.stop is not None and name == self.stop:
            self.frozen = True

    def _wait(self, s, ev):
        sem, val = ev
        k = id(sem)
        if s.waited.get(k, 0) < val:
            s.waited[k] = val
            s.ops.append(("w", sem, val))

    def _deps(self, s, reads, writes, pe_acc=False):
        for b in reads:
            if b.w is not None:
                self._wait(s, b.w)
        for b in writes:
            if b.w is not None and not (pe_acc and b.w[0] is s.sem):
                self._wait(s, b.w)
            for ev in b.r.values():
                self._wait(s, ev)

    def op(self, eng, fn, reads=(), writes=(), inc=True, pe_acc=False):
        if self.frozen:
            return
        s = self.st[eng]
        self._deps(s, reads, writes, pe_acc)
        ev = (s.sem, s.count + 1)
        if inc:
            s.count += 1
        s.ops.append(("i", fn, inc))
        for b in writes:
            b.w = ev
            b.r = {}
        for b in reads:
            b.r[id(s.sem)] = ev

    def dma(self, q, out, in_, reads=(), writes=(), **kw):
        if self.frozen:
            return
        s = self.st[q]
        pool = self.dpool[q]
        i = self.dnext[q]
        self.dnext[q] = (i + 1) % len(pool)
        ent = pool[i]
        sem = ent[0]
        if ent[1] > 0:
            self._wait(s, (sem, ent[1]))
        self._deps(s, reads, writes)
        ent[1] += 16
        ev = (sem, ent[1])
        s.ops.append(("d", out, in_, sem, kw))
        for b in writes:
            b.w = ev
            b.r = {}
        for b in reads:
            b.r[id(sem)] = ev
        return ev

    def all_events(self):
        evs = []
        for n, s in self.st.items():
            if s.count > 0:
                evs.append((s.sem, s.count))
        for q, pool in self.dpool.items():
            for sem, val in pool:
                if val > 0:
                    evs.append((sem, val))
        return evs

    def barrier(self, only=None):
        if self.frozen:
            return
        evs = self.all_events()
        for n, s in self.st.items():
            if only is not None and n not in only:
                continue
            for ev in evs:
                if ev[0] is s.sem:
                    continue
                self._wait(s, ev)

    def emit(self):
        nc = self.nc
        hmap = {"pe": "tensor", "act": "scalar", "dve": "vector", "pool": "gpsimd", "sp": "sync"}
        with nc.Block() as block:
            for n, s in self.st.items():
                def body(e, s=s):
                    for o in s.ops:
                        if o[0] == "w":
                            e.wait_ge(o[1], o[2])
                        elif o[0] == "i":
                            ins = o[1](e)
                            if o[2]:
                                ins.then_inc(s.sem, 1)
                        else:
                            e.dma_start(out=o[1], in_=o[2], **o[4]).then_inc(o[3], 16)
                getattr(block, hmap[n])(body)


class K:
    pass


def build_program(dbg=None):
    nc = bass.Bass("TRN2", target_bir_lowering=False)
    es = contextlib.ExitStack()
    with es:
        _build(nc, es, dbg)
    return nc


def _dram_inputs(nc):
    L = DEPTH
    specs = {
        "x": [S, D], "ctx": [C, D], "cvec": [2, D],
        "w_ada": [L, D, 6 * D], "b_ada": [L, 6 * D],
        "g_mix_pre": [L, D], "g_mix_post": [L, D], "g_ffn_pre": [L, D], "g_ffn_post": [L, D],
        "w_in": [L, D, D_IN],
        "lam_q1": [L, 64], "lam_k1": [L, 64], "lam_q2": [L, 64], "lam_k2": [L, 64],
        "g_diff_sub": [L, 128], "na_rpb": [L, 8, 15, 31],
        "g_qnorm": [L, 64], "g_knorm": [L, 64], "g_q_lora": [L, 256],
        "w_uq": [L, 256, 768], "g_kv_lora": [L, 128], "w_ukv": [L, 128, 1024],
        "w_br_a": [L, 512, D], "w_br_b": [L, 512, D], "w_br_c": [L, 512, D], "w_br_d": [L, 512, D],
        "w_out": [L, D, D],
        "w1_dense": [1, D, FFN_DENSE], "w3_dense": [1, D, FFN_DENSE], "w2_dense": [1, FFN_DENSE, D],
        "w_router": [1, D, N_EXPERTS],
        "w1_moe": [1, N_EXPERTS, D, FFN_EXPERT], "w3_moe": [1, N_EXPERTS, D, FFN_EXPERT],
        "w2_moe": [1, N_EXPERTS, FFN_EXPERT, D],
    }
    return {k: nc.dram_tensor(k, v, F32, kind="ExternalInput").ap() for k, v in specs.items()}


def _build(nc, es, dbg):
    fw = FW(nc, es)
    if dbg is not None:
        fw.stop = dbg.get("stop")
    I = _dram_inputs(nc)
    out = nc.dram_tensor("out", [S, D], F32, kind="ExternalOutput").ap()
    dbg_out = None
    if dbg is not None:
        dbg_out = nc.dram_tensor("dbg", list(dbg["shape"]), F32, kind="ExternalOutput").ap()
    Xd = nc.dram_tensor("Xres", [T, D], F32).ap()

    def sb(name, shape, dt):
        return es.enter_context(nc.sbuf_tensor(name, list(shape), dt))

    PSall = es.enter_context(nc.psum_tensor("psall", [128, 4096], F32))
    PS = [PSall[:, i * 512:(i + 1) * 512] for i in range(8)]
    PSB = [Buf() for _ in range(8)]

    ident = sb("ident", [128, 128], F32)
    ones_f = sb("ones_f", [128, 128], F32)
    cb = Buf()
    iot = sb("iot", [128, 128], F32)
    fw.op("pool", lambda e: e.iota(iot[:], [[1, 128]], base=0, channel_multiplier=-1,
                                  allow_small_or_imprecise_dtypes=True), writes=[cb])
    fw.op("dve", lambda e: e.tensor_single_scalar(out=ident[:], in_=iot[:], scalar=0.0, op=ALU.is_equal),
          reads=[cb], writes=[cb])
    fw.op("dve", lambda e: e.memset(ones_f[:], 1.0), writes=[cb])
    fw.mark("const0")

    colv = sb("colv", [128, 8, 8], F32)
    gb = sb("gb", [128, 4, D], F32)
    sTb = sb("sTb", [128, 8, 2, 128], BF16)
    sTf = sb("sTf", [128, 16], F32)
    cv_row = sb("cv_row", [48, 128], F32)
    modc = sb("modc", [128, 48, 2], F32)
    badc = sb("badc", [128, 48], F32)
    gcol = sb("gcol", [128, 4, 8], F32)
    XX = sb("XX", [128, 4, D], F32)
    xt = [XX[:, i, :] for i in range(2)]
    xtb = [Buf(), Buf()]
    xn = [XX[:, 2 + i, :] for i in range(2)]
    xnb = [Buf(), Buf()]
    stat = sb("stat", [128, 64], F32)
    statb = Buf()
    hT = sb("hT", [128, 8, T], BF16)
    hTb = [Buf() for _ in range(NT)]
    wt = [sb(f"wt{i}", [128, 8, 512], BF16) for i in range(3)]
    wtb = [Buf() for _ in range(3)]
    wcnt = [0]

    mb = Buf()

    def load_w(src, ncols, krows=1024):
        i = wcnt[0] % 3
        wcnt[0] += 1
        nk = krows // 128
        fw.dma("pool", wt[i][:, 0:nk, 0:ncols], src.rearrange("(k p) c -> p k c", p=128), writes=[wtb[i]])
        return wt[i], wtb[i]

    def to_cols(src_rows_ap, nrows, dst, ps_i=0):
        fw.dma("sp", cv_row[0:nrows, :], src_rows_ap, writes=[mb])
        fw.op("pe", lambda e: e.transpose(PS[ps_i][:, 0:nrows], cv_row[0:nrows, :], ident[0:nrows, 0:nrows]),
              reads=[mb, cb], writes=[PSB[ps_i]])
        fw.op("dve", lambda e: e.tensor_copy(out=dst, in_=PS[ps_i][:, 0:nrows]), reads=[PSB[ps_i]], writes=[mb])

    to_cols(I["cvec"].rearrange("j (k d) -> (j k) d", d=128), 16, sTf[:, 0:16])
    fw.op("act", lambda e: e.activation(out=sTf[:, 0:16], in_=sTf[:, 0:16], func=AF.Silu), reads=[mb], writes=[mb])
    for j in range(2):
        for kc in range(8):
            fw.op("dve", lambda e, j=j, kc=kc: e.tensor_scalar(
                out=sTb[:, kc, j, :], in0=ones_f[:, :], scalar1=sTf[:, j * 8 + kc:j * 8 + kc + 1], scalar2=None,
                op0=ALU.mult), reads=[mb, cb], writes=[mb])

    fw.mark("stb")

    def layer_vectors(li):
        to_cols(I["b_ada"][li].rearrange("(r d) -> r d", d=128), 48, badc[:, :])
        for gi, nm in enumerate(("g_mix_pre", "g_mix_post", "g_ffn_pre", "g_ffn_post")):
            to_cols(I[nm][li].rearrange("(r d) -> r d", d=128), 8, gcol[:, gi, :])
        for piece in range(12):
            w, wb = load_w(I["w_ada"][li, :, piece * 512:(piece + 1) * 512], 512)
            for q in range(4):
                ech = piece * 4 + q
                for kc in range(8):
                    fw.op("pe", lambda e, q=q, kc=kc, ech=ech, w=w: e.matmul(
                        PS[1][:, ech * 2:ech * 2 + 2], w[:, kc, q * 128:(q + 1) * 128], sTb[:, kc, :, 0],
                        start=(kc == 0), stop=(kc == 7)),
                        reads=[wb, mb], writes=[PSB[1]], inc=(kc == 7 and q == 3), pe_acc=True)
            part = piece // 2
            if part in (2, 5):
                half = piece % 2
                for j in range(2):
                    pj = 2 + j
                    for kc in range(8):
                        fw.op("pe", lambda e, kc=kc, j=j, pj=pj, w=w: e.matmul(
                            PS[pj][:, :], sTb[:, kc, j, :], w[:, kc, :], start=(kc == 0), stop=(kc == 7)),
                            reads=[wb, mb], writes=[PSB[pj]], inc=(kc == 7), pe_acc=True)
                    idx = (0 if part == 2 else 2) + j
                    bsel = j
                    fw.dma("sp", xt[bsel][:, 0:512],
                           I["b_ada"][li:li + 1, piece * 512:(piece + 1) * 512].broadcast_to([128, 512]),
                           writes=[xtb[bsel]])
                    gname = "g_mix_post" if part == 2 else "g_ffn_post"
                    fw.dma("sp", xt[bsel][:, 512:1024],
                           I[gname][li:li + 1, half * 512:(half + 1) * 512].broadcast_to([128, 512]),
                           writes=[xtb[bsel]])
                    fw.op("dve", lambda e, pj=pj, bsel=bsel: e.tensor_tensor(
                        out=xn[bsel][:, 0:512], in0=PS[pj][:, :], in1=xt[bsel][:, 0:512], op=ALU.add),
                        reads=[PSB[pj], xtb[bsel]], writes=[xnb[bsel]])
                    fw.op("dve", lambda e, idx=idx, half=half, bsel=bsel: e.tensor_tensor(
                        out=gb[:, idx, half * 512:(half + 1) * 512], in0=xn[bsel][:, 0:512],
                        in1=xt[bsel][:, 512:1024], op=ALU.mult),
                        reads=[xnb[bsel], xtb[bsel]], writes=[mb])
        fw.op("dve", lambda e: e.tensor_copy(out=modc[:].rearrange("p a j -> p (a j)"), in_=PS[1][:, 0:96]),
              reads=[PSB[1]], writes=[mb])
        for j in range(2):
            for h, (gi, sci, shi) in enumerate(((0, 1, 0), (2, 4, 3))):
                setA = h * 4 + j * 2
                fw.op("dve", lambda e, j=j, sci=sci, setA=setA: e.scalar_tensor_tensor(
                    out=colv[:, setA, :], in0=modc[:, sci * 8:(sci + 1) * 8, j], scalar=1.0,
                    in1=badc[:, sci * 8:(sci + 1) * 8], op0=ALU.add, op1=ALU.add), reads=[mb], writes=[mb])
                fw.op("dve", lambda e, gi=gi, setA=setA: e.tensor_tensor(
                    out=colv[:, setA, :], in0=colv[:, setA, :], in1=gcol[:, gi, :], op=ALU.mult),
                    reads=[mb], writes=[mb])
                fw.op("dve", lambda e, j=j, shi=shi, setA=setA: e.tensor_tensor(
                    out=colv[:, setA + 1, :], in0=modc[:, shi * 8:(shi + 1) * 8, j],
                    in1=badc[:, shi * 8:(shi + 1) * 8], op=ALU.add), reads=[mb], writes=[mb])

    def norm_phase(li, which, src_ap_fn, tiles):
        for n, tt in enumerate(tiles):
            b = n % 2
            isctx = tt < 2
            fw.dma("sp", xt[b][:], src_ap_fn(tt), writes=[xtb[b]])
            fw.op("act", lambda e, b=b, tt=tt: e.activation(out=xn[b][:], in_=xt[b][:], func=AF.Square,
                                                          accum_out=stat[:, 0:1]),
                  reads=[xtb[b]], writes=[xnb[b], statb])
            fw.op("act", lambda e: e.activation(out=stat[:, 1:2], in_=stat[:, 0:1], func=AF.Ln,
                                                scale=1.0 / D, bias=epsc[:, 0:1]), reads=[statb, cb], writes=[statb])
            fw.op("act", lambda e: e.activation(out=stat[:, 2:3], in_=stat[:, 1:2], func=AF.Exp, scale=-0.5),
                  reads=[statb], writes=[statb])
            fw.op("dve", lambda e, b=b: e.tensor_scalar(out=xn[b][:], in0=xt[b][:], scalar1=stat[:, 2:3],
                                                       scalar2=None, op0=ALU.mult),
                  reads=[xtb[b], statb], writes=[xnb[b]])
            setA = which * 4 + (2 if isctx else 0)
            for half in range(2):
                pb = 6 + half
                for q in range(4):
                    kc = half * 4 + q
                    fw.op("pe", lambda e, b=b, kc=kc, q=q, pb=pb: e.transpose(
                        PS[pb][:, q * 128:(q + 1) * 128], xn[b][:, kc * 128:(kc + 1) * 128], ident[:]),
                        reads=[xnb[b], cb], writes=[PSB[pb]], inc=(q == 3), pe_acc=True)
                for q in range(4):
                    kc = half * 4 + q
                    if q % 2 == 0:
                        fw.op("act", lambda e, kc=kc, q=q, pb=pb, tt=tt, setA=setA: e.activation(
                            out=hT[:, kc, tt * 128:(tt + 1) * 128], in_=PS[pb][:, q * 128:(q + 1) * 128],
                            func=AF.Identity, scale=colv[:, setA, kc:kc + 1], bias=colv[:, setA + 1, kc:kc + 1]),
                            reads=[PSB[pb], mb], writes=[hTb[tt]])
                    else:
                        fw.op("dve", lambda e, kc=kc, q=q, pb=pb, tt=tt, setA=setA: e.tensor_scalar(
                            out=hT[:, kc, tt * 128:(tt + 1) * 128], in0=PS[pb][:, q * 128:(q + 1) * 128],
                            scalar1=colv[:, setA, kc:kc + 1], scalar2=colv[:, setA + 1, kc:kc + 1],
                            op0=ALU.mult, op1=ALU.add),
                            reads=[PSB[pb], mb], writes=[hTb[tt]])

    epsc = sb("epsc", [128, 1], F32)
    fw.op("dve", lambda e: e.memset(epsc[:], EPS), writes=[cb])

    def x_src0(tt):
        return I["ctx"][tt * 128:(tt + 1) * 128, :] if tt < 2 else I["x"][(tt - 2) * 128:(tt - 1) * 128, :]


    ARENA_ELEMS = 44200
    AR = sb("arena", [128, ARENA_ELEMS], BF16)
    aoff = [0]

    def carve(nelem_bf16):
        o = aoff[0]
        aoff[0] += nelem_bf16
        assert aoff[0] <= ARENA_ELEMS, aoff[0]
        return AR[:, o:o + nelem_bf16]

    QT = carve(4 * T).rearrange("p (a t) -> p a t", a=4)
    KT = carve(4 * T).rearrange("p (a t) -> p a t", a=4)
    VA = carve(NT * 520).rearrange("p (a t) -> p a t", a=NT)
    OTs1 = carve(4 * 512).rearrange("p (a t) -> p a t", a=4)
    ON = carve(4 * 512).rearrange("p (a t) -> p a t", a=4)
    OFr = carve(2048)
    OF = OFr.bitcast(F32).rearrange("p (m q d) -> p m q d", m=2, q=4)
    TB2r = AR[:, aoff[0]:aoff[0] + 8192]
    TB2 = TB2r.rearrange("p (i h q) -> p i h q", i=16, h=8)
    TM = [carve(1024).bitcast(F32) for i in range(4)]
    RS = [carve(1024).bitcast(F32) for i in range(2)]
    SQ = [carve(512) for i in range(2)]
    RAW = [carve(512) for i in range(2)]
    ETp = [carve(1024) for i in range(2)]
    ET = [ETp[0][:, 0:512], ETp[0][:, 512:1024], ETp[1][:, 0:512], ETp[1][:, 512:1024]]
    KRr = AR[:, 4 * T * 2 + NT * 520 + 2048: 4 * T * 2 + NT * 520 + 2048 + 4096]
    XXb = XX[:].rearrange("p a d -> p (a d)").bitcast(BF16)
    DQ = XXb[:, 0:2 * T].rearrange("p (a t) -> p a t", a=2)
    DKV = XXb[:, 2 * T:3 * T]
    QTb = [Buf() for _ in TBLK]
    KTb = [Buf() for _ in TBLK]
    VAb = [Buf() for _ in range(NT)]
    ETb = [Buf() for _ in range(4)]
    ecnt = [0]
    RAWb = [Buf(), Buf()]
    SQb = [Buf(), Buf()]
    RSb = [Buf(), Buf()]
    TMb = [Buf() for _ in range(4)]
    rcnt = [0]
    ONb = Buf()
    OFb = Buf()
    OTs = [OTs1]
    OTsb = [Buf()]
    otcnt = [0]
    OTd = nc.dram_tensor("OTd", [16, 128, T], BF16).ap()
    identb = sb("identb", [128, 128], BF16)
    onesb = sb("onesb", [128, 128], BF16)
    bd64 = sb("bd64", [128, 128], BF16)
    pm16 = sb("pm16", [128, 128], BF16)
    pm8 = sb("pm8", [128, 128], BF16)
    cos16 = sb("cos16", [128, S], BF16)
    sin16 = sb("sin16", [128, S], BF16)
    cos8 = sb("cos8", [128, S], BF16)
    sin8 = sb("sin8", [128, S], BF16)
    pcol = sb("pcol", [128, 16], F32)
    pcoli = sb("pcoli", [128, 8], I32)
    mcoli = XX[:, 0, 384:512].bitcast(I32)
    mcolf = XX[:, 0, 256:384]
    mtmp = XX[:, 0, 0:128]
    mtmp2 = XX[:, 0, 128:256]
    brv = sb("brv", [128, 16], F32)
    brb = Buf()
    lamrow = TM[0][:, 0:256].rearrange("p (a d) -> p a d", a=4)
    psA = [0]
    psX = [0]

    def ps_main():
        i = psA[0] % 4
        psA[0] += 1
        return i

    def ps_aux():
        i = 4 + psX[0] % 3
        psX[0] += 1
        return i

    fw.op("dve", lambda e: e.tensor_copy(out=identb[:], in_=ident[:]), reads=[cb], writes=[cb])
    fw.op("dve", lambda e: e.memset(onesb[:], 1.0), writes=[cb])
    fw.op("dve", lambda e: e.memset(bd64[:], 0.0), writes=[cb])
    fw.op("dve", lambda e: e.memset(bd64[0:64, 0:64], 1.0), writes=[cb])
    fw.op("dve", lambda e: e.memset(bd64[64:128, 64:128], 1.0), writes=[cb])
    fw.op("pool", lambda e: e.iota(mcoli[:], [[1, 128]], base=0, channel_multiplier=0), writes=[cb])
    fw.op("pool", lambda e: e.iota(pcoli[:, 0:1], [[0, 1]], base=0, channel_multiplier=1), writes=[cb])

    def build_pm(pm, h):
        fw.op("dve", lambda e: e.tensor_single_scalar(out=mcoli[:], in_=mcoli[:], scalar=h, op=ALU.bitwise_and),
              reads=[cb], writes=[cb])
        fw.op("dve", lambda e: e.tensor_copy(out=mcolf[:], in_=mcoli[:]), reads=[cb], writes=[cb])
        fw.op("dve", lambda e: e.tensor_single_scalar(out=mcolf[:], in_=mcolf[:], scalar=0.5, op=ALU.is_gt),
              reads=[cb], writes=[cb])
        fw.op("dve", lambda e: e.tensor_single_scalar(out=mtmp[:], in_=iot[:], scalar=float(h), op=ALU.is_equal),
              reads=[cb], writes=[cb])
        fw.op("dve", lambda e: e.tensor_tensor(out=mtmp[:], in0=mtmp[:], in1=mcolf[:], op=ALU.mult),
              reads=[cb], writes=[cb])
        fw.op("dve", lambda e: e.tensor_single_scalar(out=mtmp2[:], in_=iot[:], scalar=float(-h), op=ALU.is_equal),
              reads=[cb], writes=[cb])
        fw.op("dve", lambda e: e.tensor_scalar(out=mcolf[:], in0=mcolf[:], scalar1=-1.0, scalar2=1.0,
                                               op0=ALU.mult, op1=ALU.add), reads=[cb], writes=[cb])
        fw.op("dve", lambda e: e.tensor_tensor(out=mtmp2[:], in0=mtmp2[:], in1=mcolf[:], op=ALU.mult),
              reads=[cb], writes=[cb])
        fw.op("dve", lambda e: e.tensor_tensor(out=pm[:], in0=mtmp[:], in1=mtmp2[:], op=ALU.subtract),
              reads=[cb], writes=[cb])
        fw.op("pool", lambda e: e.iota(mcoli[:], [[1, 128]], base=0, channel_multiplier=0), reads=[cb], writes=[cb])

    fw.mark("cmat")
    build_pm(pm16, 16)
    build_pm(pm8, 8)
    fw.barrier()
    fw.mark("pm")

    def build_rope(cosT, sinT, h):
        f32v = AR[:, 0:4 * T].bitcast(F32)
        f32w = AR[:, 4 * T:8 * T].bitcast(F32)
        Rr, Cc = f32v[:, 0:S], f32v[:, S:2 * S]
        U, Fr = f32w[:, 0:S], f32w[:, S:2 * S]
        Ui = AR[:, 8 * T:8 * T + 2 * S].bitcast(I32)
        fw.op("dve", lambda e: e.tensor_single_scalar(out=pcoli[:, 1:2], in_=pcoli[:, 0:1], scalar=h - 1,
                                                      op=ALU.bitwise_and), reads=[cb], writes=[cb])
        fw.op("dve", lambda e: e.tensor_single_scalar(out=pcoli[:, 2:3], in_=pcoli[:, 0:1], scalar=2 * h,
                                                      op=ALU.bitwise_and), reads=[cb], writes=[cb])
        fw.op("dve", lambda e: e.tensor_single_scalar(out=pcoli[:, 3:4], in_=pcoli[:, 0:1], scalar=h,
                                                      op=ALU.bitwise_and), reads=[cb], writes=[cb])
        fw.op("dve", lambda e: e.tensor_copy(out=pcol[:, 1:4], in_=pcoli[:, 1:4]), reads=[cb], writes=[cb])
        fw.op("act", lambda e: e.activation(out=pcol[:, 4:5], in_=pcol[:, 1:2], func=AF.Exp,
                                            scale=-math.log(10000.0) / h), reads=[cb], writes=[cb])
        fw.op("dve", lambda e: e.tensor_single_scalar(out=pcol[:, 5:6], in_=pcol[:, 2:3], scalar=0.5, op=ALU.is_gt),
              reads=[cb], writes=[cb])
        fw.op("dve", lambda e: e.tensor_scalar(out=pcol[:, 6:7], in0=pcol[:, 3:4], scalar1=0.5, scalar2=2.0,
                                               op0=ALU.is_gt, op1=ALU.mult), reads=[cb], writes=[cb])
        fw.op("dve", lambda e: e.tensor_scalar(out=pcol[:, 6:7], in0=pcol[:, 6:7], scalar1=-1.0, scalar2=None,
                                               op0=ALU.add), reads=[cb], writes=[cb])
        fw.op("pool", lambda e: e.iota(Rr, [[1, 32], [0, 64]], base=0, channel_multiplier=0,
                                      allow_small_or_imprecise_dtypes=True), reads=[cb], writes=[cb])
        fw.op("pool", lambda e: e.iota(Cc, [[0, 32], [1, 64]], base=0, channel_multiplier=0,
                                      allow_small_or_imprecise_dtypes=True), reads=[cb], writes=[cb])
        fw.op("dve", lambda e: e.tensor_tensor(out=Cc, in0=Cc, in1=Rr, op=ALU.subtract), reads=[cb], writes=[cb])
        fw.op("dve", lambda e: e.scalar_tensor_tensor(out=Rr, in0=Cc, scalar=pcol[:, 5:6], in1=Rr,
                                                      op0=ALU.mult, op1=ALU.add), reads=[cb], writes=[cb])
        for which, dst, off in ((0, sinT, 0.5), (1, cosT, 0.75)):
            fw.op("dve", lambda e: e.tensor_scalar(out=U, in0=Rr, scalar1=pcol[:, 4:5], scalar2=1.0 / (2 * math.pi),
                                                   op0=ALU.mult, op1=ALU.mult), reads=[cb], writes=[cb])
            fw.op("dve", lambda e, off=off: e.tensor_scalar(out=U, in0=U, scalar1=off, scalar2=None, op0=ALU.add),
                  reads=[cb], writes=[cb])
            fw.op("dve", lambda e: e.tensor_copy(out=Ui, in_=U), reads=[cb], writes=[cb])
            fw.op("dve", lambda e: e.tensor_copy(out=Fr, in_=Ui), reads=[cb], writes=[cb])
            fw.op("dve", lambda e: e.tensor_tensor(out=Fr, in0=U, in1=Fr, op=ALU.subtract), reads=[cb], writes=[cb])
            fw.op("dve", lambda e: e.tensor_single_scalar(out=U, in_=Fr, scalar=0.0, op=ALU.is_lt),
                  reads=[cb], writes=[cb])
            fw.op("dve", lambda e: e.tensor_tensor(out=Fr, in0=Fr, in1=U, op=ALU.add), reads=[cb], writes=[cb])
            fw.op("dve", lambda e: e.tensor_single_scalar(out=U, in_=Fr, scalar=1.0, op=ALU.is_ge),
                  reads=[cb], writes=[cb])
            fw.op("dve", lambda e: e.tensor_tensor(out=Fr, in0=Fr, in1=U, op=ALU.subtract), reads=[cb], writes=[cb])
            fw.op("dve", lambda e: e.tensor_scalar(out=Fr, in0=Fr, scalar1=-0.5, scalar2=2 * math.pi * (1 - 1e-6),
                                                   op0=ALU.add, op1=ALU.mult), reads=[cb], writes=[cb])
            fw.op("act", lambda e, dst=dst: e.activation(out=dst[:], in_=Fr, func=AF.Sin),
                  reads=[cb], writes=[cb])


    def evac_copy(n, dst, src, reads, writes):
        if n % 2 == 0:
            fw.op("act", lambda e: e.activation(out=dst, in_=src, func=AF.Copy), reads=reads, writes=writes)
        else:
            fw.op("dve", lambda e: e.tensor_copy(out=dst, in_=src), reads=reads, writes=writes)

    def blocks_for(li):
        return list(range(5))

    def proj_fm(li, col0, nchunks, post, tbs, wsrc=None, krows=1024, rhs_fn=None, rbufs_fn=None, group=1):
        c = 0
        while c < nchunks:
            nload = min(4, nchunks - c)
            src = (wsrc if wsrc is not None else I["w_in"][li])[:, col0 + c * 128: col0 + (c + nload) * 128]
            w, wb = load_w(src, nload * 128, krows)
            nk = krows // 128
            for g0 in range(0, nload, group):
                for tb in tbs:
                    t0, tn = TBLK[tb]
                    pis = []
                    for gi in range(group):
                        cc = g0 + gi
                        pi = ps_main()
                        pis.append(pi)
                        for kc in range(nk):
                            rhs = rhs_fn(kc, t0, tn) if rhs_fn else hT[:, kc, t0:t0 + tn]
                            rb = rbufs_fn(tb) if rbufs_fn else [hTb[t0 // 128 + q] for q in range(tn // 128)]
                            fw.op("pe", lambda e, pi=pi, cc=cc, kc=kc, rhs=rhs, tn=tn, w=w, nk=nk: e.matmul(
                                PS[pi][:, 0:tn], w[:, kc, cc * 128:(cc + 1) * 128], rhs,
                                start=(kc == 0), stop=(kc == nk - 1)),
                                reads=[wb] + rb, writes=[PSB[pi]], inc=(kc == nk - 1), pe_acc=True)
                    post(c + g0, tb, pis)
            c += nload

    def post_plain(dst, dstb):
        cnt = [0]

        def f(ci, tb, pis):
            t0, tn = TBLK[tb]
            cnt[0] += 1
            evac_copy(cnt[0], dst[:, ci, t0:t0 + tn], PS[pis[0]][:, 0:tn], [PSB[pis[0]]], [dstb[tb]])
        return f

    def rope_apply(src_ps, src_psb, raw_i, dst, dstb, t0, tn, cosT, sinT, pm, prange=(0, 128)):
        p0, p1 = prange
        s0 = t0 - C
        pa = ps_aux()
        fw.op("pe", lambda e: e.matmul(PS[pa][p0:p1, 0:tn], pm[p0:p1, p0:p1], RAW[raw_i][p0:p1, 0:tn],
                                       start=True, stop=True),
              reads=[RAWb[raw_i], cb], writes=[PSB[pa]])
        a, b = rcnt[0] % 4, (rcnt[0] + 1) % 4
        rcnt[0] += 2
        if src_ps is not None:
            fw.op("dve", lambda e: e.tensor_tensor(out=TM[a][p0:p1, 0:tn], in0=src_ps[p0:p1, 0:tn],
                                                   in1=cosT[p0:p1, s0:s0 + tn], op=ALU.mult),
                  reads=[src_psb, cb], writes=[TMb[a]])
        else:
            fw.op("pool", lambda e: e.tensor_tensor(out=TM[a][p0:p1, 0:tn], in0=RAW[raw_i][p0:p1, 0:tn],
                                                    in1=cosT[p0:p1, s0:s0 + tn], op=ALU.mult),
                  reads=[RAWb[raw_i], cb], writes=[TMb[a]])
        fw.op("dve", lambda e: e.tensor_tensor(out=TM[b][p0:p1, 0:tn], in0=PS[pa][p0:p1, 0:tn],
                                               in1=sinT[p0:p1, s0:s0 + tn], op=ALU.mult),
              reads=[PSB[pa], cb], writes=[TMb[b]])
        fw.op("pool", lambda e: e.tensor_tensor(out=dst[p0:p1], in0=TM[a][p0:p1, 0:tn], in1=TM[b][p0:p1, 0:tn],
                                                op=ALU.add),
              reads=[TMb[a], TMb[b]], writes=[dstb])

    def post_rope(dst, dstb, cosT, sinT, pm):
        cnt = [0]

        def f(ci, tb, pis):
            t0, tn = TBLK[tb]
            pi = pis[0]
            if tb == 0:
                cnt[0] += 1
                evac_copy(cnt[0], dst[:, ci, t0:t0 + tn], PS[pi][:, 0:tn], [PSB[pi]], [dstb[tb]])
                return
            r = cnt[0] % 2
            cnt[0] += 1
            fw.op("act", lambda e: e.activation(out=RAW[r][:, 0:tn], in_=PS[pi][:, 0:tn], func=AF.Copy),
                  reads=[PSB[pi]], writes=[RAWb[r]])
            rope_apply(None, None, r, dst[:, ci, t0:t0 + tn], dstb[tb], t0, tn, cosT, sinT, pm)
        return f

    def post_norm(dst, dstb, redmat, cnt_feat, gcol_fn, rope=None, dst_idx=None):
        cnt = [0]

        def f(ci, tb, pis):
            t0, tn = TBLK[tb]
            pa = ps_aux()
            sqs = []
            for n, pi in enumerate(pis):
                r = cnt[0] % 2
                cnt[0] += 1
                sqs.append(r)
                fw.op("act", lambda e, r=r, pi=pi: e.activation(out=SQ[r][:, 0:tn], in_=PS[pi][:, 0:tn],
                                                                func=AF.Square),
                      reads=[PSB[pi]], writes=[SQb[r]])
            for n, r in enumerate(sqs):
                fw.op("pe", lambda e, r=r, n=n: e.matmul(PS[pa][:, 0:tn], redmat[:, :], SQ[r][:, 0:tn],
                                                         start=(n == 0), stop=(n == len(sqs) - 1)),
                      reads=[SQb[r], cb], writes=[PSB[pa]], inc=(n == len(sqs) - 1), pe_acc=True)
            rs = cnt[0] % 2
            fw.op("act", lambda e: e.activation(out=RS[rs][:, 0:tn], in_=PS[pa][:, 0:tn], func=AF.Ln,
                                                scale=1.0 / cnt_feat, bias=epsc[:, 0:1]),
                  reads=[PSB[pa], cb], writes=[RSb[rs]])
            fw.op("act", lambda e: e.activation(out=RS[rs][:, 0:tn], in_=RS[rs][:, 0:tn], func=AF.Exp, scale=-0.5),
                  reads=[RSb[rs]], writes=[RSb[rs]])
            for n, pi in enumerate(pis):
                cidx = (ci + n) if dst_idx is None else dst_idx(ci + n)
                g = gcol_fn(ci + n)
                if rope is None or tb == 0:
                    fw.op("dve", lambda e, pi=pi, cidx=cidx, g=g: e.scalar_tensor_tensor(
                        out=dst[:, cidx, t0:t0 + tn], in0=PS[pi][:, 0:tn], scalar=g, in1=RS[rs][:, 0:tn],
                        op0=ALU.mult, op1=ALU.mult), reads=[PSB[pi], RSb[rs], brb], writes=[dstb[tb]])
                else:
                    r = cnt[0] % 2
                    cnt[0] += 1
                    fw.op("dve", lambda e, pi=pi, r=r, g=g: e.scalar_tensor_tensor(
                        out=RAW[r][:, 0:tn], in0=PS[pi][:, 0:tn], scalar=g, in1=RS[rs][:, 0:tn],
                        op0=ALU.mult, op1=ALU.mult), reads=[PSB[pi], RSb[rs], brb], writes=[RAWb[r]])
                    cosT, sinT, pm = rope
                    rope_apply(None, None, r, dst[:, cidx, t0:t0 + tn], dstb[tb], t0, tn, cosT, sinT, pm)
        return f

    def proj_v(li, col0, nheads, dv, wsrc=None):
        ncols = nheads * dv
        src = (wsrc if wsrc is not None else I["w_in"][li])[:, col0:col0 + ncols]
        w, wb = load_w(src, ncols)
        for tt in range(NT):
            pi = ps_main()
            for kc in range(8):
                fw.op("pe", lambda e, pi=pi, kc=kc, tt=tt: e.matmul(
                    PS[pi][:, 0:ncols], hT[:, kc, tt * 128:(tt + 1) * 128], w[:, kc, 0:ncols],
                    start=(kc == 0), stop=(kc == 7)),
                    reads=[wb, hTb[tt]], writes=[PSB[pi]], inc=(kc == 7), pe_acc=True)
            va = VA[:, tt, 0:nheads * (dv + 1)].rearrange("p (h d) -> p h d", d=dv + 1)
            evac_copy(tt, va[:, :, 0:dv], PS[pi][:, 0:ncols].rearrange("p (h d) -> p h d", d=dv),
                      [PSB[pi]], [VAb[tt]])
            fw.op("pool", lambda e, va=va: e.memset(va[:, :, dv:dv + 1], 1.0), writes=[VAb[tt]])

    pvset = [0]

    def attn_head(steps, qap_fn, kap_fn, vcol, dv, scale, q0, nq, kcs, finish, vap_fn=None):
        nqt = nq // 128
        pset = pvset[0] % 2
        pvset[0] += 1
        pvb = [4 + 2 * pset, 5 + 2 * pset]
        W = dv + 1
        for n, kc in enumerate(kcs):
            steps.append(dict(k=kap_fn(kc), q=qap_fn(q0, nq), nq=nq, scale=scale, nqt=nqt, pvb=pvb, W=W,
                              v=(vap_fn(kc) if vap_fn else VA[:, kc, vcol:vcol + W]), first=(n == 0),
                              last=(n == len(kcs) - 1), finish=finish, mask=None))

    def run_steps(steps, look=3):
        def qk(st):
            si = ecnt[0] % 4
            ecnt[0] += 1
            st["si"] = si
            fw.op("pe", lambda e: e.matmul(PS[si][:, 0:st["nq"]], st["k"], st["q"], start=True, stop=True),
                  reads=[], writes=[PSB[si]])

        def rest(st):
            si = st["si"]
            nq, nqt, W, pvb = st["nq"], st["nqt"], st["W"], st["pvb"]
            fw.op("act", lambda e: e.activation(out=ET[si][:, 0:nq], in_=PS[si][:, 0:nq], func=AF.Exp,
                                                scale=st["scale"]),
                  reads=[PSB[si]], writes=[ETb[si]])
            for qt in range(nqt):
                bk = pvb[qt // 2]
                col = (qt % 2) * W
                stf = st["first"] and (qt % 2 == 0)
                last = st["last"]
                fw.op("pe", lambda e, bk=bk, col=col, qt=qt, stf=stf, last=last: e.matmul(
                    PS[bk][:, col:col + W], ET[si][:, qt * 128:(qt + 1) * 128], st["v"],
                    start=stf, stop=last, skip_group_check=True),
                    reads=[ETb[si]], writes=[PSB[bk]], inc=(last and (qt == nqt - 1 or qt % 2 == 1)), pe_acc=True)
            if st["last"]:
                st["finish"](pvb, nqt, W)

        n = len(steps)
        look = 4
        for i in range(min(look, n)):
            qk(steps[i])
        for i in range(0, n, 2):
            rest(steps[i])
            if i + 1 < n:
                rest(steps[i + 1])
            for j in (i + look, i + look + 1):
                if j < n:
                    qk(steps[j])

    def interleave(a_, b_):
        o_ = []
        for x_, y_ in zip(a_, b_):
            o_ += [x_, y_]
        return o_

    def finish_plain(h, dv):
        def f(pvb, nqt, W):
            for half in range((nqt + 1) // 2):
                bk = pvb[half]
                nq2 = min(2, nqt - half * 2)
                acc = PS[bk][:, 0:nq2 * W].rearrange("p (q w) -> p q w", w=W)
                fw.op("dve", lambda e, acc=acc, nq2=nq2: e.reciprocal(out=stat[:, 8:8 + nq2], in_=acc[:, :, dv]),
                      reads=[PSB[bk]], writes=[statb])
                fw.op("dve", lambda e, acc=acc, nq2=nq2, half=half: e.tensor_tensor(
                    out=ON[:, half * 2:half * 2 + nq2, h * dv:(h + 1) * dv], in0=acc[:, :, 0:dv],
                    in1=stat[:, 8:8 + nq2].unsqueeze(2).broadcast_to([128, nq2, dv]), op=ALU.mult),
                    reads=[PSB[bk], statb], writes=[ONb])
        return f

    def flush_o(branch, q0, nq, scale_col=None, ecs=(0, 1, 2, 3)):
        nqt = nq // 128
        si = 0
        for ec in ecs:
            pb = 0
            psb16 = PS[pb][:].bitcast(BF16)
            for qt in range(nqt):
                fw.op("pe", lambda e, qt=qt, ec=ec: e.transpose(psb16[:, qt * 128:(qt + 1) * 128],
                                                               ON[:, qt, ec * 128:(ec + 1) * 128], identb[:]),
                      reads=[ONb, cb], writes=[PSB[pb]], inc=(qt == nqt - 1), pe_acc=True)
            if scale_col is None:
                evac_copy(ec, OTs[si][:, ec, 0:nq], psb16[:, 0:nq], [PSB[pb]], [OTsb[si]])
            else:
                fw.op("dve", lambda e, ec=ec: e.tensor_scalar(out=OTs[si][:, ec, 0:nq], in0=psb16[:, 0:nq],
                                                             scalar1=scale_col, scalar2=None, op0=ALU.mult),
                      reads=[PSB[pb], brb], writes=[OTsb[si]])
        fw.dma("sp", OTd[branch * 4 + ecs[0]:branch * 4 + ecs[-1] + 1, :, q0:q0 + nq].rearrange("c p q -> p c q"),
               OTs[si][:, ecs[0]:ecs[-1] + 1, 0:nq], reads=[OTsb[si]])

    ALLK = list(range(NT))
    CTXK = [0, 1]

    def qblocks(li):
        return [0, 1, 2, 3, 4] if li == 0 else [1, 2, 3, 4]

    def branch_a(li):
        lam_init = 0.8 - 0.6 * math.exp(-0.3 * li)
        for n, nm in enumerate(("lam_q1", "lam_k1", "lam_q2", "lam_k2")):
            fw.dma("sp", lamrow[:, n, :], I[nm][li:li + 1, :].broadcast_to([128, 64]), writes=[brb])
        fw.dma("sp", brv[:, 5:6], I["g_diff_sub"][li:li + 1, :].rearrange("o d -> d o"), writes=[brb],
               allow_slow_non_contiguous=True)
        for n in range(2):
            fw.op("dve", lambda e, n=n: e.tensor_tensor(out=lamrow[:, 2 * n, :], in0=lamrow[:, 2 * n, :],
                                                       in1=lamrow[:, 2 * n + 1, :], op=ALU.mult),
                  reads=[brb], writes=[brb])
            fw.op("dve", lambda e, n=n: e.reduce_sum(out=stat[:, 20 + n:21 + n], in_=lamrow[:, 2 * n, :],
                                                    axis=AX.X), reads=[brb], writes=[brb])
        fw.op("act", lambda e: e.activation(out=stat[:, 20:22], in_=stat[:, 20:22], func=AF.Exp),
              reads=[brb], writes=[brb])
        fw.op("dve", lambda e: e.tensor_tensor(out=stat[:, 22:23], in0=stat[:, 21:22], in1=stat[:, 20:21],
                                               op=ALU.subtract), reads=[brb], writes=[brb])
        fw.op("dve", lambda e: e.tensor_scalar(out=brv[:, 6:7], in0=stat[:, 22:23], scalar1=-lam_init,
                                               scalar2=None, op0=ALU.add), reads=[brb], writes=[brb])
        fw.op("dve", lambda e: e.tensor_scalar(out=brv[:, 5:6], in0=brv[:, 5:6], scalar1=1.0 - lam_init,
                                               scalar2=None, op0=ALU.mult), reads=[brb], writes=[brb])
        proj_fm(li, O_AQ, 4, post_rope(QT, QTb, cos16, sin16, pm16), qblocks(li))
        proj_fm(li, O_AK, 4, post_rope(KT, KTb, cos16, sin16, pm16), range(5))
        proj_v(li, O_AV, 4, 128)
        fw.barrier()
        fw.mark("a_kv")

        def finish_a(h, m):
            def f(pvb, nqt, W):
                for half in range((nqt + 1) // 2):
                    bk = pvb[half]
                    nq2 = min(2, nqt - half * 2)
                    acc = PS[bk][:, 0:nq2 * W].rearrange("p (q w) -> p q w", w=W)
                    fw.op("dve", lambda e, acc=acc, nq2=nq2: e.reciprocal(out=stat[:, 8:8 + nq2], in_=acc[:, :, 128]),
                          reads=[PSB[bk]], writes=[statb])
                    fw.op("dve", lambda e, acc=acc, nq2=nq2, half=half: e.tensor_tensor(
                        out=OF[:, m, half * 2:half * 2 + nq2, :], in0=acc[:, :, 0:128],
                        in1=stat[:, 8:8 + nq2].unsqueeze(2).broadcast_to([128, nq2, 128]), op=ALU.mult),
                        reads=[PSB[bk], statb], writes=[OFb])
                if m == 1:
                    fw.op("dve", lambda e: e.scalar_tensor_tensor(
                        out=OF[:, 0, 0:nqt, :], in0=OF[:, 1, 0:nqt, :], scalar=brv[:, 6:7], in1=OF[:, 0, 0:nqt, :],
                        op0=ALU.mult, op1=ALU.add), reads=[OFb, brb], writes=[OFb])
                    fw.op("pool", lambda e: e.tensor_tensor(out=OF[:, 1, 0:nqt, :], in0=OF[:, 0, 0:nqt, :],
                                                            in1=OF[:, 0, 0:nqt, :], op=ALU.mult),
                          reads=[OFb], writes=[OFb])
                    fw.op("dve", lambda e: e.reduce_sum(out=stat[:, 16:16 + nqt], in_=OF[:, 1, 0:nqt, :], axis=AX.X),
                          reads=[OFb], writes=[statb])
                    fw.op("act", lambda e: e.activation(out=stat[:, 16:16 + nqt], in_=stat[:, 16:16 + nqt],
                                                        func=AF.Ln, scale=1.0 / 128, bias=epsc[:, 0:1]),
                          reads=[statb, cb], writes=[statb])
                    fw.op("act", lambda e: e.activation(out=stat[:, 16:16 + nqt], in_=stat[:, 16:16 + nqt],
                                                        func=AF.Exp, scale=-0.5), reads=[statb], writes=[statb])
                    fw.op("dve", lambda e: e.tensor_tensor(
                        out=ON[:, 0:nqt, h * 128:(h + 1) * 128], in0=OF[:, 0, 0:nqt, :],
                        in1=stat[:, 16:16 + nqt].unsqueeze(2).broadcast_to([128, nqt, 128]), op=ALU.mult),
                        reads=[OFb, statb], writes=[ONb])
            return f

        for tb in qblocks(li):
            q0, nq = TBLK[tb]
            kcs = CTXK if tb == 0 else ALLK
            steps = []
            for h in range(4):
                sm = [[], []]
                for m in range(2):
                    attn_head(sm[m], lambda a, n, h=h, m=m: QT[m * 64:(m + 1) * 64, h, a:a + n],
                              lambda kc, h=h, m=m: KT[m * 64:(m + 1) * 64, h, kc * 128:(kc + 1) * 128],
                              h * 129, 128, 0.125, q0, nq, kcs, finish_a(h, m))
                steps += interleave(sm[0], sm[1])
            run_steps(steps)
            flush_o(0, q0, nq, scale_col=brv[:, 5:6])
        fw.barrier()

    KRt = sb("KRt", [32, T], BF16)
    DQb = [Buf() for _ in TBLK]
    DKVb = [Buf() for _ in TBLK]
    KRb = Buf()

    def branch_d(li):
        fw.dma("sp", brv[:, 2:4], I["g_q_lora"][li].rearrange("(c d) -> d c", d=128), writes=[brb],
               allow_slow_non_contiguous=True)
        fw.dma("sp", brv[:, 4:5], I["g_kv_lora"][li:li + 1, :].rearrange("o d -> d o"), writes=[brb],
               allow_slow_non_contiguous=True)
        DKV3 = DKV.rearrange("p (a t) -> p a t", a=1)
        KR = KRt[:, :]
        proj_fm(li, O_DQA, 2, post_norm(DQ, DQb, onesb, 256, lambda c: brv[:, 2 + c:3 + c]), qblocks(li), group=2)
        proj_fm(li, O_DKVA, 1, post_norm(DKV3, DKVb, onesb, 128, lambda c: brv[:, 4:5]), range(5))
        w, wb = load_w(I["w_in"][li][:, O_DKR:O_DKR + 32], 32)
        for tb in range(5):
            t0, tn = TBLK[tb]
            pi = ps_main()
            for kc in range(8):
                fw.op("pe", lambda e, pi=pi, kc=kc, t0=t0, tn=tn, w=w: e.matmul(
                    PS[pi][0:32, 0:tn], w[:, kc, 0:32], hT[:, kc, t0:t0 + tn], start=(kc == 0), stop=(kc == 7)),
                    reads=[wb] + [hTb[t0 // 128 + q] for q in range(tn // 128)], writes=[PSB[pi]],
                    inc=(kc == 7), pe_acc=True)
            if tb == 0:
                fw.op("act", lambda e, pi=pi, t0=t0, tn=tn: e.activation(out=KR[0:32, t0:t0 + tn],
                                                                       in_=PS[pi][0:32, 0:tn], func=AF.Copy),
                      reads=[PSB[pi]], writes=[KRb])
            else:
                r = tb % 2
                fw.op("act", lambda e, pi=pi, r=r, tn=tn: e.activation(out=RAW[r][0:32, 0:tn], in_=PS[pi][0:32, 0:tn],
                                                                     func=AF.Copy), reads=[PSB[pi]], writes=[RAWb[r]])
                rope_apply(None, None, r, KR[:, t0:t0 + tn], KRb, t0, tn, cos8, sin8, pm8, prange=(0, 32))
        iq = wcnt[0] % 3
        wcnt[0] += 1
        wuq = wt[iq][:].rearrange("p k c -> p (k c)")[:, 0:1536].rearrange("p (k c) -> p k c", k=2)
        fw.dma("pool", wuq, I["w_uq"][li].rearrange("(k p) c -> p k c", p=128), writes=[wtb[iq]])
        ik = wcnt[0] % 3
        wcnt[0] += 1
        wukv = wt[ik][:].rearrange("p k c -> p (k c)")[:, 0:1024]
        fw.dma("pool", wukv, I["w_ukv"][li], writes=[wtb[ik]])
        wv = wukv.rearrange("p (h e) -> p h e", e=128)[:, :, 64:128]
        for tt in range(NT):
            pi = ps_main()
            fw.op("pe", lambda e, pi=pi, tt=tt: e.matmul(PS[pi][:, :], DKV[:, tt * 128:(tt + 1) * 128], wv,
                                                         start=True, stop=True),
                  reads=[wtb[ik], DKVb[0], DKVb[1], DKVb[2], DKVb[3], DKVb[4]], writes=[PSB[pi]])
            va = VA[:, tt, 0:520].rearrange("p (h d) -> p h d", d=65)
            evac_copy(tt, va[:, :, 0:64], PS[pi][:, :].rearrange("p (h d) -> p h d", d=64), [PSB[pi]], [VAb[tt]])
            fw.op("pool", lambda e, va=va: e.memset(va[:, :, 64:65], 1.0), writes=[VAb[tt]])
        sc_d = 96.0 ** -0.5
        for g in range(2):
            for hl in range(4):
                h = g * 4 + hl
                for tb in range(5):
                    t0, tn = TBLK[tb]
                    pi = ps_main()
                    fw.op("pe", lambda e, pi=pi, h=h, t0=t0, tn=tn: e.matmul(
                        PS[pi][0:64, 0:tn], wukv[:, h * 128:h * 128 + 64], DKV[:, t0:t0 + tn], start=True, stop=True),
                        reads=[wtb[ik], DKVb[tb]], writes=[PSB[pi]])
                    evac_copy(tb, KT[0:64, hl, t0:t0 + tn], PS[pi][0:64, 0:tn], [PSB[pi]], [KTb[tb]])
                    if tb not in qblocks(li):
                        continue
                    pq = ps_main()
                    for c in range(2):
                        fw.op("pe", lambda e, pq=pq, c=c, h=h, t0=t0, tn=tn: e.matmul(
                            PS[pq][0:96, 0:tn], wuq[:, c, h * 96:(h + 1) * 96], DQ[:, c, t0:t0 + tn],
                            start=(c == 0), stop=(c == 1)),
                            reads=[wtb[iq], DQb[tb]], writes=[PSB[pq]], inc=(c == 1), pe_acc=True)
                    if tb == 0:
                        evac_copy(hl, QT[0:96, hl, t0:t0 + tn], PS[pq][0:96, 0:tn], [PSB[pq]], [QTb[tb]])
                    else:
                        evac_copy(hl, QT[0:64, hl, t0:t0 + tn], PS[pq][0:64, 0:tn], [PSB[pq]], [QTb[tb]])
                        r = (hl + tb) % 2
                        fw.op("act", lambda e, pq=pq, r=r, tn=tn: e.activation(
                            out=RAW[r][64:96, 0:tn], in_=PS[pq][64:96, 0:tn], func=AF.Copy),
                            reads=[PSB[pq]], writes=[RAWb[r]])
                        pa = ps_aux()
                        fw.op("pe", lambda e, pa=pa, r=r, tn=tn: e.matmul(
                            PS[pa][64:96, 0:tn], pm8[64:96, 64:96], RAW[r][64:96, 0:tn], start=True, stop=True),
                            reads=[RAWb[r], cb], writes=[PSB[pa]])
                        a_, b_ = rcnt[0] % 4, (rcnt[0] + 1) % 4
                        rcnt[0] += 2
                        s0 = t0 - C
                        fw.op("pool", lambda e, r=r, a_=a_, s0=s0, tn=tn: e.tensor_tensor(
                            out=TM[a_][64:96, 0:tn], in0=RAW[r][64:96, 0:tn], in1=cos8[64:96, s0:s0 + tn],
                            op=ALU.mult), reads=[RAWb[r], cb], writes=[TMb[a_]])
                        fw.op("dve", lambda e, pa=pa, b_=b_, s0=s0, tn=tn: e.tensor_tensor(
                            out=TM[b_][64:96, 0:tn], in0=PS[pa][64:96, 0:tn], in1=sin8[64:96, s0:s0 + tn],
                            op=ALU.mult), reads=[PSB[pa], cb], writes=[TMb[b_]])
                        fw.op("pool", lambda e, a_=a_, b_=b_, hl=hl, t0=t0, tn=tn: e.tensor_tensor(
                            out=QT[64:96, hl, t0:t0 + tn], in0=TM[a_][64:96, 0:tn], in1=TM[b_][64:96, 0:tn],
                            op=ALU.add), reads=[TMb[a_], TMb[b_]], writes=[QTb[tb]])
            fw.barrier()
            for hl in range(4):
                fw.dma("sp", KT[64:96, hl, :], KR[0:32, :])
            fw.barrier()
            for tb in qblocks(li):
                q0, nq = TBLK[tb]
                kcs = CTXK if tb == 0 else ALLK
                steps = []
                for hl in range(4):
                    h = g * 4 + hl
                    attn_head(steps, lambda a, n, hl=hl: QT[0:96, hl, a:a + n],
                              lambda kc, hl=hl: KT[0:96, hl, kc * 128:(kc + 1) * 128],
                              h * 65, 64, sc_d, q0, nq, kcs, finish_plain(h, 64))
                run_steps(steps)
                flush_o(3, q0, nq, ecs=(2 * g, 2 * g + 1))
            fw.barrier()

    def build_tb2(li):
        rp32 = ET[0].bitcast(F32)
        rpb16 = ET[1]
        IE = ET[2]
        winf = OFr.bitcast(F32)
        fw.dma("sp", rp32[0:31, 0:120], I["na_rpb"][li].rearrange("h r c -> c (h r)"), writes=[ETb[0]],
               allow_slow_non_contiguous=True)
        fw.op("dve", lambda e: e.tensor_copy(out=rpb16[0:31, 0:120], in_=rp32[0:31, 0:120]),
              reads=[ETb[0]], writes=[ETb[1]])
        fw.op("dve", lambda e: e.tensor_single_scalar(out=IE[0:31, 0:128], in_=iot[0:31, :], scalar=48.0,
                                                      op=ALU.is_equal), reads=[cb], writes=[ETb[2]])
        fw.op("dve", lambda e: e.tensor_copy(out=pcol[:, 7:8], in_=pcoli[:, 0:1]), reads=[cb], writes=[cb])
        qc_ = winf[0:64, 0:64]
        fw.op("dve", lambda e: e.tensor_scalar(out=qc_, in0=iot[0:64, 0:64], scalar1=pcol[0:64, 7:8], scalar2=-8.0,
                                               op0=ALU.add, op1=ALU.add), reads=[cb], writes=[OFb])
        fw.op("dve", lambda e: e.tensor_scalar(out=qc_, in0=qc_, scalar1=0.0, scalar2=48.0,
                                               op0=ALU.max, op1=ALU.min), reads=[OFb], writes=[OFb])
        fw.op("dve", lambda e: e.tensor_scalar(out=qc_, in0=qc_, scalar1=pcol[0:64, 7:8], scalar2=None,
                                               op0=ALU.subtract), reads=[OFb, cb], writes=[OFb])
        m1 = winf[0:64, 64:128]
        fw.op("dve", lambda e: e.tensor_single_scalar(out=m1, in_=qc_, scalar=0.0, op=ALU.is_le),
              reads=[OFb], writes=[OFb])
        fw.op("dve", lambda e: e.tensor_single_scalar(out=qc_, in_=qc_, scalar=-16.0, op=ALU.is_gt),
              reads=[OFb], writes=[OFb])
        fw.op("dve", lambda e: e.tensor_tensor(out=m1, in0=m1, in1=qc_, op=ALU.mult), reads=[OFb], writes=[OFb])
        tbb = Buf()
        for q0 in range(0, 64, 4):
            pi = ps_main()
            for ql in range(4):
                qc = q0 + ql
                fw.op("pe", lambda e, pi=pi, ql=ql, qc=qc: e.matmul(
                    PS[pi][0:64, ql * 120:(ql + 1) * 120], IE[0:31, 63 - qc:127 - qc], rpb16[0:31, 0:120],
                    start=True, stop=True), reads=[ETb[1], ETb[2]], writes=[PSB[pi]], inc=(ql == 3), pe_acc=True)
            fw.op("act", lambda e, pi=pi, q0=q0: e.activation(
                out=TB2[0:64, 1:16, :, q0:q0 + 4].rearrange("p r h q -> p q h r"),
                in_=PS[pi][0:64, 0:480].rearrange("p (q h r) -> p q h r", q=4, h=8), func=AF.Exp),
                reads=[PSB[pi]], writes=[tbb])
        fw.op("dve", lambda e: e.tensor_tensor(
            out=TB2[0:64, 1:16, :, :].rearrange("p i h q -> p (i h) q"),
            in0=TB2[0:64, 1:16, :, :].rearrange("p i h q -> p (i h) q"),
            in1=m1.unsqueeze(1).broadcast_to([64, 120, 64]), op=ALU.mult), reads=[tbb, OFb], writes=[tbb])
        fw.op("dve", lambda e: e.memset(TB2[0:64, 0, :, :], 0.0), writes=[tbb])
        fw.op("dve", lambda e: e.memset(TB2[64:128, 15, :, :], 0.0), writes=[tbb])
        fw.dma("sp", TB2[64:128, 0:15, :, :], TB2[0:64, 1:16, :, :], reads=[tbb], writes=[tbb])

    def branch_b(li):
        proj_fm(li, O_BQ, 4, post_plain(QT, QTb), qblocks(li))
        proj_fm(li, O_BK, 4, post_plain(KT, KTb), range(5))
        proj_v(li, O_BV, 8, 64)
        fw.barrier()
        fw.mark("b_proj")
        build_tb2(li)
        fw.barrier()
        fw.mark("b_tb2")
        if li == 0:
            steps = []
            for hg in range(4):
                sm = [[], []]
                for hh in range(2):
                    h = 2 * hg + hh
                    pb = (h % 2) * 64
                    attn_head(sm[hh], lambda a, n, h=h, pb=pb: QT[pb:pb + 64, h // 2, a:a + n],
                              lambda kc, h=h, pb=pb: KT[pb:pb + 64, h // 2, kc * 128:(kc + 1) * 128],
                              h * 65, 64, 0.125, 0, 256, CTXK, finish_plain(h, 64))
                steps += interleave(sm[0], sm[1])
            run_steps(steps)
            flush_o(1, 0, 256)
            fw.barrier()
            fw.mark("b_ctx")
        items = []
        for a in range(16):
            pset = pvset[0] % 2
            pvset[0] += 1
            pvb = [4 + 2 * pset, 5 + 2 * pset]
            for rl in range(2):
                r = 2 * a + rl
                s_ = min(max(r - 4, 0), 24)
                chunks = [("c", 0), ("c", 1)] + [("w", ap) for ap in range(s_ // 2, (s_ + 7) // 2 + 1)]
                for n, (kind, idx) in enumerate(chunks):
                    items.append(dict(a=a, rl=rl, r=r, s=s_, n=n, kind=kind, idx=idx, pvb=pvb,
                                      last=(n == len(chunks) - 1)))

        def b_qk(it, i):
            bpair = ((0, 1), (2, 3))[i % 2]
            it["bpair"] = bpair
            kcol = it["idx"] * 128 if it["kind"] == "c" else C + it["idx"] * 128
            qcol = C + it["r"] * 64
            for h in range(8):
                hp = (h % 2) * 64
                bk_s = bpair[h % 2]
                g_ = h // 2
                fw.op("pe", lambda e, bk_s=bk_s, g_=g_, h=h, hp=hp, kcol=kcol, qcol=qcol: e.matmul(
                    PS[bk_s][:, g_ * 64:(g_ + 1) * 64], KT[hp:hp + 64, h // 2, kcol:kcol + 128],
                    QT[hp:hp + 64, h // 2, qcol:qcol + 64], start=True, stop=True),
                    reads=[], writes=[PSB[bk_s]], inc=(h >= 6), pe_acc=True)

        def b_rest(it, i):
            si = i % 3
            bpair = it["bpair"]
            r, s_, idx, kind, pvb = it["r"], it["s"], it["idx"], it["kind"], it["pvb"]
            po = it["rl"] * 64
            tt = idx if kind == "c" else 2 + idx
            for par in range(2):
                bk_s = bpair[par]
                fw.op("act", lambda e, si=si, bk_s=bk_s, par=par: e.activation(
                    out=ET[si][:, :].rearrange("p (g two q) -> p g two q", two=2, q=64)[:, :, par, :],
                    in_=PS[bk_s][:, 0:256].rearrange("p (g q) -> p g q", q=64), func=AF.Exp, scale=0.125),
                    reads=[PSB[bk_s]], writes=[ETb[si]])
            if kind == "w":
                dr0 = 2 * idx - r + 7
                fw.op("dve", lambda e, si=si, dr0=dr0: e.tensor_tensor(
                    out=ET[si][:, :], in0=ET[si][:, :],
                    in1=TB2[:, dr0 + 1, :, :].rearrange("p h q -> p (h q)"), op=ALU.mult),
                    reads=[ETb[si]], writes=[ETb[si]])
                if 2 * idx < s_:
                    fw.op("pool", lambda e, si=si: e.memset(ET[si][0:64, :], 0.0), writes=[ETb[si]])
                if 2 * idx + 1 >= s_ + 8:
                    fw.op("pool", lambda e, si=si: e.memset(ET[si][64:128, :], 0.0), writes=[ETb[si]])
            n, last = it["n"], it["last"]
            for h in range(8):
                bk = pvb[h // 4]
                col = (h % 4) * 65
                fw.op("pe", lambda e, si=si, h=h, bk=bk, col=col, tt=tt, n=n, last=last, po=po: e.matmul(
                    PS[bk][po:po + 64, col:col + 65], ET[si][:, h * 64:(h + 1) * 64],
                    VA[:, tt, h * 65:(h + 1) * 65], start=(n == 0 and h % 4 == 0), stop=last,
                    skip_group_check=True),
                    reads=[ETb[si]], writes=[PSB[bk]], inc=(last and h % 4 == 3), pe_acc=True)
            if last and it["rl"] == 1:
                a = it["a"]
                qt = a % 4
                for half in range(2):
                    bk = pvb[half]
                    acc = PS[bk][:, 0:260].rearrange("p (h w) -> p h w", w=65)
                    fw.op("dve", lambda e, acc=acc: e.reciprocal(out=stat[:, 8:12], in_=acc[:, :, 64]),
                          reads=[PSB[bk]], writes=[statb])
                    fw.op("dve", lambda e, acc=acc, half=half, qt=qt: e.tensor_tensor(
                        out=ON[:, qt, half * 256:(half + 1) * 256].rearrange("p (h d) -> p h d", d=64),
                        in0=acc[:, :, 0:64], in1=stat[:, 8:12].unsqueeze(2).broadcast_to([128, 4, 64]), op=ALU.mult),
                        reads=[PSB[bk], statb], writes=[ONb])
                if qt == 3:
                    flush_o(1, C + (a // 4) * 512, 512)

        b_qk(items[0], 0)
        for i, it in enumerate(items):
            will_flush = it["last"] and it["rl"] == 1 and it["a"] % 4 == 3
            if i + 1 < len(items) and not will_flush:
                b_qk(items[i + 1], i + 1)
            b_rest(it, i)
            if i + 1 < len(items) and will_flush:
                b_qk(items[i + 1], i + 1)
        fw.barrier()

    def branch_c(li):
        fw.dma("sp", brv[0:64, 0:1], I["g_qnorm"][li:li + 1, :].rearrange("o d -> d o"), writes=[brb],
               allow_slow_non_contiguous=True)
        fw.dma("sp", brv[64:128, 0:1], I["g_qnorm"][li:li + 1, :].rearrange("o d -> d o"), writes=[brb],
               allow_slow_non_contiguous=True)
        fw.dma("sp", brv[0:64, 1:2], I["g_knorm"][li:li + 1, :].rearrange("o d -> d o"), writes=[brb],
               allow_slow_non_contiguous=True)
        fw.dma("sp", brv[64:128, 1:2], I["g_knorm"][li:li + 1, :].rearrange("o d -> d o"), writes=[brb],
               allow_slow_non_contiguous=True)
        rope = (cos16, sin16, pm16)
        proj_fm(li, O_CQ, 4, post_norm(QT, QTb, bd64, 64, lambda c: brv[:, 0:1], rope), qblocks(li))
        proj_fm(li, O_CK, 1, post_norm(KT, KTb, bd64, 64, lambda c: brv[:, 1:2], rope), range(5))
        proj_v(li, O_CV, 2, 64)
        fw.barrier()
        fw.dma("sp", KT[64:128, 1, :], KT[0:64, 0, :])
        fw.dma("sp", KT[0:64, 1, :], KT[64:128, 0, :])
        fw.barrier()
        for tb in qblocks(li):
            q0, nq = TBLK[tb]
            kcs = CTXK if tb == 0 else ALLK
            steps = []
            for hg in range(4):
                sm = [[], []]
                for hh in range(2):
                    h = 2 * hg + hh
                    kvh = h // 4
                    pb = (h % 2) * 64
                    kch = 0 if kvh * 64 == pb else 1
                    attn_head(sm[hh], lambda a, n, h=h, pb=pb: QT[pb:pb + 64, h // 2, a:a + n],
                              lambda kc, kch=kch, pb=pb: KT[pb:pb + 64, kch, kc * 128:(kc + 1) * 128],
                              kvh * 65, 64, 0.125, q0, nq, kcs, finish_plain(h, 64))
                steps += interleave(sm[0], sm[1])
            run_steps(steps)
            flush_o(2, q0, nq)
        fw.barrier()

    mT = AR[:, 0:8 * T].rearrange("p (a t) -> p a t", a=8)
    mTb = [Buf() for _ in TBLK]
    OTt = [AR[:, 18432 + i * 8192:18432 + (i + 1) * 8192].rearrange("p (c q) -> p c q", c=16) for i in range(2)]
    OTtb = [Buf(), Buf()]
    wbr = [AR[:, 34816 + i * 2048:34816 + (i + 1) * 2048].rearrange("p (c q) -> p c q", c=16) for i in range(2)]
    wbrb = [Buf(), Buf()]
    SGm = [AR[:, 38912 + i * 512:38912 + (i + 1) * 512] for i in range(2)]
    SGmb = [Buf(), Buf()]
    ACCm = [AR[:, 39936 + i * 1024:39936 + (i + 1) * 1024].bitcast(F32) for i in range(2)]
    ACCmb = [Buf(), Buf()]
    TMPm = AR[:, 41984:43008].bitcast(F32)
    TMPmb = Buf()
    wout = AR[:, 18432:18432 + 8192].rearrange("p (k c) -> p k c", k=8)
    woutb = Buf()

    def merge_phase(li):
        tbs = qblocks(li)
        brn = ("w_br_a", "w_br_b", "w_br_c", "w_br_d")
        cnt = 0
        acn = 0
        for jp in range(4):
            wgs = []
            for jl in range(2):
                j = 2 * jp + jl
                i = wcnt[0] % 3
                wcnt[0] += 1
                wg, wgb = wt[i], wtb[i]
                wgs.append((wg, wgb))
                for br in range(4):
                    c0 = O_G + br * 1024 + j * 128
                    fw.dma("pool", wg[:, :, br * 128:(br + 1) * 128],
                           I["w_in"][li][:, c0:c0 + 128].rearrange("(k p) c -> p k c", p=128), writes=[wgb])
                for br in range(4):
                    fw.dma("pool", wbr[jl][:, br * 4:(br + 1) * 4, :],
                           I[brn[br]][li][:, j * 128:(j + 1) * 128].rearrange("(e p) c -> p e c", p=128),
                           writes=[wbrb[jl]])
            for tb in tbs:
                t0, tn = TBLK[tb]
                ob = cnt % 2
                cnt += 1
                fw.dma("sp", OTt[ob][:, :, 0:tn], OTd[:, :, t0:t0 + tn].rearrange("c p q -> p c q"), writes=[OTtb[ob]])
                for jl in range(2):
                    j = 2 * jp + jl
                    wg, wgb = wgs[jl]
                    ac = acn % 2
                    acn += 1
                    for br in range(4):
                        pg = ps_main()
                        for kc in range(8):
                            fw.op("pe", lambda e, pg=pg, kc=kc, br=br, t0=t0, tn=tn, wg=wg: e.matmul(
                                PS[pg][:, 0:tn], wg[:, kc, br * 128:(br + 1) * 128], hT[:, kc, t0:t0 + tn],
                                start=(kc == 0), stop=(kc == 7)),
                                reads=[wgb] + [hTb[t0 // 128 + q] for q in range(tn // 128)], writes=[PSB[pg]],
                                inc=(kc == 7), pe_acc=True)
                        sg = br % 2
                        fw.op("act", lambda e, pg=pg, sg=sg, tn=tn: e.activation(
                            out=SGm[sg][:, 0:tn], in_=PS[pg][:, 0:tn], func=AF.Sigmoid),
                            reads=[PSB[pg]], writes=[SGmb[sg]])
                        pb = ps_aux()
                        for ec in range(4):
                            fw.op("pe", lambda e, pb=pb, ec=ec, br=br, tn=tn, jl=jl, ob=ob: e.matmul(
                                PS[pb][:, 0:tn], wbr[jl][:, br * 4 + ec, :], OTt[ob][:, br * 4 + ec, 0:tn],
                                start=(ec == 0), stop=(ec == 3)),
                                reads=[wbrb[jl], OTtb[ob]], writes=[PSB[pb]], inc=(ec == 3), pe_acc=True)
                        if br == 0:
                            fw.op("dve", lambda e, pb=pb, sg=sg, ac=ac, tn=tn: e.tensor_tensor(
                                out=ACCm[ac][:, 0:tn], in0=PS[pb][:, 0:tn], in1=SGm[sg][:, 0:tn], op=ALU.mult),
                                reads=[PSB[pb], SGmb[sg]], writes=[ACCmb[ac]])
                        else:
                            fw.op("dve", lambda e, pb=pb, sg=sg, tn=tn: e.tensor_tensor(
                                out=TMPm[:, 0:tn], in0=PS[pb][:, 0:tn], in1=SGm[sg][:, 0:tn], op=ALU.mult),
                                reads=[PSB[pb], SGmb[sg]], writes=[TMPmb])
                            if br < 3:
                                fw.op("pool", lambda e, ac=ac, tn=tn: e.tensor_tensor(
                                    out=ACCm[ac][:, 0:tn], in0=ACCm[ac][:, 0:tn], in1=TMPm[:, 0:tn], op=ALU.add),
                                    reads=[TMPmb, ACCmb[ac]], writes=[ACCmb[ac]])
                            else:
                                fw.op("pool", lambda e, ac=ac, tn=tn, j=j, t0=t0: e.tensor_tensor(
                                    out=mT[:, j, t0:t0 + tn], in0=ACCm[ac][:, 0:tn], in1=TMPm[:, 0:tn], op=ALU.add),
                                    reads=[TMPmb, ACCmb[ac]], writes=[mTb[tb]])
        fw.barrier()

    def resid_tile(n, tt, ysrc, ybufs, gidx, xsrc, dsts):
        b = n % 2
        fw.dma("sp", xt[b][:], xsrc, writes=[xtb[b]])
        for half in range(2):
            fw.op("act", lambda e, half=half, b=b: e.activation(
                out=xn[b][:, half * 512:(half + 1) * 512], in_=ysrc[half], func=AF.Square,
                accum_out=stat[:, 24 + half:25 + half]), reads=[ybufs[half]], writes=[xnb[b], statb])
        fw.op("dve", lambda e: e.tensor_tensor(out=stat[:, 26:27], in0=stat[:, 24:25], in1=stat[:, 25:26], op=ALU.add),
              reads=[statb], writes=[statb])
        fw.op("act", lambda e: e.activation(out=stat[:, 27:28], in_=stat[:, 26:27], func=AF.Ln, scale=1.0 / D,
                                            bias=epsc[:, 0:1]), reads=[statb, cb], writes=[statb])
        fw.op("act", lambda e: e.activation(out=stat[:, 27:28], in_=stat[:, 27:28], func=AF.Exp, scale=-0.5),
              reads=[statb], writes=[statb])
        for half in range(2):
            fw.op("dve", lambda e, half=half, b=b: e.scalar_tensor_tensor(
                out=xn[b][:, half * 512:(half + 1) * 512], in0=ysrc[half], scalar=stat[:, 27:28],
                in1=gb[:, gidx, half * 512:(half + 1) * 512], op0=ALU.mult, op1=ALU.mult),
                reads=[ybufs[half], statb, mb], writes=[xnb[b]])
        fw.op("pool", lambda e, b=b: e.tensor_tensor(out=xt[b][:], in0=xt[b][:], in1=xn[b][:], op=ALU.add),
              reads=[xnb[b], xtb[b]], writes=[xtb[b]])
        for d in dsts:
            fw.dma("sp", d, xt[b][:], reads=[xtb[b]])

    def wout_phase(li, xsrc_fn):
        tiles = list(range(NT)) if li == 0 else list(range(2, NT))
        fw.dma("pool", wout[:, :, :], I["w_out"][li].rearrange("(k p) c -> p k c", p=128), writes=[woutb])
        for n, tt in enumerate(tiles):
            pis = []
            for half in range(2):
                pi = ps_main()
                pis.append(pi)
                for kc in range(8):
                    fw.op("pe", lambda e, pi=pi, kc=kc, tt=tt, half=half: e.matmul(
                        PS[pi][:, :], mT[:, kc, tt * 128:(tt + 1) * 128], wout[:, kc, half * 512:(half + 1) * 512],
                        start=(kc == 0), stop=(kc == 7)),
                        reads=[woutb], writes=[PSB[pi]], inc=(kc == 7), pe_acc=True)
            gidx = 1 if tt < 2 else 0
            resid_tile(n, tt, [PS[pis[0]][:, :], PS[pis[1]][:, :]], [PSB[pis[0]], PSB[pis[1]]], gidx,
                       xsrc_fn(tt), [Xd[tt * 128:(tt + 1) * 128, :]])
        fw.barrier()

    NFC = FFN_DENSE // 128
    gTd = AR[:, 0:NFC * 768].rearrange("p (f t) -> p f t", f=NFC)
    gTdb = Buf()
    W2d = AR[:, NFC * 768:NFC * 768 + NFC * 1024].rearrange("p (f c) -> p f c", f=NFC)
    W2db = Buf()
    SLU = [AR[:, NFC * 1792 + i * 512:NFC * 1792 + (i + 1) * 512] for i in range(2)]
    SLUb = [Buf(), Buf()]

    def ffn_dense(li):
        w1, w3, w2 = I["w1_dense"][0], I["w3_dense"][0], I["w2_dense"][0]
        for g0 in range(0, NFC, 8):
            ng = min(8, NFC - g0)
            fw.dma("pool", W2d[:, g0:g0 + ng, :], w2[g0 * 128:(g0 + ng) * 128, :].rearrange("(f p) c -> p f c", p=128),
                   writes=[W2db])
        cnt = 0
        for third in range(3):
            tok0 = third * 768
            for g0 in range(0, NFC, 4):
                ng = min(4, NFC - g0)
                wa, wab = load_w(w1[:, g0 * 128:(g0 + ng) * 128], ng * 128)
                wc, wcb = load_w(w3[:, g0 * 128:(g0 + ng) * 128], ng * 128)
                for c in range(ng):
                    fc = g0 + c
                    for (s0, sn) in ((0, 512), (512, 256)):
                        t0 = tok0 + s0
                        rb = [hTb[t0 // 128 + q] for q in range(sn // 128)]
                        pa_, pb_ = ps_main(), ps_main()
                        for (pp, ww, wwb) in ((pa_, wa, wab), (pb_, wc, wcb)):
                            for kc in range(8):
                                fw.op("pe", lambda e, pp=pp, ww=ww, kc=kc, c=c, t0=t0, sn=sn: e.matmul(
                                    PS[pp][:, 0:sn], ww[:, kc, c * 128:(c + 1) * 128], hT[:, kc, t0:t0 + sn],
                                    start=(kc == 0), stop=(kc == 7)),
                                    reads=[wwb] + rb, writes=[PSB[pp]], inc=(kc == 7), pe_acc=True)
                        sl = cnt % 2
                        cnt += 1
                        fw.op("act", lambda e, pa_=pa_, sl=sl, sn=sn: e.activation(out=SLU[sl][:, 0:sn], in_=PS[pa_][:, 0:sn],
                                                                                 func=AF.Silu),
                              reads=[PSB[pa_]], writes=[SLUb[sl]])
                        fw.op("dve", lambda e, pb_=pb_, sl=sl, sn=sn, fc=fc, s0=s0: e.tensor_tensor(
                            out=gTd[:, fc, s0:s0 + sn], in0=PS[pb_][:, 0:sn], in1=SLU[sl][:, 0:sn], op=ALU.mult),
                            reads=[PSB[pb_], SLUb[sl]], writes=[gTdb])
            for q in range(6):
                tt = third * 6 + q
                pis = []
                for half in range(2):
                    pi = ps_main()
                    pis.append(pi)
                    for fc in range(NFC):
                        fw.op("pe", lambda e, pi=pi, fc=fc, q=q, half=half: e.matmul(
                            PS[pi][:, :], gTd[:, fc, q * 128:(q + 1) * 128], W2d[:, fc, half * 512:(half + 1) * 512],
                            start=(fc == 0), stop=(fc == NFC - 1)),
                            reads=[gTdb, W2db], writes=[PSB[pi]], inc=(fc == NFC - 1), pe_acc=True)
                gidx = 3 if tt < 2 else 2
                resid_tile(q, tt, [PS[pis[0]][:, :], PS[pis[1]][:, :]], [PSB[pis[0]], PSB[pis[1]]], gidx,
                           Xd[tt * 128:(tt + 1) * 128, :], [Xd[tt * 128:(tt + 1) * 128, :]])
        fw.barrier()

    NFE = FFN_EXPERT // 128
    oacc = AR[:, 0:32768].bitcast(F32).rearrange("p (t c) -> p t c", t=16)
    oaccb = [Buf() for _ in range(16)]
    W2m = [AR[:, 32768 + i * 4096:32768 + (i + 1) * 4096].rearrange("p (f c) -> p f c", f=4) for i in range(2)]
    W2mb = [Buf(), Buf()]
    SLm = [AR[:, 40960 + i * 512:40960 + (i + 1) * 512] for i in range(2)]
    SLmb = [Buf(), Buf()]
    comb = AR[:, 41984:41984 + 256].bitcast(F32).rearrange("p (t e) -> p t e", t=16)
    combb = Buf()
    gTm = [cos16, sin16, cos8, sin8]
    gTmb = [Buf() for _ in range(4)]
    LT = [(C + i * 512, 512) for i in range(4)]

    def moe_router():
        wr, wrb = load_w(I["w_router"][0], 8)
        A_, B_ = stat[:, 28:36], stat[:, 36:44]
        for t in range(16):
            tok = C + t * 128
            pi = ps_main()
            for kc in range(8):
                fw.op("pe", lambda e, pi=pi, kc=kc, tok=tok: e.matmul(
                    PS[pi][:, 0:8], hT[:, kc, tok:tok + 128], wr[:, kc, 0:8], start=(kc == 0), stop=(kc == 7)),
                    reads=[wrb, hTb[2 + t]], writes=[PSB[pi]], inc=(kc == 7), pe_acc=True)
            fw.op("dve", lambda e, pi=pi: e.tensor_copy(out=A_, in_=PS[pi][:, 0:8]), reads=[PSB[pi]], writes=[statb])
            fw.op("dve", lambda e: e.reduce_max(out=stat[:, 44:45], in_=A_, axis=AX.X), reads=[statb], writes=[statb])
            fw.op("dve", lambda e: e.tensor_scalar(out=B_, in0=A_, scalar1=stat[:, 44:45], scalar2=None,
                                                   op0=ALU.is_equal), reads=[statb], writes=[statb])
            fw.op("dve", lambda e: e.scalar_tensor_tensor(out=A_, in0=B_, scalar=-1e30, in1=A_, op0=ALU.mult,
                                                          op1=ALU.add), reads=[statb], writes=[statb])
            fw.op("dve", lambda e: e.reduce_max(out=stat[:, 45:46], in_=A_, axis=AX.X), reads=[statb], writes=[statb])
            fw.op("dve", lambda e: e.tensor_scalar(out=A_, in0=A_, scalar1=stat[:, 45:46], scalar2=None,
                                                   op0=ALU.is_equal), reads=[statb], writes=[statb])
            fw.op("dve", lambda e: e.tensor_tensor(out=stat[:, 46:47], in0=stat[:, 45:46], in1=stat[:, 44:45],
                                                   op=ALU.subtract), reads=[statb], writes=[statb])
            fw.op("act", lambda e: e.activation(out=stat[:, 47:48], in_=stat[:, 46:47], func=AF.Exp),
                  reads=[statb], writes=[statb])
            fw.op("dve", lambda e: e.tensor_scalar(out=stat[:, 48:49], in0=stat[:, 47:48], scalar1=1.0, scalar2=None,
                                                   op0=ALU.add), reads=[statb], writes=[statb])
            fw.op("dve", lambda e: e.reciprocal(out=stat[:, 48:49], in_=stat[:, 48:49]), reads=[statb], writes=[statb])
            fw.op("dve", lambda e: e.tensor_tensor(out=stat[:, 49:50], in0=stat[:, 47:48], in1=stat[:, 48:49],
                                                   op=ALU.mult), reads=[statb], writes=[statb])
            fw.op("dve", lambda e: e.tensor_scalar(out=B_, in0=B_, scalar1=stat[:, 48:49], scalar2=None,
                                                   op0=ALU.mult), reads=[statb], writes=[statb])
            fw.op("dve", lambda e, t=t: e.scalar_tensor_tensor(out=comb[:, t, :], in0=A_, scalar=stat[:, 49:50],
                                                              in1=B_, op0=ALU.mult, op1=ALU.add),
                  reads=[statb], writes=[combb])

    def moe_phase():
        moe_router()
        w1, w3, w2 = I["w1_moe"][0], I["w3_moe"][0], I["w2_moe"][0]
        cnt = 0
        first = True
        gi = 0
        for ex in range(N_EXPERTS):
            for g0 in range(0, NFE, 4):
                wa, wab = load_w(w1[ex][:, g0 * 128:(g0 + 4) * 128], 512)
                wc, wcb = load_w(w3[ex][:, g0 * 128:(g0 + 4) * 128], 512)
                wi = gi % 2
                gi += 1
                fw.dma("pool", W2m[wi][:, :, :], w2[ex][g0 * 128:(g0 + 4) * 128, :].rearrange("(f p) c -> p f c", p=128),
                       writes=[W2mb[wi]])
                for c in range(4):
                    for (t0, tn) in LT:
                        rb = [hTb[t0 // 128 + q] for q in range(4)]
                        pa_, pb_ = ps_main(), ps_main()
                        for (pp, ww, wwb) in ((pa_, wa, wab), (pb_, wc, wcb)):
                            for kc in range(8):
                                fw.op("pe", lambda e, pp=pp, ww=ww, kc=kc, c=c, t0=t0: e.matmul(
                                    PS[pp][:, :], ww[:, kc, c * 128:(c + 1) * 128], hT[:, kc, t0:t0 + 512],
                                    start=(kc == 0), stop=(kc == 7)),
                                    reads=[wwb] + rb, writes=[PSB[pp]], inc=(kc == 7), pe_acc=True)
                        sl = cnt % 2
                        cnt += 1
                        fw.op("act", lambda e, pa_=pa_, sl=sl: e.activation(out=SLm[sl][:, :], in_=PS[pa_][:, :],
                                                                          func=AF.Silu),
                              reads=[PSB[pa_]], writes=[SLmb[sl]])
                        fw.op("dve", lambda e, pb_=pb_, sl=sl, c=c, t0=t0: e.tensor_tensor(
                            out=gTm[c][:, t0 - C:t0 - C + 512], in0=PS[pb_][:, :], in1=SLm[sl][:, :], op=ALU.mult),
                            reads=[PSB[pb_], SLmb[sl]], writes=[gTmb[c]])
                for t in range(16):
                    for half in range(2):
                        pi = ps_aux()
                        for c in range(4):
                            fw.op("pe", lambda e, pi=pi, c=c, t=t, half=half, wi=wi: e.matmul(
                                PS[pi][:, :], gTm[c][:, t * 128:(t + 1) * 128], W2m[wi][:, c, half * 512:(half + 1) * 512],
                                start=(c == 0), stop=(c == 3)),
                                reads=[gTmb[c], W2mb[wi]], writes=[PSB[pi]], inc=(c == 3), pe_acc=True)
                        if first:
                            fw.op("dve", lambda e, pi=pi, t=t, half=half, ex=ex: e.tensor_scalar(
                                out=oacc[:, t, half * 512:(half + 1) * 512], in0=PS[pi][:, :],
                                scalar1=comb[:, t, ex:ex + 1], scalar2=None, op0=ALU.mult),
                                reads=[PSB[pi], combb], writes=[oaccb[t]])
                        else:
                            fw.op("dve", lambda e, pi=pi, t=t, half=half, ex=ex: e.scalar_tensor_tensor(
                                out=oacc[:, t, half * 512:(half + 1) * 512], in0=PS[pi][:, :],
                                scalar=comb[:, t, ex:ex + 1], in1=oacc[:, t, half * 512:(half + 1) * 512],
                                op0=ALU.mult, op1=ALU.add),
                                reads=[PSB[pi], combb, oaccb[t]], writes=[oaccb[t]])
                first = False
        for t in range(16):
            tt = 2 + t
            resid_tile(t, tt, [oacc[:, t, 0:512], oacc[:, t, 512:1024]], [oaccb[t], oaccb[t]], 2,
                       Xd[tt * 128:(tt + 1) * 128, :], [out[t * 128:(t + 1) * 128, :]])
        fw.barrier()

    def layer1():
        xs = lambda tt: Xd[tt * 128:(tt + 1) * 128, :]
        layer_vectors(1)
        fw.barrier()
        norm_phase(1, 0, xs, list(range(NT)))
        branch_a(1)
        branch_b(1)
        branch_c(1)
        branch_d(1)
        fw.mark("t_l1attn")
        merge_phase(1)
        wout_phase(1, xs)
        fw.mark("t_l1mix")
        norm_phase(1, 1, xs, list(range(2, NT)))
        moe_phase()

    def layer0():
        branch_a(0)
        fw.mark("t_a")
        branch_b(0)
        fw.mark("t_b")
        branch_c(0)
        fw.mark("t_c")
        branch_d(0)
        fw.mark("t_l0attn")
        merge_phase(0)
        wout_phase(0, x_src0)
        fw.mark("t_l0mix")
        norm_phase(0, 1, lambda tt: Xd[tt * 128:(tt + 1) * 128, :], list(range(NT)))
        ffn_dense(0)
        fw.mark("t_l0")

    layer_vectors(0)
    build_rope(cos16, sin16, 16)
    build_rope(cos8, sin8, 8)
    fw.barrier()
    fw.mark("lv")
    norm_phase(0, 0, x_src0, list(range(NT)))
    fw.mark("norm")
    if dbg is None or dbg["what"] == "full" or (dbg.get("stop") or "")[:2] in ("t_", "a_"):
        fw.mark("t_pre")
        layer0()
        layer1()
    if dbg is not None and dbg["what"] in ("x1", "x2"):
        branch_a(0)
        branch_b(0)
        branch_c(0)
        branch_d(0)
        merge_phase(0)
        wout_phase(0, x_src0)
        if dbg["what"] == "x2":
            norm_phase(0, 1, lambda tt: Xd[tt * 128:(tt + 1) * 128, :], list(range(NT)))
            fw.barrier()
            ffn_dense(0)
    if dbg is not None and dbg["what"] in ("oc", "qkc"):
        branch_c(0)
    if dbg is not None and dbg["what"] == "oa":
        branch_a(0)
    if dbg is not None and (dbg["what"] == "ob" or (dbg.get("stop") or "").startswith("b")):
        branch_b(0)
    if dbg is not None and dbg["what"] == "od":
        branch_d(0)

    fw.frozen = False
    fw.barrier()
    dcnt = [0]

    def dump(src, dst, rb=()):
        p, n = src.shape[0], src.shape[1]
        for c0 in range(0, n, 1024):
            w = min(1024, n - c0)
            b = dcnt[0] % 2
            dcnt[0] += 1
            fw.op("dve", lambda e, b=b, c0=c0, w=w: e.tensor_copy(out=xn[b][0:p, 0:w], in_=src[:, c0:c0 + w]),
                  reads=list(rb), writes=[xnb[b]])
            fw.dma("sp", dst[:, c0:c0 + w], xn[b][0:p, 0:w], reads=[xnb[b]])

    if dbg is not None and dbg["what"] in ("x1", "x2"):
        for tt in range(NT):
            b = tt % 2
            fw.dma("sp", xt[b][:], Xd[tt * 128:(tt + 1) * 128, :], writes=[xtb[b]])
            fw.dma("sp", dbg_out[tt * 128:(tt + 1) * 128, :], xt[b][:], reads=[xtb[b]])
    if dbg is not None and dbg["what"] == "bpv":
        for i in (3, 4):
            dump(PS[i][:, 0:260], dbg_out[i * 128:(i + 1) * 128, 0:260])
    if dbg is not None and dbg["what"] == "stage":
        dump(ident[:, :], dbg_out[:, :])
    if dbg is not None and dbg["what"] == "qkc":
        for c in range(4):
            dump(QT[:, c, :], dbg_out[c * 128:(c + 1) * 128, :])
        dump(KT[:, 0, :], dbg_out[512:640, :])
        dump(KT[:, 1, :], dbg_out[640:768, :])
    if dbg is not None and dbg["what"] == "rope":
        dump(cos16[:, :], dbg_out[0:128, :])
        dump(sin16[:, :], dbg_out[128:256, :])
        dump(pm16[:, :], dbg_out[256:384, 0:128])
        dump(cos8[:, :], dbg_out[384:512, :])
        dump(sin8[:, :], dbg_out[512:640, :])
        dump(pm8[:, :], dbg_out[640:768, 0:128])
    if dbg is not None and dbg["what"] == "hT":
        for kc in range(8):
            dump(hT[:, kc, :], dbg_out[kc * 128:(kc + 1) * 128, :])
    if dbg is not None and dbg["what"] in ("oa", "ob", "oc", "od"):
        br = "abcd".index(dbg["what"][1])
        for ec in range(4):
            for c0 in range(0, T, 512):
                w = min(512, T - c0)
                fw.dma("sp", OTs[0][:, 0, 0:w], OTd[br * 4 + ec, :, c0:c0 + w], writes=[OTsb[0]])
                dump(OTs[0][:, 0, 0:w], dbg_out[ec * 128:(ec + 1) * 128, c0:c0 + w], rb=[OTsb[0]])
    fw.barrier(only=["sp"])
    fw.emit()


def make_in_maps(inputs, cores):
    maps = []
    shared = {}
    for k, v in inputs.items():
        if k in ("x", "c", "ctx", "c_ctx"):
            continue
        a = np.ascontiguousarray(np.asarray(v, dtype=np.float32))
        shared[k] = a
    for b in cores:
        m = dict(shared)
        m["x"] = np.ascontiguousarray(np.asarray(inputs["x"][b], dtype=np.float32))
        m["ctx"] = np.ascontiguousarray(np.asarray(inputs["ctx"][b], dtype=np.float32))
        m["cvec"] = np.ascontiguousarray(
            np.stack([np.asarray(inputs["c"][b]), np.asarray(inputs["c_ctx"])]).astype(np.float32))
        maps.append(m)
    return maps


_NC_CACHE = {}


def kernel(**inputs):
    if "nc" not in _NC_CACHE:
        _NC_CACHE["nc"] = build_program()
    nc = _NC_CACHE["nc"]
    maps = make_in_maps(inputs, list(range(8)))
    res = run_bass_kernel_spmd(nc, maps, core_ids=list(range(8)))
    return np.stack([np.asarray(r["out"], dtype=np.float32) for r in res.results], axis=0)
```
